# Optimizing a Trainium2 kernel written in Bass

```python
import jax
import jax.numpy as jnp
from jax import lax
import numpy as np

D_MODEL = 1024
BATCH = 2
SEQ = 8192
DEPTH = 2

GRID_W = 64
CTX_LEN = 256
EPS = 1e-6

GLA_HEADS = 4
GLA_DK = 64
GLA_DV = 128
GLA_GATE_RANK = 16
GLA_GATE_NORM = 16.0
GLA_CHUNK = 64

MLA_HEADS = 4
MLA_NOPE = 128
MLA_ROPE = 64
MLA_V = 128
MLA_Q_RANK = 256
MLA_KV_RANK = 128
MLA_SCALE = (MLA_NOPE + MLA_ROPE) ** -0.5
ROPE_BASE = 10000.0
Q_BLOCK = 128

GLA_QK_W = GLA_HEADS * GLA_DK
GLA_V_W = GLA_HEADS * GLA_DV
MLA_V_W = MLA_HEADS * MLA_V
D_MIX = GLA_V_W + MLA_V_W

IN_PARTS = (('gla_q', GLA_QK_W), ('gla_k', GLA_QK_W), ('gla_v', GLA_V_W),
            ('gla_gf', GLA_GATE_RANK), ('gla_gb', GLA_GATE_RANK), ('gla_r', GLA_V_W),
            ('mla_cq', MLA_Q_RANK), ('mla_ckv', MLA_KV_RANK), ('mla_kr', MLA_ROPE))
D_IN = sum(w for _, w in IN_PARTS)
ALL_PARTS = tuple(n for n, _ in IN_PARTS)
CTX_STATE_PARTS = ('gla_k', 'gla_v', 'gla_gf', 'gla_gb', 'mla_ckv', 'mla_kr')

D_FF_DENSE = 2816
N_EXPERTS = 8
TOP_K = 2
D_FF_EXPERT = 3584
N_DENSE = (DEPTH + 1) // 2
N_MOE = DEPTH // 2

kernel_name = 'hybrid_gla_mla_moe_diffusion_block'


def rmsnorm(x, g):
    xf = x.astype(jnp.float32)
    y = xf * lax.rsqrt(jnp.mean(xf * xf, axis=-1, keepdims=True) + EPS)
    return (y * g.astype(jnp.float32)).astype(x.dtype)


def in_proj(h, w_in, names):
    offs, o = {}, 0
    for n, w in IN_PARTS:
        offs[n] = (o, o + w)
        o += w
    w = jnp.concatenate([w_in[:, offs[n][0]:offs[n][1]] for n in names], axis=1)
    y = h @ w
    sizes = [offs[n][1] - offs[n][0] for n in names]
    splits = np.cumsum(sizes)[:-1].tolist()
    return dict(zip(names, jnp.split(y, splits, axis=-1)))


def axial_rope_tables(n_tok):
    rows = n_tok // GRID_W
    row = jnp.repeat(jnp.arange(rows, dtype=jnp.float32), GRID_W)
    col = jnp.tile(jnp.arange(GRID_W, dtype=jnp.float32), rows)
    nfreq = MLA_ROPE // 4
    inv = ROPE_BASE ** (-jnp.arange(nfreq, dtype=jnp.float32) / nfreq)
    ar = row[:, None] * inv
    ac = col[:, None] * inv
    return (jnp.cos(ar), jnp.sin(ar), jnp.cos(ac), jnp.sin(ac))


def rope_half(x, cos, sin):
    x1, x2 = jnp.split(x, 2, axis=-1)
    return jnp.concatenate([x1 * cos - x2 * sin, x2 * cos + x1 * sin], axis=-1)


def axial_rope(x, tabs):
    cr, sr, cc, sc = tabs
    extra = x.ndim - 3
    shp = lambda t: t.reshape(t.shape[:1] + (1,) * extra + t.shape[1:]).astype(x.dtype)
    xr, xc = jnp.split(x, 2, axis=-1)
    return jnp.concatenate([rope_half(xr, shp(cr), shp(sr)), rope_half(xc, shp(cc), shp(sc))], axis=-1)


def bidir(a):
    return jnp.stack([a, jnp.flip(a, axis=-3)])


def gla_q(p):
    b, t = p.shape[:2]
    return p.reshape(b, t, GLA_HEADS, GLA_DK).astype(jnp.float32) * (GLA_DK ** -0.5)


def gla_kvg(p, w_g2, b_g2):
    b, t = p['gla_k'].shape[:2]
    k = p['gla_k'].reshape(b, t, GLA_HEADS, GLA_DK).astype(jnp.float32)
    v = p['gla_v'].reshape(b, t, GLA_HEADS, GLA_DV).astype(jnp.float32)
    codes = jnp.stack([p['gla_gf'], p['gla_gb']]).astype(jnp.float32)
    pre = jnp.einsum('zbtr,zrk->zbtk', codes, w_g2.astype(jnp.float32)) + b_g2.astype(jnp.float32)[:, None, None, :]
    g = (jax.nn.log_sigmoid(pre) / GLA_GATE_NORM).reshape(2, b, t, GLA_HEADS, GLA_DK)
    g = jnp.stack([g[0], jnp.flip(g[1], axis=-3)])
    return bidir(k), bidir(v), g


def gla_chunked(q, k, v, g, s0):
    t = q.shape[-3]
    n = t // GLA_CHUNK

    def to_chunks(a):
        a = a.reshape(a.shape[:-3] + (n, GLA_CHUNK) + a.shape[-2:])
        return jnp.swapaxes(jnp.moveaxis(a, -4, 0), -3, -2)

    qc, kc, vc, gc = to_chunks(q), to_chunks(k), to_chunks(v), to_chunks(g)
    bc = jnp.cumsum(gc, axis=-2)
    mask = jnp.tril(jnp.ones((GLA_CHUNK, GLA_CHUNK), dtype=bool))[:, :, None]

    def step(s, xs):
        qi, ki, vi, bi = xs
        b_last = bi[..., -1:, :]
        o_inter = jnp.einsum('...hcd,...hde->...hce', qi * jnp.exp(bi), s)
        diff = bi[..., :, None, :] - bi[..., None, :, :]
        decay = jnp.exp(jnp.where(mask, diff, -jnp.inf))
        attn = jnp.einsum('...htd,...hsd,...htsd->...hts', qi, ki, decay)
        o_intra = jnp.einsum('...hts,...hse->...hte', attn, vi)
        s_new = s * jnp.exp(b_last[..., 0, :])[..., None] + jnp.einsum('...hcd,...hce->...hde', ki * jnp.exp(b_last - bi), vi)
        return s_new, o_inter + o_intra

    s_fin, oc = lax.scan(step, s0, (qc, kc, vc, bc))
    o = jnp.moveaxis(jnp.swapaxes(oc, -3, -2), 0, -4)
    return o.reshape(o.shape[:-4] + (t,) + o.shape[-2:]), s_fin


def gla_final_state(k, v, g):
    b = jnp.cumsum(g, axis=-3)
    w = jnp.exp(b[..., -1:, :, :] - b)
    return jnp.einsum('...thd,...the->...hde', k * w, v)


def gla_out(o_dirs, r, gla_g):
    o = o_dirs[0] + jnp.flip(o_dirs[1], axis=-3)
    o = rmsnorm(o, gla_g)
    b, t = o.shape[:2]
    return (o.reshape(b, t, GLA_V_W) * jax.nn.silu(r.astype(jnp.float32))).astype(r.dtype)


def mla_q(cq, qn_g, w_uq, tabs):
    b, t = cq.shape[:2]
    q = (rmsnorm(cq, qn_g) @ w_uq).reshape(b, t, MLA_HEADS, MLA_NOPE + MLA_ROPE)
    if tabs is not None:
        q = jnp.concatenate([q[..., :MLA_NOPE], axial_rope(q[..., MLA_NOPE:], tabs)], axis=-1)
    return q


def mla_kv(p, kvn_g, w_ukv, tabs):
    b, t = p['mla_ckv'].shape[:2]
    kv = (rmsnorm(p['mla_ckv'], kvn_g) @ w_ukv).reshape(b, t, MLA_HEADS, MLA_NOPE + MLA_V)
    k_nope, v = kv[..., :MLA_NOPE], kv[..., MLA_NOPE:]
    kr = p['mla_kr']
    if tabs is not None:
        kr = axial_rope(kr, tabs)
    k = jnp.concatenate([k_nope, jnp.broadcast_to(kr[:, :, None, :], (b, t, MLA_HEADS, MLA_ROPE))], axis=-1)
    return k, v


def dense_attn(q, k, v):
    s = jnp.einsum('bqhd,bkhd->bhqk', q, k).astype(jnp.float32) * MLA_SCALE
    p = jax.nn.softmax(s, axis=-1).astype(v.dtype)
    return jnp.einsum('bhqk,bkhe->bqhe', p, v)


def block_attention(q, k, v):
    b, t, h, dq = q.shape
    nb = t // Q_BLOCK
    qb = jnp.moveaxis(q.reshape(b, nb, Q_BLOCK, h, dq), 1, 0)
    o = lax.map(lambda blk: dense_attn(blk, k, v), qb)
    return jnp.moveaxis(o, 0, 1).reshape(b, t, h * v.shape[-1])


def token_mixing(hl, hc, w_in, w_g2, b_g2, gla_g, qn_g, w_uq, kvn_g, w_ukv, w_out, tabs, need_ctx):
    b, t = hl.shape[:2]
    pl = in_proj(hl, w_in, ALL_PARTS)
    pc = in_proj(hc, w_in, ALL_PARTS if need_ctx else CTX_STATE_PARTS)

    kc, vc, gc = gla_kvg(pc, w_g2, b_g2)
    kl, vl, gl = gla_kvg(pl, w_g2, b_g2)
    if need_ctx:
        s0 = jnp.zeros((2, b, GLA_HEADS, GLA_DK, GLA_DV), jnp.float32)
        oc_dirs, s_ctx = gla_chunked(bidir(gla_q(pc['gla_q'])), kc, vc, gc, s0)
    else:
        s_ctx = gla_final_state(kc, vc, gc)
    ol_dirs, _ = gla_chunked(bidir(gla_q(pl['gla_q'])), kl, vl, gl, s_ctx)

    kl_m, vl_m = mla_kv(pl, kvn_g, w_ukv, tabs)
    kc_m, vc_m = mla_kv(pc, kvn_g, w_ukv, None)
    ql_m = mla_q(pl['mla_cq'], qn_g, w_uq, tabs)
    keys = jnp.concatenate([kl_m, kc_m], axis=1)
    vals = jnp.concatenate([vl_m, vc_m], axis=1)
    ml = block_attention(ql_m, keys, vals)

    yl = jnp.concatenate([gla_out(ol_dirs, pl['gla_r'], gla_g), ml], axis=-1) @ w_out
    if need_ctx:
        qc_m = mla_q(pc['mla_cq'], qn_g, w_uq, None)
        mc = dense_attn(qc_m, kc_m, vc_m).reshape(b, hc.shape[1], MLA_V_W)
        yc = jnp.concatenate([gla_out(oc_dirs, pc['gla_r'], gla_g), mc], axis=-1) @ w_out
        return yl, yc
    return yl, None


def swiglu(h, w_gate, w_up, w_down):
    return (jax.nn.silu(h @ w_gate) * (h @ w_up)) @ w_down


def moe_swiglu(h, w_router, e_gate, e_up, e_down):
    logits = jnp.einsum('btd,de->bte', h, w_router).astype(jnp.float32)
    top_v, top_i = lax.top_k(logits, TOP_K)
    wts = jax.nn.softmax(top_v, axis=-1)
    comb = jnp.einsum('btk,btke->bte', wts, jax.nn.one_hot(top_i, N_EXPERTS, dtype=jnp.float32)).astype(h.dtype)
    out = jnp.zeros_like(h)
    for e in range(N_EXPERTS):
        out = out + comb[..., e:e + 1] * swiglu(h, e_gate[e], e_up[e], e_down[e])
    return out


def channel_mix(h, i, ffn_w_gate, ffn_w_up, ffn_w_down, router_w, exp_w_gate, exp_w_up, exp_w_down):
    j = i // 2
    if i % 2 == 0:
        return swiglu(h, ffn_w_gate[j], ffn_w_up[j], ffn_w_down[j])
    return moe_swiglu(h, router_w[j], exp_w_gate[j], exp_w_up[j], exp_w_down[j])


def setup_inputs(seed: int = 0) -> dict:
    key = jax.random.key(seed)
    ks = iter(jax.random.split(key, 32))
    nrm = lambda shape, scale: jax.random.normal(next(ks), shape, jnp.float32) * scale
    gain = lambda shape: 1.0 + 0.02 * jax.random.normal(next(ks), shape, jnp.float32)
    D = D_MODEL
    return {
        'x': nrm((BATCH, SEQ, D), 1.0),
        'c': nrm((BATCH, D), 1.0),
        'ctx': nrm((BATCH, CTX_LEN, D), 1.0),
        'c_ctx': nrm((D,), 1.0),
        'w_mod': nrm((DEPTH, D, 6 * D), 0.5 * D ** -0.5),
        'b_mod': nrm((DEPTH, 6 * D), 0.02),
        'ln1_g': gain((DEPTH, D)),
        'ln2_g': gain((DEPTH, D)),
        'w_in': nrm((DEPTH, D, D_IN), D ** -0.5),
        'w_gla_g2': nrm((DEPTH, 2, GLA_GATE_RANK, GLA_QK_W), GLA_GATE_RANK ** -0.5),
        'b_gla_g2': nrm((DEPTH, 2, GLA_QK_W), 0.1),
        'gla_norm_g': gain((DEPTH, GLA_DV)),
        'mla_q_norm_g': gain((DEPTH, MLA_Q_RANK)),
        'w_uq': nrm((DEPTH, MLA_Q_RANK, MLA_HEADS * (MLA_NOPE + MLA_ROPE)), MLA_Q_RANK ** -0.5),
        'mla_kv_norm_g': gain((DEPTH, MLA_KV_RANK)),
        'w_ukv': nrm((DEPTH, MLA_KV_RANK, MLA_HEADS * (MLA_NOPE + MLA_V)), MLA_KV_RANK ** -0.5),
        'w_out': nrm((DEPTH, D_MIX, D), D_MIX ** -0.5),
        'ffn_w_gate': nrm((N_DENSE, D, D_FF_DENSE), D ** -0.5),
        'ffn_w_up': nrm((N_DENSE, D, D_FF_DENSE), D ** -0.5),
        'ffn_w_down': nrm((N_DENSE, D_FF_DENSE, D), D_FF_DENSE ** -0.5),
        'router_w': nrm((N_MOE, D, N_EXPERTS), D ** -0.5),
        'exp_w_gate': nrm((N_MOE, N_EXPERTS, D, D_FF_EXPERT), D ** -0.5),
        'exp_w_up': nrm((N_MOE, N_EXPERTS, D, D_FF_EXPERT), D ** -0.5),
        'exp_w_down': nrm((N_MOE, N_EXPERTS, D_FF_EXPERT, D), D_FF_EXPERT ** -0.5),
        'final_norm_g': gain((D,)),
    }


def reference(x, c, ctx, c_ctx, w_mod, b_mod, ln1_g, ln2_g, w_in, w_gla_g2, b_gla_g2, gla_norm_g,
              mla_q_norm_g, w_uq, mla_kv_norm_g, w_ukv, w_out, ffn_w_gate, ffn_w_up, ffn_w_down,
              router_w, exp_w_gate, exp_w_up, exp_w_down, final_norm_g):
    tabs = axial_rope_tables(x.shape[1])
    xl, xc = x, ctx
    for i in range(DEPTH):
        need_ctx = i < DEPTH - 1
        mod_l = (jax.nn.silu(c) @ w_mod[i] + b_mod[i])[:, None, :]
        mod_c = (jax.nn.silu(c_ctx) @ w_mod[i] + b_mod[i])[None, None, :]
        sh1, sc1, g1, sh2, sc2, g2 = jnp.split(mod_l, 6, axis=-1)
        ch1, cs1, cg1, ch2, cs2, cg2 = jnp.split(mod_c, 6, axis=-1)

        hl = rmsnorm(xl, ln1_g[i]) * (1.0 + sc1) + sh1
        hc = rmsnorm(xc, ln1_g[i]) * (1.0 + cs1) + ch1
        yl, yc = token_mixing(hl, hc, w_in[i], w_gla_g2[i], b_gla_g2[i], gla_norm_g[i],
                              mla_q_norm_g[i], w_uq[i], mla_kv_norm_g[i], w_ukv[i], w_out[i],
                              tabs, need_ctx)
        xl = xl + g1 * yl

        hl2 = rmsnorm(xl, ln2_g[i]) * (1.0 + sc2) + sh2
        xl = xl + g2 * channel_mix(hl2, i, ffn_w_gate, ffn_w_up, ffn_w_down,
                                   router_w, exp_w_gate, exp_w_up, exp_w_down)
        if need_ctx:
            xc = xc + cg1 * yc
            hc2 = rmsnorm(xc, ln2_g[i]) * (1.0 + cs2) + ch2
            xc = xc + cg2 * channel_mix(hc2, i, ffn_w_gate, ffn_w_up, ffn_w_down,
                                        router_w, exp_w_gate, exp_w_up, exp_w_down)
    return rmsnorm(xl, final_norm_g)
```

```python
import math
from contextlib import ExitStack

import numpy as np
import ml_dtypes

import concourse.bass as bass
import concourse.mybir as mybir
from concourse.bass_utils import run_bass_kernel_spmd

F32 = mybir.dt.float32
BF16 = mybir.dt.bfloat16
AF = mybir.ActivationFunctionType
ALU = mybir.AluOpType

D = 1024
NL = 2048
NCX = 256
NT = NL + NCX
DEPTH = 2
EPS = 1e-6
DIN = 2016
DINA = 2080
NKEY = NCX + 4 * NL
NKT = NKEY // 128
DFF0 = 2816
DFFE = 3584
NEXP = 8
MLA_SCALE = (128 + 64) ** -0.5

ENG_NAMES = ("pe", "act", "dve", "pool", "sp")


class Op:
    __slots__ = ("eng", "fn", "reads", "writes", "is_dma", "idx", "waits", "marked",
                 "semval", "dma_sem", "dma_val")

    def __init__(self, eng, fn, reads, writes, is_dma):
        self.eng = eng
        self.fn = fn
        self.reads = reads
        self.writes = writes
        self.is_dma = is_dma
        self.waits = []
        self.marked = False
        self.semval = 0
        self.dma_sem = None
        self.dma_val = 0


class Sched:
    def __init__(self, nc, n_dma_sems=12, same_engine_sync=True):
        self.nc = nc
        self.ops = {e: [] for e in ENG_NAMES}
        self.order = []
        self.n_dma_sems = n_dma_sems
        self.same_engine_sync = same_engine_sync
        self.cc_ops = set()

    muted = False

    def op(self, eng, fn, reads=(), writes=()):
        writes = tuple(writes) + tuple(k for k in reads if k.startswith("pb") and k not in writes)
        o = Op(eng, fn, tuple(reads), tuple(writes), False)
        if self.muted:
            return o
        o.idx = len(self.ops[eng])
        self.ops[eng].append(o)
        self.order.append(o)
        return o

    def dma(self, eng, fn, reads=(), writes=()):
        o = Op(eng, fn, tuple(reads), tuple(writes), True)
        if self.muted:
            return o
        o.idx = len(self.ops[eng])
        self.ops[eng].append(o)
        self.order.append(o)
        return o

    def barrier(self):
        self.order.append("BARRIER")

    def coll(self, eng, fn, reads=(), writes=()):
        o = self.dma(eng, fn, reads, writes)
        self.cc_ops.add(id(o))
        return o

    def analyze(self):
        last_w = {}
        readers = {}
        seen = {e: {f: -1 for f in ENG_NAMES} for e in ENG_NAMES}
        seen_dma = {e: set() for e in ENG_NAMES}
        dma_ctr = {e: 0 for e in ENG_NAMES}
        dma_cnt = {}
        dma_last = {}
        bar_deps = []
        bar_pending = {e: False for e in ENG_NAMES}
        last_op = {}
        for o in self.order:
            if isinstance(o, str):
                bar_deps = list(last_op.values()) + list(dma_last.values())
                bar_pending = {e: True for e in ENG_NAMES}
                continue
            deps = []
            if bar_pending[o.eng]:
                deps.extend(bar_deps)
                bar_pending[o.eng] = False
            if not o.is_dma:
                last_op[o.eng] = o
            for k in o.reads:
                w = last_w.get(k)
                if w is not None:
                    deps.append(w)
            for k in o.writes:
                w = last_w.get(k)
                if w is not None:
                    deps.append(w)
                for r in readers.get(k, ()):
                    deps.append(r)
            if o.is_dma:
                e = o.eng
                if id(o) in self.cc_ops:
                    sidx = ("cc", 0)
                    inc = 1
                else:
                    sidx = (e, dma_ctr[e] % self.n_dma_sems)
                    dma_ctr[e] += 1
                    inc = 16
                prev = dma_last.get(sidx)
                if prev is not None:
                    deps.append(prev)
                dma_cnt[sidx] = dma_cnt.get(sidx, 0) + inc
                o.dma_sem = sidx
                o.dma_val = dma_cnt[sidx]
                dma_last[sidx] = o
            need = {}
            for d in deps:
                if d is o:
                    continue
                if d.is_dma:
                    if id(d) in seen_dma[o.eng]:
                        continue
                    need[("dma", id(d))] = d
                else:
                    if d.eng == o.eng and not o.is_dma:
                        if o.eng == "pe" or not self.same_engine_sync:
                            continue
                    if seen[o.eng][d.eng] >= d.idx:
                        continue
                    cur = need.get(("eng", d.eng))
                    if cur is None or cur.idx < d.idx:
                        need[("eng", d.eng)] = d
            for key, d in need.items():
                d.marked = True
                o.waits.append(d)
                if d.is_dma:
                    seen_dma[o.eng].add(id(d))
                else:
                    seen[o.eng][d.eng] = d.idx
            for k in o.writes:
                last_w[k] = o
                readers[k] = []
            for k in o.reads:
                if k not in o.writes:
                    readers.setdefault(k, []).append(o)
        self.dma_last_all = list(dma_last.values())
        for e in ENG_NAMES:
            c = 0
            for o in self.ops[e]:
                if not o.is_dma and o.marked:
                    c += 1
                    o.semval = c

    def emit(self, final_wait_ops=()):
        nc = self.nc
        self.analyze()
        with ExitStack() as es:
            esem = {e: es.enter_context(nc.semaphore("sem_" + e)) for e in ENG_NAMES}
            dsem = {}
            for e in ENG_NAMES:
                if any(o.is_dma for o in self.ops[e]):
                    for i in range(self.n_dma_sems):
                        dsem[(e, i)] = es.enter_context(nc.semaphore("dsem_%s_%d" % (e, i)))
            if self.cc_ops:
                dsem[("cc", 0)] = es.enter_context(nc.semaphore("ccsem"))
            block = es.enter_context(nc.Block())
            hw = {"pe": block.tensor, "act": block.scalar, "dve": block.vector,
                  "pool": block.gpsimd, "sp": block.sync}

            def make(e):
                def body(eng):
                    for o in self.ops[e]:
                        for d in o.waits:
                            if d.is_dma:
                                eng.wait_ge(dsem[d.dma_sem], d.dma_val)
                            else:
                                eng.wait_ge(esem[d.eng], d.semval)
                        ins = o.fn(eng)
                        if o.is_dma and id(o) in self.cc_ops:
                            ins.then_inc(dsem[o.dma_sem])
                        elif o.is_dma:
                            ins.then_inc(dsem[o.dma_sem], 16)
                        elif o.marked:
                            ins.then_inc(esem[e], 1)
                    if e == "sp":
                        for d in self.dma_last_all:
                            eng.wait_ge(dsem[d.dma_sem], d.dma_val)
                return body

            for e in ENG_NAMES:
                if self.ops[e] or e == "sp":
                    hw[e](make(e))


class Ctx:
    pass


class _Stop(Exception):
    pass


import os
_STOP = float(os.environ.get("MK_STOP", "99"))


_CUR = [None]


def _stage(n):
    if _STOP <= n:
        _CUR[0].muted = True


def _mm(S, out, lhsT, rhs, start, stop, r, w):
    S.op("pe", lambda e: e.matmul(out, lhsT=lhsT, rhs=rhs, start=start, stop=stop), reads=r, writes=w)


def _act(S, out, in_, func, r, w, scale=1.0, bias=None, accum=None):
    def f(e):
        kw = {}
        if bias is not None:
            kw["bias"] = bias
        if accum is not None:
            kw["accum_out"] = accum
        return e.activation(out=out, in_=in_, func=func, scale=scale, **kw)
    S.op("act", f, reads=r, writes=w)


def _tt(S, eng, out, in0, in1, op, r, w):
    S.op(eng, lambda e: e.tensor_tensor(out=out, in0=in0, in1=in1, op=op), reads=r, writes=w)


def _stt(S, eng, out, in0, scalar, in1, op0, op1, r, w):
    S.op(eng, lambda e: e.scalar_tensor_tensor(out=out, in0=in0, scalar=scalar, in1=in1, op0=op0, op1=op1),
         reads=r, writes=w)


def _ts(S, eng, out, in0, s1, s2, op0, op1, r, w):
    if s2 is None:
        S.op(eng, lambda e: e.tensor_scalar(out=out, in0=in0, scalar1=s1, scalar2=None, op0=op0), reads=r, writes=w)
    else:
        S.op(eng, lambda e: e.tensor_scalar(out=out, in0=in0, scalar1=s1, scalar2=s2, op0=op0, op1=op1),
             reads=r, writes=w)


def _cp(S, eng, out, in_, r, w):
    if eng == "act":
        S.op(eng, lambda e: e.activation(out=out, in_=in_, func=AF.Identity, scale=1.0), reads=r, writes=w)
    else:
        S.op(eng, lambda e: e.tensor_copy(out=out, in_=in_), reads=r, writes=w)


def _dma(S, q, out, in_, r, w):
    return S.dma(q, lambda e: e.dma_start(out=out, in_=in_), reads=r, writes=w)


def _memset(S, eng, ap, val, w):
    S.op(eng, lambda e: e.memset(ap, val), writes=w)


def _recip(S, out, in_, r, w):
    S.op("dve", lambda e: e.reciprocal(out=out, in_=in_), reads=r, writes=w)


def make_dram(K, mode):
    nc = K.nc
    K.dram = {}
    P1 = mode in ("P1", "FUSED")
    P2 = mode in ("P2", "FUSED")

    def decl(name, shape, dt, role, need=True):
        if not need:
            return
        if role == "in":
            kind = "ExternalInput"
        elif role == "st":
            kind = {"P1": "ExternalOutput", "P2": "ExternalInput", "FUSED": "Internal"}[mode]
        elif role == "own":
            kind = {"P1": "ExternalOutput", "P2": None, "FUSED": "Internal"}[mode]
        elif role == "gat":
            kind = {"P1": None, "P2": "ExternalInput", "FUSED": "Internal"}[mode]
        elif role == "xout":
            kind = {"P1": None, "P2": "ExternalOutput", "FUSED": "Internal"}[mode]
        elif role == "p2s":
            kind = {"P1": None, "P2": "Internal", "FUSED": "Internal"}[mode]
        elif role == "out":
            kind = "ExternalOutput"
        if kind is None:
            return
        if kind == "Internal":
            t = nc.dram_tensor(name, shape, dt)
        else:
            t = nc.dram_tensor(name, shape, dt, kind=kind)
        if kind == "ExternalInput":
            _INPUT_NAMES.setdefault(id(nc), []).append(name)
        K.dram[name] = t.ap()

    nlay = K.nlay
    decl("xT", [D, NL], F32, "in")
    decl("ctxT", [D, NCX], F32, "in")
    decl("cT", [D, 2], F32, "in", P1)
    decl("cst", [128, 5 * 128], F32, "in")
    decl("rmask", [64, 8], F32, "in", P2)
    decl("ropec", [64, NL], F32, "in", P1)
    decl("ropes", [64, NL], F32, "in", P1)
    decl("w_mod", [nlay, D, 6 * D], F32, "in", P1)
    decl("bmodT", [nlay, 128, 48], F32, "in", P1)
    decl("lnT", [nlay, 128, 16], F32, "in", P1)
    decl("w_in", [nlay, D, DINA], F32, "in", P1)
    decl("wg2", [nlay, 33, 512], F32, "in", P1)
    decl("gvec", [nlay, 128, 8], F32, "in")
    decl("kvg_row", [nlay, 1, 128], F32, "in", P1)
    decl("w_uq", [nlay, 256, 1024], F32, "in", P1)
    decl("w_ukT", [nlay, 128, 512], F32, "in", P1)
    decl("w_uv", [nlay, 128, 512], F32, "in", P2)
    decl("w_out", [nlay, D, D], F32, "in", P2)
    decl("ffn_g", [D, DFF0], F32, "in", P2 and K.has_dense)
    decl("ffn_u", [D, DFF0], F32, "in", P2 and K.has_dense)
    decl("ffn_d", [DFF0, D], F32, "in", P2 and K.has_dense)
    decl("router", [D, NEXP], F32, "in", P2 and K.has_moe)
    decl("exp_g", [NEXP, D, DFFE], F32, "in", P2 and K.has_moe)
    decl("exp_u", [NEXP, D, DFFE], F32, "in", P2 and K.has_moe)
    decl("exp_d", [NEXP, DFFE, D], F32, "in", P2 and K.has_moe)
    decl("combT", [NEXP, NL], F32, "xout", P2 and K.has_moe)
    decl("fin_g", [128, 8], F32, "in", P2 and K.has_last)
    decl("dbg_lg", [128, 128], F32, "out", mode == "P2" and K.has_moe and bool(os.environ.get("MK_DBGLG")))
    decl("st_qp", [128, 4, NT], BF16, "st")
    decl("st_qr", [64, 4, NT], BF16, "st")
    decl("st_oi", [128, 4, NT], F32, "st")
    decl("st_qf", [64, 4, NT], BF16, "st")
    decl("st_qb", [64, 4, NT], BF16, "st")
    decl("st_sr", [128, 4, NT], BF16, "st")
    decl("st_U", [64, 18, 8, 128], F32, "st")
    decl("st_dec", [64, 18, 8], F32, "st")
    decl("st_par", [128, 2, 6, 8], F32, "st")
    decl("st_S", [64, 18, 8, 128], BF16, "p2s")
    decl("c_kl", [128, NCX], BF16, "st")
    decl("c_kr", [64, NCX], BF16, "st")
    decl("c_v", [NCX, 128], BF16, "st")
    decl("x_kl", [128, NL], BF16, "own")
    decl("x_kr", [64, NL], BF16, "own")
    decl("x_v", [NL, 128], BF16, "own")
    decl("x_L", [64, 1024], F32, "own")
    decl("x_A", [64, 8], F32, "own")
    decl("g_kl", [4 * 128, NL], BF16, "gat")
    decl("g_kr", [4 * 64, NL], BF16, "gat")
    decl("g_v", [4 * NL, 128], BF16, "gat")
    decl("g_L", [4 * 64, 1024], F32, "gat")
    decl("g_A", [4 * 64, 8], F32, "gat")
    decl("xT_out", [D, NL], F32, "xout", P2 and not (mode == "P2" and K.has_last))
    decl("ctxT_out", [D, NCX], F32, "xout", P2 and not (mode == "P2" and K.has_last))
    decl("outT", [D, NL], F32, "out", P2 and K.has_last)


BLOCKS = [("x", 0, i * 512, 512, i * 512) for i in range(4)] + [("c", 1, 0, 256, NL)]


def phase1(K, lw, xin, xkey, cin, ckey):
    nc, S, Dm = K.nc, K.S, K.dram
    with ExitStack() as es:
        def sb(name, shape, dt):
            return es.enter_context(nc.sbuf_tensor("p1_%d_%s" % (lw, name), shape, dt))
        cst = K.cst
        triF = cst[:, 128:256]
        triB = cst[:, 256:384]
        triSU = cst[:, 384:512]
        triSL = cst[:, 512:640]
        PB = K.pb
        ones = K.ones_bf
        onesf = K.ones_f
        win = sb("win", [128, 8, DINA], BF16)
        for dc in range(8):
            _dma(S, "pool", win[:, dc, :], Dm["w_in"][lw, dc * 128:(dc + 1) * 128, :], [], ["win"])
        wuq = sb("wuq", [128, 2, 1024], BF16)
        for kc in range(2):
            _dma(S, "pool", wuq[:, kc, :], Dm["w_uq"][lw, kc * 128:(kc + 1) * 128, :], [], ["wuq"])
        wukT = sb("wukT", [128, 512], BF16)
        _dma(S, "pool", wukT[:], Dm["w_ukT"][lw], [], ["wukT"])
        wg2 = sb("wg2", [33, 512], F32)
        _dma(S, "sp", wg2[:], Dm["wg2"][lw], [], ["wg2"])
        gvec = sb("gvec", [128, 8], F32)
        _dma(S, "sp", gvec[:], Dm["gvec"][lw], [], ["gvec"])
        kvgb = sb("kvgb", [128, 128], F32)
        _dma(S, "sp", kvgb[:], Dm["kvg_row"][lw].partition_broadcast(128), [], ["kvgb"])
        lnT = sb("lnT", [128, 16], F32)
        _dma(S, "sp", lnT[:], Dm["lnT"][lw], [], ["lnT"])
        bmodT = sb("bmodT", [128, 48], F32)
        _dma(S, "sp", bmodT[:], Dm["bmodT"][lw], [], ["bmodT"])
        cT = sb("cT", [128, 8, 2], F32)
        _dma(S, "sp", cT[:], Dm["cT"].rearrange("(dc p) c -> p dc c", p=128), [], ["cT"])
        sT = sb("sT", [128, 8, 2], F32)
        _act(S, sT[:], cT[:], AF.Silu, ["cT"], ["sT"])
        modraw = sb("modraw", [128, 96], F32)
        wm = [sb("wm%d" % i, [128, 8, 256], F32) for i in range(2)]
        mps = PB[0]
        for j in range(24):
            buf = wm[j % 2]
            key = "wm%d" % (j % 2)
            _dma(S, "sp", buf[:], Dm["w_mod"][lw, :, j * 256:(j + 1) * 256].rearrange("(dc p) n -> p dc n", p=128),
                 [], [key])
            for f in range(2):
                fc = j * 2 + f
                for dc in range(8):
                    _mm(S, mps[:, fc * 2:fc * 2 + 2], buf[:, dc, f * 128:(f + 1) * 128], sT[:, dc, :],
                        dc == 0, dc == 7, [key, "sT"], ["pb0"])
        _cp(S, "dve", modraw[:], mps[:, 0:96], ["pb0"], ["modraw"])
        par = sb("par", [128, 2, 6, 8], F32)
        mr = modraw[:].rearrange("p (f c) -> p c f", c=2)
        for col in range(2):
            _tt(S, "dve", par[:, col, :, :].rearrange("p a b -> p (a b)"), mr[:, col, :], bmodT[:], ALU.add,
                ["modraw", "bmodT"], ["par"])
        for col in range(2):
            _stt(S, "dve", par[:, col, 1, :], par[:, col, 1, :], 1.0, lnT[:, 0:8], ALU.add, ALU.mult,
                 ["par", "lnT"], ["par"])
            _stt(S, "dve", par[:, col, 4, :], par[:, col, 4, :], 1.0, lnT[:, 8:16], ALU.add, ALU.mult,
                 ["par", "lnT"], ["par"])
        _dma(S, "sp", Dm["st_par"], par[:], ["par"], ["st_par"])
        _stage(1)
        xb = sb("xb", [128, 8, 512], F32)
        sq = [sb("sq%d" % i, [128, 512], BF16) for i in range(2)]
        rstd = sb("rstd", [128, 512], F32)
        rtmp = sb("rtmp", [128, 512], F32)
        hT = sb("hT", [128, 8, 512], BF16)
        htmp = [sb("htmp%d" % i, [128, 512], F32) for i in range(2)]
        qf = sb("qf", [64, 4, 512], F32)
        kf = sb("kf", [64, 4, 512], F32)
        srs = sb("srs", [128, 4, 512], BF16)
        cqf = sb("cqf", [128, 2, 512], F32)
        cqn = sb("cqn", [128, 2, 512], BF16)
        ckvf = sb("ckvf", [128, 512], F32)
        krf = sb("krf", [64, 2, 512], F32)
        codT = sb("codT", [33, 512], F32)
        _memset(S, "dve", codT[32:33, :], 1.0, ["codT"])
        vtm = sb("vtm", [128, 4, 512], BF16)
        ktm = sb("ktm", [128, 4, 256], F32)
        qnh = sb("qnh", [128, 512], BF16)
        qps = sb("qps", [128, 4, 512], BF16)
        qrs = sb("qrs", [64, 4, 512], BF16)
        rt1 = sb("rt1", [64, 512], F32)
        rt2 = sb("rt2", [64, 512], F32)
        cosb = sb("cosb", [64, 512], F32)
        sinb = sb("sinb", [64, 512], F32)
        kls = sb("kls", [128, 512], BF16)
        krs = sb("krs", [64, 512], BF16)
        vls = sb("vls", [128, 4, 128], BF16)
        vss = sb("vss", [128, 4], F32)
        vjunk = sb("vjunk", [128, 128], F32)
        vsq = sb("vsq", [128, 128], F32)
        vs1 = sb("vs1", [128, 4], F32)
        vs2 = sb("vs2", [128, 4], F32)
        Gn = sb("Gn", [128, 512], F32)
        Ge = sb("Ge", [128, 512], F32)
        E1 = sb("E1", [64, 2, 512], F32)
        E2 = sb("E2", [64, 2, 512], F32)
        E3 = sb("E3", [128, 512], F32)
        qtf = sb("qtf", [64, 4, 512], BF16)
        qtb = sb("qtb", [64, 4, 512], BF16)
        ktf = sb("ktf", [64, 4, 128], BF16)
        ktb = sb("ktb", [64, 4, 128], BF16)
        khf = sb("khf", [128, 256], BF16)
        khb = sb("khb", [128, 256], BF16)
        amt = sb("amt", [128, 128], BF16)
        am1 = sb("am1", [128, 128], F32)
        am2 = sb("am2", [128, 128], F32)
        ois = sb("ois", [128, 4, 512], F32)
        Us = sb("Us", [64, 8, 128], F32)
        decs = sb("decs", [64, 4, 8], F32)
        Lf = sb("Lf", [64, 4, 128], F32)
        Lb = sb("Lb", [64, 4, 128], F32)
        PA = sb("PA", [64, 8], F32)

        for (src, col, n0, N, g0) in BLOCKS:
            xdr = (xin if src == "x" else cin)
            ntile = N // 128
            a1 = par[:, col, 1, :]
            sh1 = par[:, col, 0, :]
            _dma(S, "sp", xb[:, :, :N], xdr[:, n0:n0 + N].rearrange("(dc p) n -> p dc n", p=128),
                 [xkey if src == "x" else ckey], ["xb"])
            for dc in range(8):
                sqb, sqk = sq[dc % 2], "sq%d" % (dc % 2)
                _tt(S, "pool", sqb[:, :N], xb[:, dc, :N], xb[:, dc, :N], ALU.mult, ["xb"], [sqk])
                _mm(S, PB[2][:, :N], ones[:], sqb[:, :N], dc == 0, dc == 7, [sqk, "ones"], ["pb2"])
            _stage(1.3)
            _act(S, rtmp[:, :N], PB[2][:, :N], AF.Sqrt, ["pb2"], ["rtmp"], scale=1.0 / D, bias=K.eps_ap)
            _recip(S, rstd[:, :N], rtmp[:, :N], ["rtmp"], ["rstd"])
            _stage(1.6)
            for dc in range(8):
                ht = htmp[dc % 2]
                hk = "htmp%d" % (dc % 2)
                _stt(S, "dve", ht[:, :N], xb[:, dc, :N], a1[:, dc:dc + 1], rstd[:, :N], ALU.mult, ALU.mult,
                     ["xb", "par", "rstd"], [hk])
                _act(S, hT[:, dc, :N], ht[:, :N], AF.Identity, [hk, "par"], ["hT"], bias=sh1[:, dc:dc + 1])
            _stage(2)
            pbi = [0]

            def nextpb():
                b = pbi[0] % 2
                pbi[0] += 1
                return PB[b], "pb%d" % b

            def proj(c0, ncols, evac):
                ps, pk = nextpb()
                for dc in range(8):
                    _mm(S, ps[0:ncols, :N], win[:, dc, c0:c0 + ncols], hT[:, dc, :N], dc == 0, dc == 7,
                        ["win", "hT"], [pk])
                evac(ps, pk)

            for h in range(4):
                proj(h * 64, 64, lambda ps, pk, h=h: _ts(S, "dve", qf[:, h, :N], ps[0:64, :N], 0.125, None, ALU.mult, None,
                                                         [pk], ["qf"]))
            for h in range(4):
                proj(256 + h * 64, 64, lambda ps, pk, h=h: _cp(S, "dve", kf[:, h, :N], ps[0:64, :N], [pk], ["kf"]))
            for c in range(4):
                proj(1056 + c * 128, 128, lambda ps, pk, c=c: _act(S, srs[:, c, :N], ps[:, :N], AF.Silu, [pk], ["srs"]))
            _dma(S, "sp", Dm["st_sr"][:, :, g0:g0 + N], srs[:, :, :N], ["srs"], ["st_sr"])
            for c in range(2):
                proj(1568 + c * 128, 128, lambda ps, pk, c=c: _cp(S, "dve", cqf[:, c, :N], ps[:, :N], [pk], ["cqf"]))
            proj(1824, 128, lambda ps, pk: _cp(S, "dve", ckvf[:, :N], ps[:, :N], [pk], ["ckvf"]))
            proj(1952, 64, lambda ps, pk: _cp(S, "dve", krf[:, 0, :N], ps[0:64, :N], [pk], ["krf"]))
            proj(2016, 64, lambda ps, pk: _cp(S, "dve", krf[:, 1, :N], ps[0:64, :N], [pk], ["krf"]))
            proj(1024, 32, lambda ps, pk: _cp(S, "dve", codT[0:32, :N], ps[0:32, :N], [pk], ["codT"]))
            _stage(3)
            _memset(S, "dve", vss[:], 0.0, ["vss"])
            for t in range(ntile):
                tsl = slice(t * 128, (t + 1) * 128)
                ps, pk = nextpb()
                for dc in range(8):
                    _mm(S, ps[:, 0:512], hT[:, dc, tsl], win[:, dc, 512:1024], dc == 0, dc == 7, ["win", "hT"], [pk])
                _cp(S, "act", vtm[:, t, :], ps[:, 0:512], [pk], ["vtm"])
                ps, pk = nextpb()
                for dc in range(8):
                    _mm(S, ps[:, 0:256], hT[:, dc, tsl], win[:, dc, 256:512], dc == 0, dc == 7, ["win", "hT"], [pk])
                for dc in range(8):
                    _mm(S, ps[:, 256:384], hT[:, dc, tsl], win[:, dc, 1824:1952], dc == 0, dc == 7, ["win", "hT"], [pk])
                _stage(3.2)
                _cp(S, "dve", ktm[:, t, :], ps[:, 0:256], [pk], ["ktm"])
                _stage(3.4)
                _cp(S, "dve", vjunk[:], ps[:, 256:384], [pk], ["vjunk"])
                _tt(S, "pool", vsq[:], vjunk[:], vjunk[:], ALU.mult, ["vjunk"], ["vsq"])
                S.op("dve", lambda e, t=t: e.reduce_sum(out=vss[:, t:t + 1], in_=vsq[:], axis=mybir.AxisListType.X),
                     reads=["vsq"], writes=["vss"])
                _stage(3.5)
                _act(S, vs1[:, t:t + 1], vss[:, t:t + 1], AF.Sqrt, ["vss"], ["vs1"], scale=1.0 / 128, bias=K.eps_ap)
                _stage(3.6)
                _recip(S, vs2[:, t:t + 1], vs1[:, t:t + 1], ["vs1"], ["vs2"])
                _stage(3.7)
                _stt(S, "dve", vls[:, t, :], vjunk[:], vs2[:, t:t + 1], kvgb[:], ALU.mult, ALU.mult,
                     ["vjunk", "vs2", "kvgb"], ["vls"])
            vdst = (Dm["x_v"][n0:n0 + N, :] if src == "x" else Dm["c_v"][:, :]).rearrange("(t p) r -> p t r", p=128)
            vkey = "x_v" if src == "x" else "c_v"
            _stage(3.8)
            _dma(S, "sp", vdst, vls[:, :ntile, :], ["vls"], [vkey])
            _stage(4)
            for c in range(2):
                sqb, sqk = sq[c % 2], "sq%d" % (c % 2)
                _tt(S, "pool", sqb[:, :N], cqf[:, c, :N], cqf[:, c, :N], ALU.mult, ["cqf"], [sqk])
                _mm(S, PB[2][:, :N], ones[:], sqb[:, :N], c == 0, c == 1, [sqk, "ones"], ["pb2"])
            _act(S, rtmp[:, :N], PB[2][:, :N], AF.Sqrt, ["pb2"], ["rtmp"], scale=1.0 / 256, bias=K.eps_ap)
            _recip(S, rstd[:, :N], rtmp[:, :N], ["rtmp"], ["rstd"])
            for c in range(2):
                _stt(S, "dve", cqn[:, c, :N], cqf[:, c, :N], gvec[:, 1 + c:2 + c], rstd[:, :N], ALU.mult, ALU.mult,
                     ["cqf", "gvec", "rstd"], ["cqn"])
            if src == "x":
                _dma(S, "sp", cosb[:, :N], Dm["ropec"][:, n0:n0 + N], [], ["cosb"])
                _dma(S, "sp", sinb[:, :N], Dm["ropes"][:, n0:n0 + N], [], ["sinb"])
            for h in range(4):
                ps, pk = nextpb()
                for kc in range(2):
                    _mm(S, ps[:, :N], wuq[:, kc, h * 128:(h + 1) * 128], cqn[:, kc, :N], kc == 0, kc == 1,
                        ["wuq", "cqn"], [pk])
                _cp(S, "act", qnh[:, :N], ps[:, :N], [pk], ["qnh"])
                ps, pk = nextpb()
                _mm(S, ps[:, :N], wukT[:, h * 128:(h + 1) * 128], qnh[:, :N], True, True, ["wukT", "qnh"], [pk])
                _cp(S, "dve", qps[:, h, :N], ps[:, :N], [pk], ["qps"])
                ps, pk = nextpb()
                for kc in range(2):
                    _mm(S, ps[0:64, :N], wuq[:, kc, 512 + h * 64:512 + (h + 1) * 64], cqn[:, kc, :N], kc == 0, kc == 1,
                        ["wuq", "cqn"], [pk])
                if src == "x":
                    ps2, pk2 = nextpb()
                    for kc in range(2):
                        _mm(S, ps2[0:64, :N], wuq[:, kc, 768 + h * 64:768 + (h + 1) * 64], cqn[:, kc, :N], kc == 0, kc == 1,
                            ["wuq", "cqn"], [pk2])
                    _tt(S, "dve", rt1[:, :N], ps[0:64, :N], cosb[:, :N], ALU.mult, [pk, "cosb"], ["rt1"])
                    _tt(S, "dve", rt2[:, :N], ps2[0:64, :N], sinb[:, :N], ALU.mult, [pk2, "sinb"], ["rt2"])
                    _tt(S, "pool", qrs[:, h, :N], rt1[:, :N], rt2[:, :N], ALU.add, ["rt1", "rt2"], ["qrs"])
                else:
                    _cp(S, "dve", qrs[:, h, :N], ps[0:64, :N], [pk], ["qrs"])
            _dma(S, "sp", Dm["st_qp"][:, :, g0:g0 + N], qps[:, :, :N], ["qps"], ["st_qp"])
            _dma(S, "sp", Dm["st_qr"][:, :, g0:g0 + N], qrs[:, :, :N], ["qrs"], ["st_qr"])
            _tt(S, "pool", sq[0][:, :N], ckvf[:, :N], ckvf[:, :N], ALU.mult, ["ckvf"], ["sq0"])
            _mm(S, PB[2][:, :N], ones[:], sq[0][:, :N], True, True, ["sq0", "ones"], ["pb2"])
            _act(S, rtmp[:, :N], PB[2][:, :N], AF.Sqrt, ["pb2"], ["rtmp"], scale=1.0 / 128, bias=K.eps_ap)
            _recip(S, rstd[:, :N], rtmp[:, :N], ["rtmp"], ["rstd"])
            _stt(S, "dve", kls[:, :N], ckvf[:, :N], gvec[:, 3:4], rstd[:, :N], ALU.mult, ALU.mult,
                 ["ckvf", "gvec", "rstd"], ["kls"])
            if src == "x":
                _tt(S, "dve", rt1[:, :N], krf[:, 0, :N], cosb[:, :N], ALU.mult, ["krf", "cosb"], ["rt1"])
                _tt(S, "dve", rt2[:, :N], krf[:, 1, :N], sinb[:, :N], ALU.mult, ["krf", "sinb"], ["rt2"])
                _tt(S, "pool", krs[:, :N], rt1[:, :N], rt2[:, :N], ALU.add, ["rt1", "rt2"], ["krs"])
                _dma(S, "sp", Dm["x_kl"][:, n0:n0 + N], kls[:, :N], ["kls"], ["x_kl"])
                _dma(S, "sp", Dm["x_kr"][:, n0:n0 + N], krs[:, :N], ["krs"], ["x_kr"])
            else:
                _cp(S, "dve", krs[:, :N], krf[:, 0, :N], ["krf"], ["krs"])
                _dma(S, "sp", Dm["c_kl"][:, :], kls[:, :N], ["kls"], ["c_kl"])
                _dma(S, "sp", Dm["c_kr"][:, :], krs[:, :N], ["krs"], ["c_kr"])
            _stage(5)
            if src == "x" and n0 == 0:
                _memset(S, "dve", Lf[:], 0.0, ["Lf"])
                _memset(S, "dve", Lb[:], 0.0, ["Lb"])
                _memset(S, "dve", PA[:], 1.0, ["PA"])
            for t in range(ntile):
                tsl = slice(t * 128, (t + 1) * 128)
                gt = (g0 // 128) + t
                _mm(S, PB[3][:, :], codT[0:33, tsl], wg2[:], True, True, ["codT", "wg2"], ["pb3"])
                _act(S, Ge[:], PB[3][:, :], AF.Exp, ["pb3"], ["Ge"], scale=-1.0)
                _act(S, Gn[:], Ge[:], AF.Ln, ["Ge"], ["Gn"], scale=1.0, bias=K.one_ap)
                _stage(5.1)
                for h in range(4):
                    _mm(S, PB[4][0:64, h * 128:(h + 1) * 128], Gn[:, h * 64:(h + 1) * 64], triF, True, True,
                        ["Gn", "cst"], ["pb4"])
                    _mm(S, PB[5][0:64, h * 128:(h + 1) * 128], Gn[:, 256 + h * 64:256 + (h + 1) * 64], triB, True, True,
                        ["Gn", "cst"], ["pb5"])
                _mm(S, PB[3][:, 0:256], triSU, Gn[:, 0:256], True, True, ["Gn", "cst"], ["pb3"])
                _mm(S, PB[3][:, 256:512], triSL, Gn[:, 256:512], True, True, ["Gn", "cst"], ["pb3"])
                _stage(5.3)
                _act(S, E1[:, 0, :], PB[4][0:64, :], AF.Exp, ["pb4"], ["E1"], scale=-1.0 / 16)
                _act(S, E1[:, 1, :], PB[5][0:64, :], AF.Exp, ["pb5"], ["E1"], scale=-1.0 / 16)
                _act(S, E2[:, 0, :], PB[4][0:64, :], AF.Exp, ["pb4"], ["E2"], scale=1.0 / 16)
                _act(S, E2[:, 1, :], PB[5][0:64, :], AF.Exp, ["pb5"], ["E2"], scale=1.0 / 16)
                _act(S, E3[:], PB[3][:, :], AF.Exp, ["pb3"], ["E3"], scale=-1.0 / 16)
                _stage(5.4)
                e1f = E1[:, 0, :].rearrange("p (h t) -> p h t", h=4)
                e1b = E1[:, 1, :].rearrange("p (h t) -> p h t", h=4)
                e2f = E2[:, 0, :].rearrange("p (h t) -> p h t", h=4)
                e2b = E2[:, 1, :].rearrange("p (h t) -> p h t", h=4)
                _tt(S, "dve", qtf[:, :, tsl], qf[:, :, tsl], e1f, ALU.mult, ["qf", "E1"], ["qtf"])
                _tt(S, "pool", qtb[:, :, tsl], qf[:, :, tsl], e1b, ALU.mult, ["qf", "E1"], ["qtb"])
                _tt(S, "dve", ktf[:], kf[:, :, tsl], e2f, ALU.mult, ["kf", "E2"], ["ktf"])
                _tt(S, "pool", ktb[:], kf[:, :, tsl], e2b, ALU.mult, ["kf", "E2"], ["ktb"])
                _cp(S, "dve", decs[:, t, 0:4], e1f[:, :, 127], ["E1"], ["decs"])
                _cp(S, "dve", decs[:, t, 4:8], e1b[:, :, 0], ["E1"], ["decs"])
                _tt(S, "dve", khf[:], ktm[:, t, :], E3[:, 0:256], ALU.mult, ["ktm", "E3"], ["khf"])
                _tt(S, "pool", khb[:], ktm[:, t, :], E3[:, 256:512], ALU.mult, ["ktm", "E3"], ["khb"])
                _stage(5.5)
                for h in range(int(os.environ.get("MK_NH", "4"))):
                    ps, pk = PB[6], "pb6"
                    _mm(S, ps[:, 0:128], ktf[:, h, :], qtf[:, h, tsl], True, True, ["ktf", "qtf"], [pk])
                    _mm(S, ps[:, 128:256], ktb[:, h, :], qtb[:, h, tsl], True, True, ["ktb", "qtb"], [pk])
                    _stage(5.6)
                    _tt(S, "dve", am1[:], ps[:, 0:128], triF, ALU.mult, [pk, "cst"], ["am1"])
                    _tt(S, "dve", am2[:], ps[:, 128:256], triB, ALU.mult, [pk, "cst"], ["am2"])
                    _tt(S, "pool", amt[:], am1[:], am2[:], ALU.add, ["am1", "am2"], ["amt"])
                    _stage(5.7)
                    _mm(S, PB[7][:, 0:128], vtm[:, t, h * 128:(h + 1) * 128], amt[:], True, True, ["vtm", "amt"], ["pb7"])
                    _mm(S, PB[7][0:64, 128:256], khf[:, h * 64:(h + 1) * 64], vtm[:, t, h * 128:(h + 1) * 128], True, True,
                        ["khf", "vtm"], ["pb7"])
                    _mm(S, PB[7][0:64, 256:384], khb[:, h * 64:(h + 1) * 64], vtm[:, t, h * 128:(h + 1) * 128], True, True,
                        ["khb", "vtm"], ["pb7"])
                    _stage(5.8)
                    _cp(S, "act", ois[:, h, tsl], PB[7][:, 0:128], ["pb7"], ["ois"])
                    _cp(S, "dve", Us[:, h, :], PB[7][0:64, 128:256], ["pb7"], ["Us"])
                    _cp(S, "dve", Us[:, 4 + h, :], PB[7][0:64, 256:384], ["pb7"], ["Us"])
                    _stage(5.85)
                    if src == "x" and not os.environ.get("MK_NOL"):
                        _stt(S, "dve", Lf[:, h, :], Lf[:, h, :], decs[:, t, h:h + 1], Us[:, h, :], ALU.mult, ALU.add,
                             ["Lf", "decs", "Us"], ["Lf"])
                        if not os.environ.get("MK_NOLB"):
                            _stt(S, "dve", Lb[:, h, :], Us[:, 4 + h, :], PA[:, 4 + h:5 + h], Lb[:, h, :], ALU.mult, ALU.add,
                                 ["Lb", "PA", "Us"], ["Lb"])
                _stage(5.9)
                _dma(S, "sp", Dm["st_U"][:, gt, :, :], Us[:], ["Us"], ["st_U"])
                _stage(5.92)
                if src == "x":
                    _tt(S, "dve", PA[:], PA[:], decs[:, t, :], ALU.mult, ["PA", "decs"], ["PA"])
            t0 = g0 // 128
            _stage(5.95)
            _dma(S, "sp", Dm["st_oi"][:, :, g0:g0 + N], ois[:, :, :N], ["ois"], ["st_oi"])
            _dma(S, "sp", Dm["st_qf"][:, :, g0:g0 + N], qtf[:, :, :N], ["qtf"], ["st_qf"])
            _dma(S, "sp", Dm["st_qb"][:, :, g0:g0 + N], qtb[:, :, :N], ["qtb"], ["st_qb"])
            _dma(S, "sp", Dm["st_dec"][:, t0:t0 + ntile, :], decs[:, :ntile, :], ["decs"], ["st_dec"])
            if src == "x" and n0 == NL - 512:
                _dma(S, "sp", Dm["x_L"][:, 0:512].rearrange("p (h v) -> p h v", h=4), Lf[:], ["Lf"], ["x_L"])
                _dma(S, "sp", Dm["x_L"][:, 512:1024].rearrange("p (h v) -> p h v", h=4), Lb[:], ["Lb"], ["x_L"])
                _dma(S, "sp", Dm["x_A"][:, :], PA[:], ["PA"], ["x_A"])
    S.barrier()


def phase2(K, li, lw, xin, xkey, cin, ckey, is_last):
    nc, S, Dm = K.nc, K.S, K.dram
    need_ctx = li < DEPTH - 1
    dense = (li % 2 == 0)
    PB = K.pb
    ones = K.ones_bf
    onesf = K.ones_f
    cst = K.cst
    ident = cst[:, 0:128]
    blocks = [b for b in BLOCKS if (b[0] == "x" or need_ctx)]
    with ExitStack() as es0:
        def sb0(name, shape, dt):
            return es0.enter_context(nc.sbuf_tensor("p2_%d_%s" % (li, name), shape, dt))
        xs = sb0("xs", [128, 8, NT], F32)
        _dma(S, "sp", xs[:, :, 0:NL], xin.rearrange("(dc p) n -> p dc n", p=128), [xkey], ["xs"])
        _dma(S, "sp", xs[:, :, NL:NT], cin.rearrange("(dc p) n -> p dc n", p=128), [ckey], ["xs"])
        par = sb0("par", [128, 2, 6, 8], F32)
        _dma(S, "sp", par[:], Dm["st_par"], ["st_par"], ["par"])
        gvec = sb0("gvec", [128, 8], F32)
        _dma(S, "sp", gvec[:], Dm["gvec"][lw], [], ["gvec"])

        with ExitStack() as es:
            def sb(name, shape, dt):
                return es.enter_context(nc.sbuf_tensor("p2c_%d_%s" % (li, name), shape, dt))
            dec = sb("dec", [64, 18, 8], F32)
            _dma(S, "sp", dec[:], Dm["st_dec"], ["st_dec"], ["dec"])
            gL = sb("gL", [64, 4, 1024], F32)
            _dma(S, "sp", gL[:], Dm["g_L"].rearrange("(r p) n -> p r n", p=64), ["g_L"], ["gL"])
            gA = sb("gA", [64, 4, 8], F32)
            _dma(S, "sp", gA[:], Dm["g_A"].rearrange("(r p) n -> p r n", p=64), ["g_A"], ["gA"])
            rmask = sb("rmask", [64, 8], F32)
            _dma(S, "sp", rmask[:], Dm["rmask"], [], ["rmask"])
            Ub = [sb("U%d" % i, [64, 8, 128], F32) for i in range(2)]
            cur = sb("cur", [64, 8, 128], F32)
            Sb_ = [sb("Sb%d" % i, [64, 8, 128], BF16) for i in range(2)]
            uctr = [0]

            def loadU(gt):
                i = uctr[0] % 2
                uctr[0] += 1
                _dma(S, "sp", Ub[i][:], Dm["st_U"][:, gt, :, :], ["st_U"], ["U%d" % i])
                return Ub[i], "U%d" % i

            U16, k16 = loadU(16)
            U17, k17 = loadU(17)
            _memset(S, "dve", Sb_[0][:], 0.0, ["Sb0"])
            _memset(S, "dve", Sb_[1][:], 0.0, ["Sb1"])
            _cp(S, "dve", Sb_[1][:, 0:4, :], U16[:, 0:4, :], [k16], ["Sb1"])
            _cp(S, "dve", Sb_[0][:, 4:8, :], U17[:, 4:8, :], [k17], ["Sb0"])
            _dma(S, "sp", Dm["st_S"][:, 16, :, :], Sb_[0][:], ["Sb0"], ["st_S"])
            _dma(S, "sp", Dm["st_S"][:, 17, :, :], Sb_[1][:], ["Sb1"], ["st_S"])
            for h in range(4):
                _stt(S, "dve", cur[:, h, :], U16[:, h, :], dec[:, 17, h:h + 1], U17[:, h, :], ALU.mult, ALU.add,
                     [k16, k17, "dec"], ["cur"])
                _stt(S, "dve", cur[:, 4 + h, :], U17[:, 4 + h, :], dec[:, 16, 4 + h:5 + h], U16[:, 4 + h, :], ALU.mult, ALU.add,
                     [k16, k17, "dec"], ["cur"])
            Ap = sb("Ap", [64, 4, 8], F32)
            for r in range(4):
                _ts(S, "dve", Ap[:, r, 0:4], gA[:, r, 0:4], -1.0, rmask[:, r:r + 1], ALU.add, ALU.mult, ["gA", "rmask"], ["Ap"])
                _ts(S, "dve", Ap[:, r, 4:8], gA[:, r, 4:8], -1.0, rmask[:, 4 + r:5 + r], ALU.add, ALU.mult, ["gA", "rmask"], ["Ap"])
            _ts(S, "dve", Ap[:], Ap[:], 1.0, None, ALU.add, None, ["Ap"], ["Ap"])
            for r in range(4):
                _ts(S, "dve", gL[:, r, 0:512], gL[:, r, 0:512], rmask[:, r:r + 1], None, ALU.mult, None, ["gL", "rmask"], ["gL"])
                _ts(S, "dve", gL[:, r, 512:1024], gL[:, r, 512:1024], rmask[:, 4 + r:5 + r], None, ALU.mult, None,
                    ["gL", "rmask"], ["gL"])
            for r in range(4):
                for h in range(4):
                    _stt(S, "dve", cur[:, h, :], cur[:, h, :], Ap[:, r, h:h + 1], gL[:, r, h * 128:(h + 1) * 128],
                         ALU.mult, ALU.add, ["cur", "Ap", "gL"], ["cur"])
            for r in (3, 2, 1, 0):
                for h in range(4):
                    _stt(S, "dve", cur[:, 4 + h, :], cur[:, 4 + h, :], Ap[:, r, 4 + h:5 + h],
                         gL[:, r, 512 + h * 128:512 + (h + 1) * 128], ALU.mult, ALU.add, ["cur", "Ap", "gL"], ["cur"])
            Sf = sb("Sf", [64, 16, 4, 128], BF16)
            for i in range(16):
                U, uk = loadU(i)
                _cp(S, "pool", Sf[:, i, :, :], cur[:, 0:4, :], ["cur"], ["Sf"])
                for h in range(4):
                    _stt(S, "dve", cur[:, h, :], cur[:, h, :], dec[:, i, h:h + 1], U[:, h, :], ALU.mult, ALU.add,
                         ["cur", "dec", uk], ["cur"])
            sctr = 0
            for i in range(15, -1, -1):
                U, uk = loadU(i)
                sbuf_, sk = Sb_[sctr % 2], "Sb%d" % (sctr % 2)
                sctr += 1
                _cp(S, "pool", sbuf_[:, 0:4, :], Sf[:, i, :, :], ["Sf"], [sk])
                _cp(S, "pool", sbuf_[:, 4:8, :], cur[:, 4:8, :], ["cur"], [sk])
                _dma(S, "sp", Dm["st_S"][:, i, :, :], sbuf_[:], [sk], ["st_S"])
                for h in range(4):
                    _stt(S, "dve", cur[:, 4 + h, :], cur[:, 4 + h, :], dec[:, i, 4 + h:5 + h], U[:, 4 + h, :], ALU.mult, ALU.add,
                         ["cur", "dec", uk], ["cur"])
        S.barrier()

        with ExitStack() as es:
            def sb(name, shape, dt):
                return es.enter_context(nc.sbuf_tensor("p2a_%d_%s" % (li, name), shape, dt))
            KlT = sb("KlT", [128, NKEY], BF16)
            KrT = sb("KrT", [64, NKEY], BF16)
            Vl = sb("Vl", [128, NKT, 128], BF16)
            _dma(S, "sp", KlT[:, 0:NCX], Dm["c_kl"], ["c_kl"], ["KlT"])
            _dma(S, "sp", KrT[:, 0:NCX], Dm["c_kr"], ["c_kr"], ["KrT"])
            _dma(S, "sp", Vl[:, 0:2, :], Dm["c_v"].rearrange("(t p) r -> p t r", p=128), ["c_v"], ["Vl"])
            for r in range(4):
                _dma(S, "sp", KlT[:, NCX + r * NL:NCX + (r + 1) * NL], Dm["g_kl"][r * 128:(r + 1) * 128, :], ["g_kl"], ["KlT"])
                _dma(S, "sp", KrT[:, NCX + r * NL:NCX + (r + 1) * NL], Dm["g_kr"][r * 64:(r + 1) * 64, :], ["g_kr"], ["KrT"])
                _dma(S, "sp", Vl[:, 2 + 16 * r:2 + 16 * (r + 1), :],
                     Dm["g_v"][r * NL:(r + 1) * NL, :].rearrange("(t p) r -> p t r", p=128), ["g_v"], ["Vl"])
            wout = sb("wout", [128, 8, D], BF16)
            _dma(S, "pool", wout[:], Dm["w_out"][lw].rearrange("(c p) n -> p c n", p=128), [], ["wout"])
            wuv = sb("wuv", [128, 512], BF16)
            _dma(S, "pool", wuv[:], Dm["w_uv"][lw], [], ["wuv"])
            oi = sb("oi", [128, 4, 512], F32)
            qf = sb("qf", [64, 4, 512], BF16)
            qb = sb("qb", [64, 4, 512], BF16)
            sr = sb("sr", [128, 4, 512], BF16)
            qp = sb("qp", [128, 4, 512], BF16)
            qr = sb("qr", [64, 4, 512], BF16)
            Sblk = sb("Sblk", [64, 4, 8, 128], BF16)
            mix = sb("mix", [128, 8, 512], BF16)
            pT = [sb("pT%d" % i, [128, 512], BF16) for i in range(3)]
            racc = sb("racc", [128, 512], F32)
            rinv = sb("rinv", [128, 512], F32)
            rtmp = sb("rtmp", [128, 512], F32)
            sqb = sb("sqb", [128, 512], BF16)
            otmp = sb("otmp", [128, 512], F32)
            oln = sb("oln", [128, 512], BF16)
            for (src, col, n0, N, g0) in blocks:
                ntile = N // 128
                t0 = g0 // 128
                _dma(S, "sp", oi[:, :, :N], Dm["st_oi"][:, :, g0:g0 + N], ["st_oi"], ["oi"])
                _dma(S, "sp", qf[:, :, :N], Dm["st_qf"][:, :, g0:g0 + N], ["st_qf"], ["qf"])
                _dma(S, "sp", qb[:, :, :N], Dm["st_qb"][:, :, g0:g0 + N], ["st_qb"], ["qb"])
                _dma(S, "sp", sr[:, :, :N], Dm["st_sr"][:, :, g0:g0 + N], ["st_sr"], ["sr"])
                _dma(S, "sp", qp[:, :, :N], Dm["st_qp"][:, :, g0:g0 + N], ["st_qp"], ["qp"])
                _dma(S, "sp", qr[:, :, :N], Dm["st_qr"][:, :, g0:g0 + N], ["st_qr"], ["qr"])
                _dma(S, "sp", Sblk[:, :ntile, :, :], Dm["st_S"][:, t0:t0 + ntile, :, :], ["st_S"], ["Sblk"])
                for h in range(4):
                    for t in range(ntile):
                        tsl = slice(t * 128, (t + 1) * 128)
                        _mm(S, PB[3][:, tsl], Sblk[:, t, h, :], qf[:, h, tsl], True, False, ["Sblk", "qf"], ["pb3"])
                        _mm(S, PB[3][:, tsl], Sblk[:, t, 4 + h, :], qb[:, h, tsl], False, True, ["Sblk", "qb"], ["pb3"])
                    _tt(S, "dve", oi[:, h, :N], oi[:, h, :N], PB[3][:, :N], ALU.add, ["oi", "pb3"], ["oi"])
                    _tt(S, "pool", sqb[:, :N], oi[:, h, :N], oi[:, h, :N], ALU.mult, ["oi"], ["sqb"])
                    _mm(S, PB[4][:, :N], ones[:], sqb[:, :N], True, True, ["sqb", "ones"], ["pb4"])
                    _act(S, rtmp[:, :N], PB[4][:, :N], AF.Sqrt, ["pb4"], ["rtmp"], scale=1.0 / 128, bias=K.eps_ap)
                    _recip(S, rinv[:, :N], rtmp[:, :N], ["rtmp"], ["rinv"])
                    _stt(S, "dve", otmp[:, :N], oi[:, h, :N], gvec[:, 0:1], rinv[:, :N], ALU.mult, ALU.mult,
                         ["oi", "gvec", "rinv"], ["otmp"])
                    _tt(S, "dve", mix[:, h, :N], otmp[:, :N], sr[:, h, :N], ALU.mult, ["otmp", "sr"], ["mix"])
                kts = list(range(NKT)) if src == "x" else [0, 1]
                _dbg = os.environ.get("MK_P2DBG%d" % li, "")
                if "nogla" in _dbg:
                    _memset(S, "dve", mix[:, 0:4, :N], 0.0, ["mix"])
                if "nomla" in _dbg:
                    _memset(S, "dve", mix[:, 4:8, :N], 0.0, ["mix"])
                for h in (range(4) if "nomla" not in _dbg else []):
                    sbanks = [(PB[0], "pb0"), (PB[1], "pb1"), (PB[7], "pb7")]

                    def qk(ki):
                        ps, pk = sbanks[ki % 3]
                        ksl = slice(kts[ki] * 128, (kts[ki] + 1) * 128)
                        _mm(S, ps[:, :N], KlT[:, ksl], qp[:, h, :N], True, False, ["KlT", "qp"], [pk])
                        _mm(S, ps[:, :N], KrT[:, ksl], qr[:, h, :N], False, True, ["KrT", "qr"], [pk])
                    qk(0)
                    if len(kts) > 1:
                        qk(1)
                    for ki, kt in enumerate(kts):
                        ps, pk = sbanks[ki % 3]
                        if ki + 2 < len(kts):
                            qk(ki + 2)
                        p_, ppk = pT[ki % 3], "pT%d" % (ki % 3)
                        _act(S, p_[:, :N], ps[:, :N], AF.Exp, [pk], [ppk], scale=MLA_SCALE)
                        _mm(S, PB[2][:, :N], Vl[:, kt, :], p_[:, :N], ki == 0, ki == len(kts) - 1, ["Vl", ppk], ["pb2"])
                        if ki == 0:
                            _cp(S, "dve", racc[:, :N], p_[:, :N], [ppk], ["racc"])
                        else:
                            _tt(S, "dve", racc[:, :N], racc[:, :N], p_[:, :N], ALU.add, ["racc", ppk], ["racc"])
                    _mm(S, PB[6][:, :N], onesf[:], racc[:, :N], True, True, ["racc", "onesf"], ["pb6"])
                    _recip(S, rinv[:, :N], PB[6][:, :N], ["pb6"], ["rinv"])
                    _tt(S, "dve", oln[:, :N], PB[2][:, :N], rinv[:, :N], ALU.mult, ["pb2", "rinv"], ["oln"])
                    _mm(S, PB[3][:, :N], wuv[:, h * 128:(h + 1) * 128], oln[:, :N], True, True, ["wuv", "oln"], ["pb3"])
                    _cp(S, "act", mix[:, 4 + h, :N], PB[3][:, :N], ["pb3"], ["mix"])
                g1 = par[:, col, 2, :]
                for dco in range(8):
                    b = 4 + dco % 2
                    ps, pk = PB[b], "pb%d" % b
                    for c in range(8):
                        _mm(S, ps[:, :N], wout[:, c, dco * 128:(dco + 1) * 128], mix[:, c, :N], c == 0, c == 7,
                            ["wout", "mix"], [pk])
                    _stt(S, "dve", xs[:, dco, g0:g0 + N], ps[:, :N], g1[:, dco:dco + 1], xs[:, dco, g0:g0 + N],
                         ALU.mult, ALU.add, [pk, "par", "xs"], ["xs"])
        S.barrier()

        with ExitStack() as es:
            def sb(name, shape, dt):
                return es.enter_context(nc.sbuf_tensor("p2f_%d_%s" % (li, name), shape, dt))
            h2 = sb("h2", [128, 8, NT], BF16)
            sq = [sb("sq%d" % i, [128, 512], BF16) for i in range(2)]
            rstd = sb("rstd", [128, 512], F32)
            rtmp = sb("rtmp", [128, 512], F32)
            htmp = [sb("htmp%d" % i, [128, 512], F32) for i in range(2)]
            if not dense:
                rw = sb("rw", [128, 8, NEXP], F32)
                _dma(S, "sp", rw[:], Dm["router"].rearrange("(dc p) e -> p dc e", p=128), [], ["rw"])
                lg = sb("lg", [128, 32], F32)
                mx8 = sb("mx8", [128, 8], F32)
                nv1 = sb("nv1", [128, 1], F32)
                msk = sb("msk", [128, 8], F32)
                ex = sb("ex", [128, 8], F32)
                ssum = sb("ssum", [128, 1], F32)
                comb = sb("comb", [128, 8], F32)
                cTs = sb("cTs", [8, 512], F32)
            for (src, col, n0, N, g0) in blocks:
                a2 = par[:, col, 4, :]
                sh2 = par[:, col, 3, :]
                for dc in range(8):
                    sqt, sqk = sq[dc % 2], "sq%d" % (dc % 2)
                    _tt(S, "pool", sqt[:, :N], xs[:, dc, g0:g0 + N], xs[:, dc, g0:g0 + N], ALU.mult, ["xs"], [sqk])
                    _mm(S, PB[0][:, :N], ones[:], sqt[:, :N], dc == 0, dc == 7, [sqk, "ones"], ["pb0"])
                _act(S, rtmp[:, :N], PB[0][:, :N], AF.Sqrt, ["pb0"], ["rtmp"], scale=1.0 / D, bias=K.eps_ap)
                _recip(S, rstd[:, :N], rtmp[:, :N], ["rtmp"], ["rstd"])
                for dc in range(8):
                    ht, hk = htmp[dc % 2], "htmp%d" % (dc % 2)
                    _stt(S, "dve", ht[:, :N], xs[:, dc, g0:g0 + N], a2[:, dc:dc + 1], rstd[:, :N], ALU.mult, ALU.mult,
                         ["xs", "par", "rstd"], [hk])
                    if dense:
                        _act(S, h2[:, dc, g0:g0 + N], ht[:, :N], AF.Identity, [hk, "par"], ["h2"], bias=sh2[:, dc:dc + 1])
                    else:
                        _act(S, ht[:, :N], ht[:, :N], AF.Identity, [hk, "par"], [hk], bias=sh2[:, dc:dc + 1])
                        _cp(S, "pool", h2[:, dc, g0:g0 + N], ht[:, :N], [hk], ["h2"])
                        for t in range(N // 128):
                            _mm(S, PB[1 + t][:, 0:8], ht[:, t * 128:(t + 1) * 128], rw[:, dc, :], dc == 0, dc == 7,
                                [hk, "rw"], ["pb%d" % (1 + t)])
                if not dense:
                    for t in range(N // 128):
                        _cp(S, "dve", lg[:, t * 8:(t + 1) * 8], PB[1 + t][:, 0:8], ["pb%d" % (1 + t)], ["lg"])
                    if "dbg_lg" in Dm:
                        _dma(S, "sp", Dm["dbg_lg"][:, (n0 // 512) * 32:(n0 // 512 + 1) * 32], lg[:], ["lg"], ["dbg_lg"])
                    for t in range(N // 128):
                        lsl = slice(t * 8, (t + 1) * 8)
                        S.op("dve", lambda e, lsl=lsl: e.max(out=mx8[:], in_=lg[:, lsl]), reads=["lg"], writes=["mx8"])
                        _ts(S, "dve", nv1[:], mx8[:, 0:1], -1.0, None, ALU.mult, None, ["mx8"], ["nv1"])
                        _ts(S, "dve", msk[:], lg[:, lsl], mx8[:, 1:2], None, ALU.is_ge, None, ["lg", "mx8"], ["msk"])
                        _act(S, ex[:], lg[:, lsl], AF.Exp, ["lg", "nv1"], ["ex"], bias=nv1[:, 0:1])
                        _tt(S, "dve", ex[:], ex[:], msk[:], ALU.mult, ["ex", "msk"], ["ex"])
                        S.op("dve", lambda e: e.reduce_sum(out=ssum[:], in_=ex[:], axis=mybir.AxisListType.X),
                             reads=["ex"], writes=["ssum"])
                        _recip(S, ssum[:], ssum[:], ["ssum"], ["ssum"])
                        _ts(S, "dve", comb[:], ex[:], ssum[:, 0:1], None, ALU.mult, None, ["ex", "ssum"], ["comb"])
                        _mm(S, PB[5][0:8, t * 128:(t + 1) * 128], comb[:], ident, True, True, ["comb", "cst"], ["pb5"])
                    _cp(S, "dve", cTs[:, :N], PB[5][0:8, :N], ["pb5"], ["cTs"])
                    _dma(S, "sp", Dm["combT"][:, n0:n0 + N], cTs[:, :N], ["cTs"], ["combT"])
            GS = 4
            wg = [sb("wg%d" % i, [128, 8, GS * 128], BF16) for i in range(2)]
            wu = [sb("wu%d" % i, [128, 8, GS * 128], BF16) for i in range(2)]
            wd = [sb("wd%d" % i, [128, GS, D], BF16) for i in range(2)]
            actb = [sb("actb%d" % i, [128, GS, 512], BF16) for i in range(2)]
            sg = [sb("sg%d" % i, [128, 512], BF16) for i in range(2)]
            ytmp = sb("ytmp", [128, 512], F32)
            if not dense:
                cb = [sb("cb%d" % i, [128, NL], F32) for i in range(2)]
            g2 = None
            nexp = 1 if dense else NEXP
            dff = DFF0 if dense else DFFE
            nch = dff // 128
            gctr = 0
            actr = 0
            for ex_i in (range(nexp) if "noffn" not in os.environ.get("MK_P2DBG%d" % li, "") else []):
                if dense:
                    Wg, Wu, Wd = Dm["ffn_g"], Dm["ffn_u"], Dm["ffn_d"]
                else:
                    Wg, Wu, Wd = Dm["exp_g"][ex_i], Dm["exp_u"][ex_i], Dm["exp_d"][ex_i]
                    cbb, cbk = cb[ex_i % 2], "cb%d" % (ex_i % 2)
                    _dma(S, "sp", cbb[:], Dm["combT"][ex_i:ex_i + 1, :].partition_broadcast(128), ["combT"], [cbk])
                for f0 in range(0, nch, GS):
                    gs = min(GS, nch - f0)
                    wi = gctr % 2
                    gctr += 1
                    kg, ku, kd = "wg%d" % wi, "wu%d" % wi, "wd%d" % wi
                    _dma(S, "pool", wg[wi][:, :, :gs * 128],
                         Wg[:, f0 * 128:(f0 + gs) * 128].rearrange("(dc p) n -> p dc n", p=128), [], [kg])
                    _dma(S, "pool", wu[wi][:, :, :gs * 128],
                         Wu[:, f0 * 128:(f0 + gs) * 128].rearrange("(dc p) n -> p dc n", p=128), [], [ku])
                    _dma(S, "pool", wd[wi][:, :gs, :],
                         Wd[f0 * 128:(f0 + gs) * 128, :].rearrange("(j p) n -> p j n", p=128), [], [kd])
                    for (src, col, n0, N, g0) in blocks:
                        g2 = par[:, col, 5, :]
                        ai = actr % 2
                        actr += 1
                        ak = "actb%d" % ai
                        for j in range(gs):
                            b = j % 2
                            pg, pgk = PB[b], "pb%d" % b
                            pu, puk = PB[2 + b], "pb%d" % (2 + b)
                            for dc in range(8):
                                _mm(S, pg[:, :N], wg[wi][:, dc, j * 128:(j + 1) * 128], h2[:, dc, g0:g0 + N], dc == 0, dc == 7,
                                    [kg, "h2"], [pgk])
                            for dc in range(8):
                                _mm(S, pu[:, :N], wu[wi][:, dc, j * 128:(j + 1) * 128], h2[:, dc, g0:g0 + N], dc == 0, dc == 7,
                                    [ku, "h2"], [puk])
                            _act(S, sg[b][:, :N], pg[:, :N], AF.Silu, [pgk], ["sg%d" % b])
                            _tt(S, "dve", actb[ai][:, j, :N], sg[b][:, :N], pu[:, :N], ALU.mult, ["sg%d" % b, puk], [ak])
                        for dco in range(8):
                            b = 4 + dco % 4
                            py, pyk = PB[b], "pb%d" % b
                            for j in range(gs):
                                _mm(S, py[:, :N], wd[wi][:, j, dco * 128:(dco + 1) * 128], actb[ai][:, j, :N], j == 0, j == gs - 1,
                                    [kd, ak], [pyk])
                            if dense:
                                _stt(S, "dve", xs[:, dco, g0:g0 + N], py[:, :N], g2[:, dco:dco + 1], xs[:, dco, g0:g0 + N],
                                     ALU.mult, ALU.add, [pyk, "par", "xs"], ["xs"])
                            else:
                                _tt(S, "dve", ytmp[:, :N], py[:, :N], cbb[:, n0:n0 + N], ALU.mult, [pyk, cbk], ["ytmp"])
                                _stt(S, "dve", xs[:, dco, g0:g0 + N], ytmp[:, :N], g2[:, dco:dco + 1], xs[:, dco, g0:g0 + N],
                                     ALU.mult, ALU.add, ["ytmp", "par", "xs"], ["xs"])
            if is_last:
                fing = sb("fing", [128, 8], F32)
                _dma(S, "sp", fing[:], Dm["fin_g"], [], ["fing"])
                K.final_dmas = []
                for (src, col, n0, N, g0) in blocks:
                    if src != "x":
                        continue
                    for dc in range(8):
                        sqt, sqk = sq[dc % 2], "sq%d" % (dc % 2)
                        _tt(S, "pool", sqt[:, :N], xs[:, dc, g0:g0 + N], xs[:, dc, g0:g0 + N], ALU.mult, ["xs"], [sqk])
                        _mm(S, PB[0][:, :N], ones[:], sqt[:, :N], dc == 0, dc == 7, [sqk, "ones"], ["pb0"])
                    _act(S, rtmp[:, :N], PB[0][:, :N], AF.Sqrt, ["pb0"], ["rtmp"], scale=1.0 / D, bias=K.eps_ap)
                    _recip(S, rstd[:, :N], rtmp[:, :N], ["rtmp"], ["rstd"])
                    for dc in range(8):
                        ht, hk = htmp[dc % 2], "htmp%d" % (dc % 2)
                        _stt(S, "dve", ht[:, :N], xs[:, dc, g0:g0 + N], fing[:, dc:dc + 1], rstd[:, :N], ALU.mult, ALU.mult,
                             ["xs", "fing", "rstd"], [hk])
                        d = _dma(S, "sp", Dm["outT"][dc * 128:(dc + 1) * 128, n0:n0 + N], ht[:, :N], [hk], ["outT"])
                        K.final_dmas.append(d)
            else:
                K.final_dmas = []
                d = _dma(S, "sp", K.xout.rearrange("(dc p) n -> p dc n", p=128), xs[:, :, 0:NL], ["xs"], [K.xout_key])
                K.final_dmas.append(d)
                d = _dma(S, "sp", K.cout.rearrange("(dc p) n -> p dc n", p=128), xs[:, :, NL:NT], ["xs"], [K.cout_key])
                K.final_dmas.append(d)
        S.barrier()


def build(mode, li):
    nc = bass.Bass("TRN2", target_bir_lowering=False)
    K = Ctx()
    K.nc = nc
    K.S = Sched(nc, same_engine_sync=not os.environ.get('MK_NOSES'))
    S = K.S
    _CUR[0] = S
    layers = [li] if mode != "FUSED" else list(range(DEPTH))
    K.nlay = len(layers)
    K.has_dense = any(l % 2 == 0 for l in layers)
    K.has_moe = any(l % 2 == 1 for l in layers)
    K.has_last = (DEPTH - 1) in layers
    make_dram(K, mode)
    Dm = K.dram
    with ExitStack() as es:
        K.pb = [es.enter_context(nc.psum_tensor("pb%d" % i, [128, 512], F32)) for i in range(8)]
        cst = es.enter_context(nc.sbuf_tensor("cst_sb", [128, 640], F32))
        _dma(S, "sp", cst[:], Dm["cst"], [], ["cst"])
        K.cst = cst
        ones_bf = es.enter_context(nc.sbuf_tensor("ones_bf", [128, 128], BF16))
        _memset(S, "dve", ones_bf[:], 1.0, ["ones"])
        K.ones_bf = ones_bf
        ones_f = es.enter_context(nc.sbuf_tensor("ones_f", [128, 128], F32))
        _memset(S, "dve", ones_f[:], 1.0, ["onesf"])
        K.ones_f = ones_f
        cvec = es.enter_context(nc.sbuf_tensor("cvec", [128, 2], F32))
        _memset(S, "dve", cvec[:, 0:1], EPS, ["cvec"])
        _memset(S, "dve", cvec[:, 1:2], 1.0, ["cvec"])
        K.eps_ap = cvec[:, 0:1]
        K.one_ap = cvec[:, 1:2]
        K.final_dmas = []
        if mode == "P1":
            try:
                phase1(K, 0, Dm["xT"], "xT", Dm["ctxT"], "ctxT")
            except _Stop:
                pass
        elif mode == "P2":
            if not K.has_last:
                K.xout, K.xout_key = Dm["xT_out"], "xT_out"
                K.cout, K.cout_key = Dm["ctxT_out"], "ctxT_out"
            phase2(K, li, 0, Dm["xT"], "xT", Dm["ctxT"], "ctxT", K.has_last)
        else:
            K.xout, K.xout_key = Dm["xT_out"], "xT_out"
            K.cout, K.cout_key = Dm["ctxT_out"], "ctxT_out"
            groups = [[0, 1, 2, 3], [4, 5, 6, 7]]
            for l in range(DEPTH):
                if l == 0:
                    xin, xkey, cin, ckey = Dm["xT"], "xT", Dm["ctxT"], "ctxT"
                else:
                    xin, xkey, cin, ckey = Dm["xT_out"], "xT_out", Dm["ctxT_out"], "ctxT_out"
                phase1(K, l, xin, xkey, cin, ckey)
                for nm in ("kl", "kr", "v", "L", "A"):
                    src, dst = Dm["x_" + nm], Dm["g_" + nm]
                    S.coll("pool", lambda e, src=src, dst=dst: e.collective_compute(
                        "AllGather", ALU.bypass, replica_groups=groups, ins=[src.opt()], outs=[dst.opt()]),
                        reads=["x_" + nm], writes=["g_" + nm])
                S.barrier()
                phase2(K, l, l, xin, xkey, cin, ckey, l == DEPTH - 1)
        S.emit()
    return nc


def _bf(a):
    return np.ascontiguousarray(a)


_PERM = np.concatenate([np.arange(16, 32), np.arange(0, 16), np.arange(48, 64), np.arange(32, 48)])


def host_prep(inp):
    f = lambda a: np.ascontiguousarray(np.asarray(a, dtype=np.float32))
    W = {}
    nl = DEPTH
    w_in = f(inp["w_in"])
    W["w_in"] = np.ascontiguousarray(np.concatenate([w_in, w_in[:, :, 1952:2016][:, :, _PERM]], axis=2))
    W["w_mod"] = f(inp["w_mod"])
    W["bmodT"] = np.ascontiguousarray(f(inp["b_mod"]).reshape(nl, 48, 128).transpose(0, 2, 1))
    ln1 = f(inp["ln1_g"]).reshape(nl, 8, 128).transpose(0, 2, 1)
    ln2 = f(inp["ln2_g"]).reshape(nl, 8, 128).transpose(0, 2, 1)
    W["lnT"] = np.ascontiguousarray(np.concatenate([ln1, ln2], axis=2))
    wg2 = np.zeros((nl, 33, 512), np.float32)
    g2 = f(inp["w_gla_g2"])
    b2 = f(inp["b_gla_g2"])
    wg2[:, 0:16, 0:256] = g2[:, 0]
    wg2[:, 16:32, 256:512] = g2[:, 1]
    wg2[:, 32, 0:256] = b2[:, 0]
    wg2[:, 32, 256:512] = b2[:, 1]
    W["wg2"] = wg2
    gvec = np.zeros((nl, 128, 8), np.float32)
    gvec[:, :, 0] = f(inp["gla_norm_g"])
    qn = f(inp["mla_q_norm_g"])
    gvec[:, :, 1] = qn[:, 0:128]
    gvec[:, :, 2] = qn[:, 128:256]
    gvec[:, :, 3] = f(inp["mla_kv_norm_g"])
    W["gvec"] = gvec
    W["kvg_row"] = np.ascontiguousarray(f(inp["mla_kv_norm_g"]).reshape(nl, 1, 128))
    wuq = f(inp["w_uq"])
    wq = np.zeros((nl, 256, 1024), np.float32)
    for h in range(4):
        wq[:, :, h * 128:(h + 1) * 128] = wuq[:, :, 192 * h:192 * h + 128]
        rp = wuq[:, :, 192 * h + 128:192 * h + 192]
        wq[:, :, 512 + h * 64:512 + (h + 1) * 64] = rp
        wq[:, :, 768 + h * 64:768 + (h + 1) * 64] = rp[:, :, _PERM]
    W["w_uq"] = wq
    wukv = f(inp["w_ukv"])
    ukT = np.zeros((nl, 128, 512), np.float32)
    uv = np.zeros((nl, 128, 512), np.float32)
    for h in range(4):
        ukT[:, :, h * 128:(h + 1) * 128] = wukv[:, :, 256 * h:256 * h + 128].transpose(0, 2, 1)
        uv[:, :, h * 128:(h + 1) * 128] = wukv[:, :, 256 * h + 128:256 * h + 256]
    W["w_ukT"] = ukT
    W["w_uv"] = uv
    W["w_out"] = f(inp["w_out"])
    W["ffn_g"] = f(inp["ffn_w_gate"])[0]
    W["ffn_u"] = f(inp["ffn_w_up"])[0]
    W["ffn_d"] = f(inp["ffn_w_down"])[0]
    W["router"] = f(inp["router_w"])[0]
    W["exp_g"] = f(inp["exp_w_gate"])[0]
    W["exp_u"] = f(inp["exp_w_up"])[0]
    W["exp_d"] = f(inp["exp_w_down"])[0]
    W["fin_g"] = np.ascontiguousarray(f(inp["final_norm_g"]).reshape(8, 128).T)
    i = np.arange(128)
    ident = (i[:, None] == i[None, :]).astype(np.float32)
    triF = (i[:, None] <= i[None, :]).astype(np.float32)
    triB = (i[:, None] >= i[None, :]).astype(np.float32)
    triSU = (i[:, None] > i[None, :]).astype(np.float32)
    triSL = (i[:, None] < i[None, :]).astype(np.float32)
    W["cst"] = np.ascontiguousarray(np.concatenate([ident, triF, triB, triSU, triSL], axis=1))
    x = f(inp["x"])
    c = f(inp["c"])
    ctx = f(inp["ctx"])
    cc = f(inp["c_ctx"])
    inv = (np.float32(10000.0) ** (-np.arange(16, dtype=np.float32) / np.float32(16))).astype(np.float32)
    cores = []
    for core in range(8):
        b, j = core // 4, core % 4
        t0 = j * NL
        P = {}
        P["xT"] = np.ascontiguousarray(x[b, t0:t0 + NL].T)
        P["ctxT"] = np.ascontiguousarray(ctx[b].T)
        P["cT"] = np.ascontiguousarray(np.stack([c[b], cc], axis=1))
        pos = np.arange(t0, t0 + NL)
        row = (pos // 64).astype(np.float32)
        colp = (pos % 64).astype(np.float32)
        ar = (inv[:, None] * row[None, :]).astype(np.float32)
        ac = (inv[:, None] * colp[None, :]).astype(np.float32)
        P["ropec"] = np.ascontiguousarray(np.concatenate([np.cos(ar), np.cos(ar), np.cos(ac), np.cos(ac)], axis=0).astype(np.float32))
        P["ropes"] = np.ascontiguousarray(np.concatenate([-np.sin(ar), np.sin(ar), -np.sin(ac), np.sin(ac)], axis=0).astype(np.float32))
        rm = np.zeros((64, 8), np.float32)
        for r in range(4):
            rm[:, r] = 1.0 if r < j else 0.0
            rm[:, 4 + r] = 1.0 if r > j else 0.0
        P["rmask"] = rm
        cores.append(P)
    return W, cores


_LAYER_KEYS = ["w_mod", "bmodT", "lnT", "w_in", "wg2", "gvec", "kvg_row", "w_uq", "w_ukT", "w_uv", "w_out"]
_NC_CACHE = {}


def _get_nc(mode, li):
    key = (mode, li)
    if key not in _NC_CACHE:
        _NC_CACHE[key] = build(mode, li)
    return _NC_CACHE[key]


def _run(mode, li, W, per_core):
    nc = _get_nc(mode, li)
    names = [n for n, ap in _dram_inputs(nc)]
    in_maps = []
    for core in range(8):
        m = {}
        for n in names:
            if n in per_core[core]:
                m[n] = per_core[core][n]
            elif n in _LAYER_KEYS and mode != "FUSED":
                m[n] = W[n][li:li + 1]
            else:
                m[n] = W[n]
        in_maps.append(m)
    ncores = int(os.environ.get("MK_NCORES", "8"))
    res = run_bass_kernel_spmd(nc, in_maps[:ncores], core_ids=list(range(ncores)))
    return res.results


_INPUT_NAMES = {}


def _dram_inputs(nc):
    return [(n, None) for n in _INPUT_NAMES[id(nc)]]


def kernel_unfused(**inp):
    W, cores = host_prep(inp)
    out = np.zeros((2, 8192, D), np.float32)
    state = [dict(xT=cores[i]["xT"], ctxT=cores[i]["ctxT"]) for i in range(8)]
    for li in range(DEPTH):
        pc = []
        for i in range(8):
            m = dict(cores[i])
            m.update(state[i])
            pc.append(m)
        r1 = _run("P1", li, W, pc)
        for i in range(8):
            b = i // 4
            grp = [r1[4 * b + r] for r in range(4)]
            for k in r1[i]:
                if k.startswith("st_") or k.startswith("c_"):
                    pc[i][k] = r1[i][k]
            pc[i]["g_kl"] = np.concatenate([g["x_kl"] for g in grp], axis=0)
            pc[i]["g_kr"] = np.concatenate([g["x_kr"] for g in grp], axis=0)
            pc[i]["g_v"] = np.concatenate([g["x_v"] for g in grp], axis=0)
            pc[i]["g_L"] = np.concatenate([g["x_L"] for g in grp], axis=0)
            pc[i]["g_A"] = np.concatenate([g["x_A"] for g in grp], axis=0)
        r2 = _run("P2", li, W, pc)
        if li == DEPTH - 1:
            for i in range(8):
                b, j = i // 4, i % 4
                out[b, j * NL:(j + 1) * NL, :] = r2[i]["outT"].T
        else:
            for i in range(8):
                state[i] = dict(xT=r2[i]["xT_out"], ctxT=r2[i]["ctxT_out"])
    return out


def kernel(**inp):
    W, cores = host_prep(inp)
    r = _run("FUSED", 0, W, cores)
    out = np.zeros((2, 8192, D), np.float32)
    for i in range(8):
        b, j = i // 4, i % 4
        out[b, j * NL:(j + 1) * NL, :] = r[i]["outT"].T
    return out
```

```python
import math
from contextlib import ExitStack

import numpy as np
import ml_dtypes

import concourse.bass as bass
import concourse.mybir as mybir
from concourse.bass_utils import run_bass_kernel_spmd

F32 = mybir.dt.float32
BF16 = mybir.dt.bfloat16
AF = mybir.ActivationFunctionType
ALU = mybir.AluOpType

D = 1024
NL = 2048
NCX = 256
NT = NL + NCX
DEPTH = 2
EPS = 1e-6
DIN = 2016
DINA = 2080
NKEY = NCX + 4 * NL
NKT = NKEY // 128
DFF0 = 2816
DFFE = 3584
NEXP = 8
MLA_SCALE = (128 + 64) ** -0.5

ENG_NAMES = ("pe", "act", "dve", "pool", "sp")


class Op:
    __slots__ = ("eng", "fn", "reads", "writes", "is_dma", "idx", "waits", "marked",
                 "semval", "dma_sem", "dma_val")

    def __init__(self, eng, fn, reads, writes, is_dma):
        self.eng = eng
        self.fn = fn
        self.reads = reads
        self.writes = writes
        self.is_dma = is_dma
        self.waits = []
        self.marked = False
        self.semval = 0
        self.dma_sem = None
        self.dma_val = 0


class Sched:
    def __init__(self, nc, n_dma_sems=12, same_engine_sync=True):
        self.nc = nc
        self.ops = {e: [] for e in ENG_NAMES}
        self.order = []
        self.n_dma_sems = n_dma_sems
        self.same_engine_sync = same_engine_sync
        self.cc_ops = set()

    muted = False

    def op(self, eng, fn, reads=(), writes=()):
        writes = tuple(writes) + tuple(k for k in reads if k.startswith("pb") and k not in writes)
        o = Op(eng, fn, tuple(reads), tuple(writes), False)
        if self.muted:
            return o
        o.idx = len(self.ops[eng])
        self.ops[eng].append(o)
        self.order.append(o)
        return o

    def dma(self, eng, fn, reads=(), writes=()):
        o = Op(eng, fn, tuple(reads), tuple(writes), True)
        if self.muted:
            return o
        o.idx = len(self.ops[eng])
        self.ops[eng].append(o)
        self.order.append(o)
        return o

    def barrier(self):
        self.order.append("BARRIER")

    def coll(self, eng, fn, reads=(), writes=()):
        o = self.dma(eng, fn, reads, writes)
        self.cc_ops.add(id(o))
        return o

    def analyze(self):
        last_w = {}
        readers = {}
        seen = {e: {f: -1 for f in ENG_NAMES} for e in ENG_NAMES}
        seen_dma = {e: set() for e in ENG_NAMES}
        dma_ctr = {e: 0 for e in ENG_NAMES}
        dma_cnt = {}
        dma_last = {}
        bar_deps = []
        bar_pending = {e: False for e in ENG_NAMES}
        last_op = {}
        for o in self.order:
            if isinstance(o, str):
                bar_deps = list(last_op.values()) + list(dma_last.values())
                bar_pending = {e: True for e in ENG_NAMES}
                continue
            deps = []
            if bar_pending[o.eng]:
                deps.extend(bar_deps)
                bar_pending[o.eng] = False
            if not o.is_dma:
                last_op[o.eng] = o
            for k in o.reads:
                w = last_w.get(k)
                if w is not None:
                    deps.append(w)
            for k in o.writes:
                w = last_w.get(k)
                if w is not None:
                    deps.append(w)
                for r in readers.get(k, ()):
                    deps.append(r)
            if o.is_dma:
                e = o.eng
                if id(o) in self.cc_ops:
                    sidx = ("cc", 0)
                    inc = 1
                else:
                    sidx = (e, dma_ctr[e] % self.n_dma_sems)
                    dma_ctr[e] += 1
                    inc = 16
                prev = dma_last.get(sidx)
                if prev is not None:
                    deps.append(prev)
                dma_cnt[sidx] = dma_cnt.get(sidx, 0) + inc
                o.dma_sem = sidx
                o.dma_val = dma_cnt[sidx]
                dma_last[sidx] = o
            need = {}
            for d in deps:
                if d is o:
                    continue
                if d.is_dma:
                    if id(d) in seen_dma[o.eng]:
                        continue
                    need[("dma", id(d))] = d
                else:
                    if d.eng == o.eng and not o.is_dma:
                        if o.eng == "pe" or not self.same_engine_sync:
                            continue
                    if seen[o.eng][d.eng] >= d.idx:
                        continue
                    cur = need.get(("eng", d.eng))
                    if cur is None or cur.idx < d.idx:
                        need[("eng", d.eng)] = d
            for key, d in need.items():
                d.marked = True
                o.waits.append(d)
                if d.is_dma:
                    seen_dma[o.eng].add(id(d))
                else:
                    seen[o.eng][d.eng] = d.idx
            for k in o.writes:
                last_w[k] = o
                readers[k] = []
            for k in o.reads:
                if k not in o.writes:
                    readers.setdefault(k, []).append(o)
        self.dma_last_all = list(dma_last.values())
        for e in ENG_NAMES:
            c = 0
            for o in self.ops[e]:
                if not o.is_dma and o.marked:
                    c += 1
                    o.semval = c

    def emit(self, final_wait_ops=()):
        nc = self.nc
        self.analyze()
        with ExitStack() as es:
            esem = {e: es.enter_context(nc.semaphore("sem_" + e)) for e in ENG_NAMES}
            dsem = {}
            for e in ENG_NAMES:
                if any(o.is_dma for o in self.ops[e]):
                    for i in range(self.n_dma_sems):
                        dsem[(e, i)] = es.enter_context(nc.semaphore("dsem_%s_%d" % (e, i)))
            if self.cc_ops:
                dsem[("cc", 0)] = es.enter_context(nc.semaphore("ccsem"))
            block = es.enter_context(nc.Block())
            hw = {"pe": block.tensor, "act": block.scalar, "dve": block.vector,
                  "pool": block.gpsimd, "sp": block.sync}

            def make(e):
                def body(eng):
                    for o in self.ops[e]:
                        for d in o.waits:
                            if d.is_dma:
                                eng.wait_ge(dsem[d.dma_sem], d.dma_val)
                            else:
                                eng.wait_ge(esem[d.eng], d.semval)
                        ins = o.fn(eng)
                        if o.is_dma and id(o) in self.cc_ops:
                            ins.then_inc(dsem[o.dma_sem])
                        elif o.is_dma:
                            ins.then_inc(dsem[o.dma_sem], 16)
                        elif o.marked:
                            ins.then_inc(esem[e], 1)
                    if e == "sp":
                        for d in self.dma_last_all:
                            eng.wait_ge(dsem[d.dma_sem], d.dma_val)
                return body

            for e in ENG_NAMES:
                if self.ops[e] or e == "sp":
                    hw[e](make(e))


class Ctx:
    pass


class _Stop(Exception):
    pass


import os
_STOP = float(os.environ.get("MK_STOP", "99"))


_CUR = [None]


def _stage(n):
    if _STOP <= n:
        _CUR[0].muted = True


def _mm(S, out, lhsT, rhs, start, stop, r, w):
    S.op("pe", lambda e: e.matmul(out, lhsT=lhsT, rhs=rhs, start=start, stop=stop), reads=r, writes=w)


def _act(S, out, in_, func, r, w, scale=1.0, bias=None, accum=None):
    def f(e):
        kw = {}
        if bias is not None:
            kw["bias"] = bias
        if accum is not None:
            kw["accum_out"] = accum
        return e.activation(out=out, in_=in_, func=func, scale=scale, **kw)
    S.op("act", f, reads=r, writes=w)


def _tt(S, eng, out, in0, in1, op, r, w):
    S.op(eng, lambda e: e.tensor_tensor(out=out, in0=in0, in1=in1, op=op), reads=r, writes=w)


def _stt(S, eng, out, in0, scalar, in1, op0, op1, r, w):
    S.op(eng, lambda e: e.scalar_tensor_tensor(out=out, in0=in0, scalar=scalar, in1=in1, op0=op0, op1=op1),
         reads=r, writes=w)


def _ts(S, eng, out, in0, s1, s2, op0, op1, r, w):
    if s2 is None:
        S.op(eng, lambda e: e.tensor_scalar(out=out, in0=in0, scalar1=s1, scalar2=None, op0=op0), reads=r, writes=w)
    else:
        S.op(eng, lambda e: e.tensor_scalar(out=out, in0=in0, scalar1=s1, scalar2=s2, op0=op0, op1=op1),
             reads=r, writes=w)


def _cp(S, eng, out, in_, r, w):
    if eng == "act":
        S.op(eng, lambda e: e.activation(out=out, in_=in_, func=AF.Identity, scale=1.0), reads=r, writes=w)
    else:
        S.op(eng, lambda e: e.tensor_copy(out=out, in_=in_), reads=r, writes=w)


def _dma(S, q, out, in_, r, w):
    return S.dma(q, lambda e: e.dma_start(out=out, in_=in_), reads=r, writes=w)


def _memset(S, eng, ap, val, w):
    S.op(eng, lambda e: e.memset(ap, val), writes=w)


def _recip(S, out, in_, r, w):
    S.op("dve", lambda e: e.reciprocal(out=out, in_=in_), reads=r, writes=w)


def make_dram(K, mode):
    nc = K.nc
    K.dram = {}
    P1 = mode in ("P1", "FUSED")
    P2 = mode in ("P2", "FUSED")

    def decl(name, shape, dt, role, need=True):
        if not need:
            return
        if role == "in":
            kind = "ExternalInput"
        elif role == "st":
            kind = {"P1": "ExternalOutput", "P2": "ExternalInput", "FUSED": "Internal"}[mode]
        elif role == "own":
            kind = {"P1": "ExternalOutput", "P2": None, "FUSED": "Internal"}[mode]
        elif role == "gat":
            kind = {"P1": None, "P2": "ExternalInput", "FUSED": "Internal"}[mode]
        elif role == "xout":
            kind = {"P1": None, "P2": "ExternalOutput", "FUSED": "Internal"}[mode]
        elif role == "p2s":
            kind = {"P1": None, "P2": "Internal", "FUSED": "Internal"}[mode]
        elif role == "out":
            kind = "ExternalOutput"
        if kind is None:
            return
        if kind == "Internal":
            t = nc.dram_tensor(name, shape, dt)
        else:
            t = nc.dram_tensor(name, shape, dt, kind=kind)
        if kind == "ExternalInput":
            _INPUT_NAMES.setdefault(id(nc), []).append(name)
        K.dram[name] = t.ap()

    nlay = K.nlay
    decl("xT", [D, NL], F32, "in")
    decl("ctxT", [D, NCX], F32, "in")
    decl("cT", [D, 2], F32, "in", P1)
    decl("cst", [128, 5 * 128], F32, "in")
    decl("rmask", [64, 8], F32, "in", P2)
    decl("ropec", [64, NL], F32, "in", P1)
    decl("ropes", [64, NL], F32, "in", P1)
    decl("w_mod", [nlay, D, 6 * D], F32, "in", P1)
    decl("bmodT", [nlay, 128, 48], F32, "in", P1)
    decl("lnT", [nlay, 128, 16], F32, "in", P1)
    decl("w_in", [nlay, D, DINA], F32, "in", P1)
    decl("wg2", [nlay, 33, 512], F32, "in", P1)
    decl("gvec", [nlay, 128, 8], F32, "in")
    decl("kvg_row", [nlay, 1, 128], F32, "in", P1)
    decl("w_uq", [nlay, 256, 1024], F32, "in", P1)
    decl("w_ukT", [nlay, 128, 512], F32, "in", P1)
    decl("w_uv", [nlay, 128, 512], F32, "in", P2)
    decl("w_out", [nlay, D, D], F32, "in", P2)
    decl("ffn_g", [D, DFF0], F32, "in", P2 and K.has_dense)
    decl("ffn_u", [D, DFF0], F32, "in", P2 and K.has_dense)
    decl("ffn_d", [DFF0, D], F32, "in", P2 and K.has_dense)
    decl("router", [D, NEXP], F32, "in", P2 and K.has_moe)
    decl("exp_g", [NEXP, D, DFFE], F32, "in", P2 and K.has_moe)
    decl("exp_u", [NEXP, D, DFFE], F32, "in", P2 and K.has_moe)
    decl("exp_d", [NEXP, DFFE, D], F32, "in", P2 and K.has_moe)
    decl("combT", [NEXP, NL], F32, "xout", P2 and K.has_moe)
    decl("fin_g", [128, 8], F32, "in", P2 and K.has_last)
    decl("dbg_lg", [128, 128], F32, "out", mode == "P2" and K.has_moe and bool(os.environ.get("MK_DBGLG")))
    decl("st_qp", [128, 4, NT], BF16, "st")
    decl("st_qr", [64, 4, NT], BF16, "st")
    decl("st_oi", [128, 4, NT], F32, "st")
    decl("st_qf", [64, 4, NT], BF16, "st")
    decl("st_qb", [64, 4, NT], BF16, "st")
    decl("st_sr", [128, 4, NT], BF16, "st")
    decl("st_U", [64, 18, 8, 128], F32, "st")
    decl("st_dec", [64, 18, 8], F32, "st")
    decl("st_par", [128, 2, 6, 8], F32, "st")
    decl("st_par2", [128, 2, 6, 8], F32, "p2s", mode == "FUSED")
    decl("st_S", [64, 18, 8, 128], BF16, "p2s")
    decl("c_kl", [128, NCX], BF16, "st")
    decl("c_kr", [64, NCX], BF16, "st")
    decl("c_v", [NCX, 128], BF16, "st")
    decl("x_kl", [128, NL], BF16, "own")
    decl("x_kr", [64, NL], BF16, "own")
    decl("x_v", [NL, 128], BF16, "own")
    decl("x_L", [64, 1024], F32, "own")
    decl("x_A", [64, 8], F32, "own")
    decl("g_kl", [4 * 128, NL], BF16, "gat")
    decl("g_kr", [4 * 64, NL], BF16, "gat")
    decl("g_v", [4 * NL, 128], BF16, "gat")
    decl("g_L", [4 * 64, 1024], F32, "gat")
    decl("g_A", [4 * 64, 8], F32, "gat")
    decl("xT_out", [D, NL], F32, "xout", P2 and not (mode == "P2" and K.has_last))
    decl("ctxT_out", [D, NCX], F32, "xout", P2 and not (mode == "P2" and K.has_last))
    decl("outT", [D, NL], F32, "out", P2 and K.has_last)


def mod_stage_steps(K, lw, sb, bank, bkey, par, pkey, sT, skey, bmodT, bkey2, lnT, lkey):
    S, Dm = K.S, K.dram
    modraw = sb("modraw", [128, 96], F32)
    wm = [sb("wm%d" % i, [128, 8, 256], F32) for i in range(2)]
    mk = "modraw_%d" % lw

    def step(j):
        buf = wm[j % 2]
        key = "wm%d_%d" % (j % 2, lw)
        _dma(S, "sp", buf[:], Dm["w_mod"][lw, :, j * 256:(j + 1) * 256].rearrange("(dc p) n -> p dc n", p=128),
             [], [key])
        for f in range(2):
            fc = j * 2 + f
            for dc in range(8):
                _mm(S, bank[:, fc * 2:fc * 2 + 2], buf[:, dc, f * 128:(f + 1) * 128], sT[:, dc, :],
                    dc == 0, dc == 7, [key, skey], [bkey])

    def fin():
        _cp(S, "dve", modraw[:], bank[:, 0:96], [bkey], [mk])
        mr = modraw[:].rearrange("p (f c) -> p c f", c=2)
        for col in range(2):
            _tt(S, "dve", par[:, col, :, :].rearrange("p a b -> p (a b)"), mr[:, col, :], bmodT[:], ALU.add,
                [mk, bkey2], [pkey])
        for col in range(2):
            _stt(S, "dve", par[:, col, 1, :], par[:, col, 1, :], 1.0, lnT[:, 0:8], ALU.add, ALU.mult,
                 [pkey, lkey], [pkey])
            _stt(S, "dve", par[:, col, 4, :], par[:, col, 4, :], 1.0, lnT[:, 8:16], ALU.add, ALU.mult,
                 [pkey, lkey], [pkey])

    return [(lambda j=j: step(j)) for j in range(24)], fin


BLOCKS = [("x", 0, i * 512, 512, i * 512) for i in range(4)] + [("c", 1, 0, 256, NL)]


def phase1(K, lw, xin, xkey, cin, ckey):
    nc, S, Dm = K.nc, K.S, K.dram
    with ExitStack() as es:
        def sb(name, shape, dt):
            return es.enter_context(nc.sbuf_tensor("p1_%d_%s" % (lw, name), shape, dt))
        cst = K.cst
        triF = cst[:, 128:256]
        triB = cst[:, 256:384]
        triSU = cst[:, 384:512]
        triSL = cst[:, 512:640]
        PB = K.pb
        ones = K.ones_bf
        onesf = K.ones_f
        win = sb("win", [128, 8, DINA], BF16)
        for dc in range(8):
            _dma(S, "pool", win[:, dc, :], Dm["w_in"][lw, dc * 128:(dc + 1) * 128, :], [], ["win"])
        wuq = sb("wuq", [128, 2, 1024], BF16)
        for kc in range(2):
            _dma(S, "pool", wuq[:, kc, :], Dm["w_uq"][lw, kc * 128:(kc + 1) * 128, :], [], ["wuq"])
        wukT = sb("wukT", [128, 512], BF16)
        _dma(S, "pool", wukT[:], Dm["w_ukT"][lw], [], ["wukT"])
        wg2 = sb("wg2", [33, 512], F32)
        _dma(S, "sp", wg2[:], Dm["wg2"][lw], [], ["wg2"])
        gvec = sb("gvec", [128, 8], F32)
        _dma(S, "sp", gvec[:], Dm["gvec"][lw], [], ["gvec"])
        kvgb = sb("kvgb", [128, 128], F32)
        _dma(S, "sp", kvgb[:], Dm["kvg_row"][lw].partition_broadcast(128), [], ["kvgb"])
        lnT = sb("lnT", [128, 16], F32)
        _dma(S, "sp", lnT[:], Dm["lnT"][lw], [], ["lnT"])
        bmodT = sb("bmodT", [128, 48], F32)
        _dma(S, "sp", bmodT[:], Dm["bmodT"][lw], [], ["bmodT"])
        cT = sb("cT", [128, 8, 2], F32)
        _dma(S, "sp", cT[:], Dm["cT"].rearrange("(dc p) c -> p dc c", p=128), [], ["cT"])
        sT = sb("sT", [128, 8, 2], F32)
        _act(S, sT[:], cT[:], AF.Silu, ["cT"], ["sT"])
        par = sb("par", [128, 2, 6, 8], F32)
        if lw in K.par_pre:
            _dma(S, "sp", par[:], Dm["st_par2"], ["st_par2"], ["par"])
        else:
            steps, fin = mod_stage_steps(K, lw, sb, PB[0], "pb0", par, "par", sT, "sT", bmodT, "bmodT", lnT, "lnT")
            for st_ in steps:
                st_()
            fin()
        _dma(S, "sp", Dm["st_par"], par[:], ["par"], ["st_par"])
        _stage(1)
        xb = sb("xb", [128, 8, 512], F32)
        sq = [sb("sq%d" % i, [128, 512], BF16) for i in range(2)]
        rstd = sb("rstd", [128, 512], F32)
        rtmp = sb("rtmp", [128, 512], F32)
        hT = sb("hT", [128, 8, 512], BF16)
        htmp = [sb("htmp%d" % i, [128, 512], F32) for i in range(2)]
        qf = sb("qf", [64, 4, 512], F32)
        kf = sb("kf", [64, 4, 512], F32)
        srs = sb("srs", [128, 4, 512], BF16)
        cqf = sb("cqf", [128, 2, 512], F32)
        cqn = sb("cqn", [128, 2, 512], BF16)
        ckvf = sb("ckvf", [128, 512], F32)
        krf = sb("krf", [64, 2, 512], F32)
        codT = sb("codT", [33, 512], F32)
        _memset(S, "dve", codT[32:33, :], 1.0, ["codT"])
        vtm = sb("vtm", [128, 4, 512], BF16)
        ktm = sb("ktm", [128, 4, 256], F32)
        qnh = sb("qnh", [128, 512], BF16)
        qps = sb("qps", [128, 4, 512], BF16)
        qrs = sb("qrs", [64, 4, 512], BF16)
        rt1 = sb("rt1", [64, 512], F32)
        rt2 = sb("rt2", [64, 512], F32)
        cosb = sb("cosb", [64, 512], F32)
        sinb = sb("sinb", [64, 512], F32)
        kls = sb("kls", [128, 512], BF16)
        krs = sb("krs", [64, 512], BF16)
        vls = sb("vls", [128, 4, 128], BF16)
        vss = sb("vss", [128, 4], F32)
        vjunk = sb("vjunk", [128, 128], F32)
        vsq = sb("vsq", [128, 128], F32)
        vs1 = sb("vs1", [128, 4], F32)
        vs2 = sb("vs2", [128, 4], F32)
        Gn = sb("Gn", [128, 512], F32)
        Ge = sb("Ge", [128, 512], F32)
        E1 = sb("E1", [64, 2, 512], F32)
        E2 = sb("E2", [64, 2, 512], F32)
        E3 = sb("E3", [128, 512], F32)
        qtf = sb("qtf", [64, 4, 512], BF16)
        qtb = sb("qtb", [64, 4, 512], BF16)
        ktf = sb("ktf", [64, 4, 128], BF16)
        ktb = sb("ktb", [64, 4, 128], BF16)
        khf = sb("khf", [128, 256], BF16)
        khb = sb("khb", [128, 256], BF16)
        amt = sb("amt", [128, 128], BF16)
        am1 = sb("am1", [128, 128], F32)
        am2 = sb("am2", [128, 128], F32)
        ois = sb("ois", [128, 4, 512], F32)
        Us = sb("Us", [64, 8, 128], F32)
        decs = sb("decs", [64, 4, 8], F32)
        Lf = sb("Lf", [64, 4, 128], F32)
        Lb = sb("Lb", [64, 4, 128], F32)
        PA = sb("PA", [64, 8], F32)

        for (src, col, n0, N, g0) in BLOCKS:
            xdr = (xin if src == "x" else cin)
            ntile = N // 128
            a1 = par[:, col, 1, :]
            sh1 = par[:, col, 0, :]
            _dma(S, "sp", xb[:, :, :N], xdr[:, n0:n0 + N].rearrange("(dc p) n -> p dc n", p=128),
                 [xkey if src == "x" else ckey], ["xb"])
            for dc in range(8):
                sqb, sqk = sq[dc % 2], "sq%d" % (dc % 2)
                _tt(S, "pool", sqb[:, :N], xb[:, dc, :N], xb[:, dc, :N], ALU.mult, ["xb"], [sqk])
                _mm(S, PB[2][:, :N], ones[:], sqb[:, :N], dc == 0, dc == 7, [sqk, "ones"], ["pb2"])
            _stage(1.3)
            _act(S, rtmp[:, :N], PB[2][:, :N], AF.Sqrt, ["pb2"], ["rtmp"], scale=1.0 / D, bias=K.eps_ap)
            _recip(S, rstd[:, :N], rtmp[:, :N], ["rtmp"], ["rstd"])
            _stage(1.6)
            for dc in range(8):
                ht = htmp[dc % 2]
                hk = "htmp%d" % (dc % 2)
                _stt(S, "dve", ht[:, :N], xb[:, dc, :N], a1[:, dc:dc + 1], rstd[:, :N], ALU.mult, ALU.mult,
                     ["xb", "par", "rstd"], [hk])
                _act(S, hT[:, dc, :N], ht[:, :N], AF.Identity, [hk, "par"], ["hT"], bias=sh1[:, dc:dc + 1])
            _stage(2)
            pbi = [0]

            def nextpb():
                b = pbi[0] % 2
                pbi[0] += 1
                return PB[b], "pb%d" % b

            def proj(c0, ncols, evac):
                ps, pk = nextpb()
                for dc in range(8):
                    _mm(S, ps[0:ncols, :N], win[:, dc, c0:c0 + ncols], hT[:, dc, :N], dc == 0, dc == 7,
                        ["win", "hT"], [pk])
                evac(ps, pk)

            for h in range(4):
                proj(h * 64, 64, lambda ps, pk, h=h: _ts(S, "dve", qf[:, h, :N], ps[0:64, :N], 0.125, None, ALU.mult, None,
                                                         [pk], ["qf"]))
            for h in range(4):
                proj(256 + h * 64, 64, lambda ps, pk, h=h: _cp(S, "dve", kf[:, h, :N], ps[0:64, :N], [pk], ["kf"]))
            for c in range(4):
                proj(1056 + c * 128, 128, lambda ps, pk, c=c: _act(S, srs[:, c, :N], ps[:, :N], AF.Silu, [pk], ["srs"]))
            _dma(S, "sp", Dm["st_sr"][:, :, g0:g0 + N], srs[:, :, :N], ["srs"], ["st_sr"])
            for c in range(2):
                proj(1568 + c * 128, 128, lambda ps, pk, c=c: _cp(S, "dve", cqf[:, c, :N], ps[:, :N], [pk], ["cqf"]))
            proj(1824, 128, lambda ps, pk: _cp(S, "dve", ckvf[:, :N], ps[:, :N], [pk], ["ckvf"]))
            proj(1952, 64, lambda ps, pk: _cp(S, "dve", krf[:, 0, :N], ps[0:64, :N], [pk], ["krf"]))
            proj(2016, 64, lambda ps, pk: _cp(S, "dve", krf[:, 1, :N], ps[0:64, :N], [pk], ["krf"]))
            proj(1024, 32, lambda ps, pk: _cp(S, "dve", codT[0:32, :N], ps[0:32, :N], [pk], ["codT"]))
            _stage(3)
            _memset(S, "dve", vss[:], 0.0, ["vss"])
            for t in range(ntile):
                tsl = slice(t * 128, (t + 1) * 128)
                ps, pk = nextpb()
                for dc in range(8):
                    _mm(S, ps[:, 0:512], hT[:, dc, tsl], win[:, dc, 512:1024], dc == 0, dc == 7, ["win", "hT"], [pk])
                _cp(S, "act", vtm[:, t, :], ps[:, 0:512], [pk], ["vtm"])
                ps, pk = nextpb()
                for dc in range(8):
                    _mm(S, ps[:, 0:256], hT[:, dc, tsl], win[:, dc, 256:512], dc == 0, dc == 7, ["win", "hT"], [pk])
                for dc in range(8):
                    _mm(S, ps[:, 256:384], hT[:, dc, tsl], win[:, dc, 1824:1952], dc == 0, dc == 7, ["win", "hT"], [pk])
                _stage(3.2)
                _cp(S, "dve", ktm[:, t, :], ps[:, 0:256], [pk], ["ktm"])
                _stage(3.4)
                _cp(S, "dve", vjunk[:], ps[:, 256:384], [pk], ["vjunk"])
                _tt(S, "pool", vsq[:], vjunk[:], vjunk[:], ALU.mult, ["vjunk"], ["vsq"])
                S.op("dve", lambda e, t=t: e.reduce_sum(out=vss[:, t:t + 1], in_=vsq[:], axis=mybir.AxisListType.X),
                     reads=["vsq"], writes=["vss"])
                _stage(3.5)
                _act(S, vs1[:, t:t + 1], vss[:, t:t + 1], AF.Sqrt, ["vss"], ["vs1"], scale=1.0 / 128, bias=K.eps_ap)
                _stage(3.6)
                _recip(S, vs2[:, t:t + 1], vs1[:, t:t + 1], ["vs1"], ["vs2"])
                _stage(3.7)
                _stt(S, "dve", vls[:, t, :], vjunk[:], vs2[:, t:t + 1], kvgb[:], ALU.mult, ALU.mult,
                     ["vjunk", "vs2", "kvgb"], ["vls"])
            vdst = (Dm["x_v"][n0:n0 + N, :] if src == "x" else Dm["c_v"][:, :]).rearrange("(t p) r -> p t r", p=128)
            vkey = "x_v" if src == "x" else "c_v"
            _stage(3.8)
            _dma(S, "sp", vdst, vls[:, :ntile, :], ["vls"], [vkey])
            _stage(4)
            for c in range(2):
                sqb, sqk = sq[c % 2], "sq%d" % (c % 2)
                _tt(S, "pool", sqb[:, :N], cqf[:, c, :N], cqf[:, c, :N], ALU.mult, ["cqf"], [sqk])
                _mm(S, PB[2][:, :N], ones[:], sqb[:, :N], c == 0, c == 1, [sqk, "ones"], ["pb2"])
            _act(S, rtmp[:, :N], PB[2][:, :N], AF.Sqrt, ["pb2"], ["rtmp"], scale=1.0 / 256, bias=K.eps_ap)
            _recip(S, rstd[:, :N], rtmp[:, :N], ["rtmp"], ["rstd"])
            for c in range(2):
                _stt(S, "dve", cqn[:, c, :N], cqf[:, c, :N], gvec[:, 1 + c:2 + c], rstd[:, :N], ALU.mult, ALU.mult,
                     ["cqf", "gvec", "rstd"], ["cqn"])
            if src == "x":
                _dma(S, "sp", cosb[:, :N], Dm["ropec"][:, n0:n0 + N], [], ["cosb"])
                _dma(S, "sp", sinb[:, :N], Dm["ropes"][:, n0:n0 + N], [], ["sinb"])
            for h in range(4):
                ps, pk = nextpb()
                for kc in range(2):
                    _mm(S, ps[:, :N], wuq[:, kc, h * 128:(h + 1) * 128], cqn[:, kc, :N], kc == 0, kc == 1,
                        ["wuq", "cqn"], [pk])
                _cp(S, "act", qnh[:, :N], ps[:, :N], [pk], ["qnh"])
                ps, pk = nextpb()
                _mm(S, ps[:, :N], wukT[:, h * 128:(h + 1) * 128], qnh[:, :N], True, True, ["wukT", "qnh"], [pk])
                _cp(S, "dve", qps[:, h, :N], ps[:, :N], [pk], ["qps"])
                ps, pk = nextpb()
                for kc in range(2):
                    _mm(S, ps[0:64, :N], wuq[:, kc, 512 + h * 64:512 + (h + 1) * 64], cqn[:, kc, :N], kc == 0, kc == 1,
                        ["wuq", "cqn"], [pk])
                if src == "x":
                    ps2, pk2 = nextpb()
                    for kc in range(2):
                        _mm(S, ps2[0:64, :N], wuq[:, kc, 768 + h * 64:768 + (h + 1) * 64], cqn[:, kc, :N], kc == 0, kc == 1,
                            ["wuq", "cqn"], [pk2])
                    _tt(S, "dve", rt1[:, :N], ps[0:64, :N], cosb[:, :N], ALU.mult, [pk, "cosb"], ["rt1"])
                    _tt(S, "dve", rt2[:, :N], ps2[0:64, :N], sinb[:, :N], ALU.mult, [pk2, "sinb"], ["rt2"])
                    _tt(S, "pool", qrs[:, h, :N], rt1[:, :N], rt2[:, :N], ALU.add, ["rt1", "rt2"], ["qrs"])
                else:
                    _cp(S, "dve", qrs[:, h, :N], ps[0:64, :N], [pk], ["qrs"])
            _dma(S, "sp", Dm["st_qp"][:, :, g0:g0 + N], qps[:, :, :N], ["qps"], ["st_qp"])
            _dma(S, "sp", Dm["st_qr"][:, :, g0:g0 + N], qrs[:, :, :N], ["qrs"], ["st_qr"])
            _tt(S, "pool", sq[0][:, :N], ckvf[:, :N], ckvf[:, :N], ALU.mult, ["ckvf"], ["sq0"])
            _mm(S, PB[2][:, :N], ones[:], sq[0][:, :N], True, True, ["sq0", "ones"], ["pb2"])
            _act(S, rtmp[:, :N], PB[2][:, :N], AF.Sqrt, ["pb2"], ["rtmp"], scale=1.0 / 128, bias=K.eps_ap)
            _recip(S, rstd[:, :N], rtmp[:, :N], ["rtmp"], ["rstd"])
            _stt(S, "dve", kls[:, :N], ckvf[:, :N], gvec[:, 3:4], rstd[:, :N], ALU.mult, ALU.mult,
                 ["ckvf", "gvec", "rstd"], ["kls"])
            if src == "x":
                _tt(S, "dve", rt1[:, :N], krf[:, 0, :N], cosb[:, :N], ALU.mult, ["krf", "cosb"], ["rt1"])
                _tt(S, "dve", rt2[:, :N], krf[:, 1, :N], sinb[:, :N], ALU.mult, ["krf", "sinb"], ["rt2"])
                _tt(S, "pool", krs[:, :N], rt1[:, :N], rt2[:, :N], ALU.add, ["rt1", "rt2"], ["krs"])
                _dma(S, "sp", Dm["x_kl"][:, n0:n0 + N], kls[:, :N], ["kls"], ["x_kl"])
                _dma(S, "sp", Dm["x_kr"][:, n0:n0 + N], krs[:, :N], ["krs"], ["x_kr"])
            else:
                _cp(S, "dve", krs[:, :N], krf[:, 0, :N], ["krf"], ["krs"])
                _dma(S, "sp", Dm["c_kl"][:, :], kls[:, :N], ["kls"], ["c_kl"])
                _dma(S, "sp", Dm["c_kr"][:, :], krs[:, :N], ["krs"], ["c_kr"])
            _stage(5)
            if src == "x" and n0 == 0:
                _memset(S, "dve", Lf[:], 0.0, ["Lf"])
                _memset(S, "dve", Lb[:], 0.0, ["Lb"])
                _memset(S, "dve", PA[:], 1.0, ["PA"])
            for t in range(ntile):
                tsl = slice(t * 128, (t + 1) * 128)
                gt = (g0 // 128) + t
                _mm(S, PB[3][:, :], codT[0:33, tsl], wg2[:], True, True, ["codT", "wg2"], ["pb3"])
                _act(S, Ge[:], PB[3][:, :], AF.Exp, ["pb3"], ["Ge"], scale=-1.0)
                _act(S, Gn[:], Ge[:], AF.Ln, ["Ge"], ["Gn"], scale=1.0, bias=K.one_ap)
                _stage(5.1)
                for h in range(4):
                    _mm(S, PB[4][0:64, h * 128:(h + 1) * 128], Gn[:, h * 64:(h + 1) * 64], triF, True, True,
                        ["Gn", "cst"], ["pb4"])
                    _mm(S, PB[5][0:64, h * 128:(h + 1) * 128], Gn[:, 256 + h * 64:256 + (h + 1) * 64], triB, True, True,
                        ["Gn", "cst"], ["pb5"])
                _mm(S, PB[3][:, 0:256], triSU, Gn[:, 0:256], True, True, ["Gn", "cst"], ["pb3"])
                _mm(S, PB[3][:, 256:512], triSL, Gn[:, 256:512], True, True, ["Gn", "cst"], ["pb3"])
                _stage(5.3)
                _act(S, E1[:, 0, :], PB[4][0:64, :], AF.Exp, ["pb4"], ["E1"], scale=-1.0 / 16)
                _act(S, E1[:, 1, :], PB[5][0:64, :], AF.Exp, ["pb5"], ["E1"], scale=-1.0 / 16)
                _act(S, E2[:, 0, :], PB[4][0:64, :], AF.Exp, ["pb4"], ["E2"], scale=1.0 / 16)
                _act(S, E2[:, 1, :], PB[5][0:64, :], AF.Exp, ["pb5"], ["E2"], scale=1.0 / 16)
                _act(S, E3[:], PB[3][:, :], AF.Exp, ["pb3"], ["E3"], scale=-1.0 / 16)
                _stage(5.4)
                e1f = E1[:, 0, :].rearrange("p (h t) -> p h t", h=4)
                e1b = E1[:, 1, :].rearrange("p (h t) -> p h t", h=4)
                e2f = E2[:, 0, :].rearrange("p (h t) -> p h t", h=4)
                e2b = E2[:, 1, :].rearrange("p (h t) -> p h t", h=4)
                _tt(S, "dve", qtf[:, :, tsl], qf[:, :, tsl], e1f, ALU.mult, ["qf", "E1"], ["qtf"])
                _tt(S, "pool", qtb[:, :, tsl], qf[:, :, tsl], e1b, ALU.mult, ["qf", "E1"], ["qtb"])
                _tt(S, "dve", ktf[:], kf[:, :, tsl], e2f, ALU.mult, ["kf", "E2"], ["ktf"])
                _tt(S, "pool", ktb[:], kf[:, :, tsl], e2b, ALU.mult, ["kf", "E2"], ["ktb"])
                _cp(S, "dve", decs[:, t, 0:4], e1f[:, :, 127], ["E1"], ["decs"])
                _cp(S, "dve", decs[:, t, 4:8], e1b[:, :, 0], ["E1"], ["decs"])
                _tt(S, "dve", khf[:], ktm[:, t, :], E3[:, 0:256], ALU.mult, ["ktm", "E3"], ["khf"])
                _tt(S, "pool", khb[:], ktm[:, t, :], E3[:, 256:512], ALU.mult, ["ktm", "E3"], ["khb"])
                _stage(5.5)
                for h in range(int(os.environ.get("MK_NH", "4"))):
                    ps, pk = PB[6], "pb6"
                    _mm(S, ps[:, 0:128], ktf[:, h, :], qtf[:, h, tsl], True, True, ["ktf", "qtf"], [pk])
                    _mm(S, ps[:, 128:256], ktb[:, h, :], qtb[:, h, tsl], True, True, ["ktb", "qtb"], [pk])
                    _stage(5.6)
                    _tt(S, "dve", am1[:], ps[:, 0:128], triF, ALU.mult, [pk, "cst"], ["am1"])
                    _tt(S, "dve", am2[:], ps[:, 128:256], triB, ALU.mult, [pk, "cst"], ["am2"])
                    _tt(S, "pool", amt[:], am1[:], am2[:], ALU.add, ["am1", "am2"], ["amt"])
                    _stage(5.7)
                    _mm(S, PB[7][:, 0:128], vtm[:, t, h * 128:(h + 1) * 128], amt[:], True, True, ["vtm", "amt"], ["pb7"])
                    _mm(S, PB[7][0:64, 128:256], khf[:, h * 64:(h + 1) * 64], vtm[:, t, h * 128:(h + 1) * 128], True, True,
                        ["khf", "vtm"], ["pb7"])
                    _mm(S, PB[7][0:64, 256:384], khb[:, h * 64:(h + 1) * 64], vtm[:, t, h * 128:(h + 1) * 128], True, True,
                        ["khb", "vtm"], ["pb7"])
                    _stage(5.8)
                    _cp(S, "act", ois[:, h, tsl], PB[7][:, 0:128], ["pb7"], ["ois"])
                    _cp(S, "dve", Us[:, h, :], PB[7][0:64, 128:256], ["pb7"], ["Us"])
                    _cp(S, "dve", Us[:, 4 + h, :], PB[7][0:64, 256:384], ["pb7"], ["Us"])
                    _stage(5.85)
                    if src == "x" and not os.environ.get("MK_NOL"):
                        _stt(S, "dve", Lf[:, h, :], Lf[:, h, :], decs[:, t, h:h + 1], Us[:, h, :], ALU.mult, ALU.add,
                             ["Lf", "decs", "Us"], ["Lf"])
                        if not os.environ.get("MK_NOLB"):
                            _stt(S, "dve", Lb[:, h, :], Us[:, 4 + h, :], PA[:, 4 + h:5 + h], Lb[:, h, :], ALU.mult, ALU.add,
                                 ["Lb", "PA", "Us"], ["Lb"])
                _stage(5.9)
                _dma(S, "sp", Dm["st_U"][:, gt, :, :], Us[:], ["Us"], ["st_U"])
                _stage(5.92)
                if src == "x":
                    _tt(S, "dve", PA[:], PA[:], decs[:, t, :], ALU.mult, ["PA", "decs"], ["PA"])
            t0 = g0 // 128
            _stage(5.95)
            _dma(S, "sp", Dm["st_oi"][:, :, g0:g0 + N], ois[:, :, :N], ["ois"], ["st_oi"])
            _dma(S, "sp", Dm["st_qf"][:, :, g0:g0 + N], qtf[:, :, :N], ["qtf"], ["st_qf"])
            _dma(S, "sp", Dm["st_qb"][:, :, g0:g0 + N], qtb[:, :, :N], ["qtb"], ["st_qb"])
            _dma(S, "sp", Dm["st_dec"][:, t0:t0 + ntile, :], decs[:, :ntile, :], ["decs"], ["st_dec"])
            if src == "x" and n0 == NL - 512:
                _dma(S, "sp", Dm["x_L"][:, 0:512].rearrange("p (h v) -> p h v", h=4), Lf[:], ["Lf"], ["x_L"])
                _dma(S, "sp", Dm["x_L"][:, 512:1024].rearrange("p (h v) -> p h v", h=4), Lb[:], ["Lb"], ["x_L"])
                _dma(S, "sp", Dm["x_A"][:, :], PA[:], ["PA"], ["x_A"])
    S.barrier()


def phase2(K, li, lw, xin, xkey, cin, ckey, is_last):
    nc, S, Dm = K.nc, K.S, K.dram
    need_ctx = li < DEPTH - 1
    dense = (li % 2 == 0)
    PB = K.pb
    ones = K.ones_bf
    onesf = K.ones_f
    cst = K.cst
    ident = cst[:, 0:128]
    blocks = [b for b in BLOCKS if (b[0] == "x" or need_ctx)]
    with ExitStack() as es0:
        def sb0(name, shape, dt):
            return es0.enter_context(nc.sbuf_tensor("p2_%d_%s" % (li, name), shape, dt))
        xs = sb0("xs", [128, 8, NT], F32)
        _dma(S, "sp", xs[:, :, 0:NL], xin.rearrange("(dc p) n -> p dc n", p=128), [xkey], ["xs"])
        _dma(S, "sp", xs[:, :, NL:NT], cin.rearrange("(dc p) n -> p dc n", p=128), [ckey], ["xs"])
        par = sb0("par", [128, 2, 6, 8], F32)
        _dma(S, "sp", par[:], Dm["st_par"], ["st_par"], ["par"])
        gvec = sb0("gvec", [128, 8], F32)
        _dma(S, "sp", gvec[:], Dm["gvec"][lw], [], ["gvec"])

        with ExitStack() as es:
            def sb(name, shape, dt):
                return es.enter_context(nc.sbuf_tensor("p2c_%d_%s" % (li, name), shape, dt))
            dec = sb("dec", [64, 18, 8], F32)
            _dma(S, "sp", dec[:], Dm["st_dec"], ["st_dec"], ["dec"])
            gL = sb("gL", [64, 4, 1024], F32)
            _dma(S, "sp", gL[:], Dm["g_L"].rearrange("(r p) n -> p r n", p=64), ["g_L"], ["gL"])
            gA = sb("gA", [64, 4, 8], F32)
            _dma(S, "sp", gA[:], Dm["g_A"].rearrange("(r p) n -> p r n", p=64), ["g_A"], ["gA"])
            rmask = sb("rmask", [64, 8], F32)
            _dma(S, "sp", rmask[:], Dm["rmask"], [], ["rmask"])
            Ub = [sb("U%d" % i, [64, 8, 128], F32) for i in range(2)]
            cur = sb("cur", [64, 8, 128], F32)
            Sb_ = [sb("Sb%d" % i, [64, 8, 128], BF16) for i in range(2)]
            uctr = [0]

            def loadU(gt):
                i = uctr[0] % 2
                uctr[0] += 1
                _dma(S, "sp", Ub[i][:], Dm["st_U"][:, gt, :, :], ["st_U"], ["U%d" % i])
                return Ub[i], "U%d" % i

            U16, k16 = loadU(16)
            U17, k17 = loadU(17)
            _memset(S, "dve", Sb_[0][:], 0.0, ["Sb0"])
            _memset(S, "dve", Sb_[1][:], 0.0, ["Sb1"])
            _cp(S, "dve", Sb_[1][:, 0:4, :], U16[:, 0:4, :], [k16], ["Sb1"])
            _cp(S, "dve", Sb_[0][:, 4:8, :], U17[:, 4:8, :], [k17], ["Sb0"])
            _dma(S, "sp", Dm["st_S"][:, 16, :, :], Sb_[0][:], ["Sb0"], ["st_S"])
            _dma(S, "sp", Dm["st_S"][:, 17, :, :], Sb_[1][:], ["Sb1"], ["st_S"])
            for h in range(4):
                _stt(S, "dve", cur[:, h, :], U16[:, h, :], dec[:, 17, h:h + 1], U17[:, h, :], ALU.mult, ALU.add,
                     [k16, k17, "dec"], ["cur"])
                _stt(S, "dve", cur[:, 4 + h, :], U17[:, 4 + h, :], dec[:, 16, 4 + h:5 + h], U16[:, 4 + h, :], ALU.mult, ALU.add,
                     [k16, k17, "dec"], ["cur"])
            Ap = sb("Ap", [64, 4, 8], F32)
            for r in range(4):
                _ts(S, "dve", Ap[:, r, 0:4], gA[:, r, 0:4], -1.0, rmask[:, r:r + 1], ALU.add, ALU.mult, ["gA", "rmask"], ["Ap"])
                _ts(S, "dve", Ap[:, r, 4:8], gA[:, r, 4:8], -1.0, rmask[:, 4 + r:5 + r], ALU.add, ALU.mult, ["gA", "rmask"], ["Ap"])
            _ts(S, "dve", Ap[:], Ap[:], 1.0, None, ALU.add, None, ["Ap"], ["Ap"])
            for r in range(4):
                _ts(S, "dve", gL[:, r, 0:512], gL[:, r, 0:512], rmask[:, r:r + 1], None, ALU.mult, None, ["gL", "rmask"], ["gL"])
                _ts(S, "dve", gL[:, r, 512:1024], gL[:, r, 512:1024], rmask[:, 4 + r:5 + r], None, ALU.mult, None,
                    ["gL", "rmask"], ["gL"])
            for r in range(4):
                for h in range(4):
                    _stt(S, "dve", cur[:, h, :], cur[:, h, :], Ap[:, r, h:h + 1], gL[:, r, h * 128:(h + 1) * 128],
                         ALU.mult, ALU.add, ["cur", "Ap", "gL"], ["cur"])
            for r in (3, 2, 1, 0):
                for h in range(4):
                    _stt(S, "dve", cur[:, 4 + h, :], cur[:, 4 + h, :], Ap[:, r, 4 + h:5 + h],
                         gL[:, r, 512 + h * 128:512 + (h + 1) * 128], ALU.mult, ALU.add, ["cur", "Ap", "gL"], ["cur"])
            Sf = sb("Sf", [64, 16, 4, 128], BF16)
            for i in range(16):
                U, uk = loadU(i)
                _cp(S, "pool", Sf[:, i, :, :], cur[:, 0:4, :], ["cur"], ["Sf"])
                for h in range(4):
                    _stt(S, "dve", cur[:, h, :], cur[:, h, :], dec[:, i, h:h + 1], U[:, h, :], ALU.mult, ALU.add,
                         ["cur", "dec", uk], ["cur"])
            sctr = 0
            for i in range(15, -1, -1):
                U, uk = loadU(i)
                sbuf_, sk = Sb_[sctr % 2], "Sb%d" % (sctr % 2)
                sctr += 1
                _cp(S, "pool", sbuf_[:, 0:4, :], Sf[:, i, :, :], ["Sf"], [sk])
                _cp(S, "pool", sbuf_[:, 4:8, :], cur[:, 4:8, :], ["cur"], [sk])
                _dma(S, "sp", Dm["st_S"][:, i, :, :], sbuf_[:], [sk], ["st_S"])
                for h in range(4):
                    _stt(S, "dve", cur[:, 4 + h, :], cur[:, 4 + h, :], dec[:, i, 4 + h:5 + h], U[:, 4 + h, :], ALU.mult, ALU.add,
                         ["cur", "dec", uk], ["cur"])
        S.barrier()

        with ExitStack() as es:
            def sb(name, shape, dt):
                return es.enter_context(nc.sbuf_tensor("p2a_%d_%s" % (li, name), shape, dt))
            KlT = sb("KlT", [128, NKEY], BF16)
            KrT = sb("KrT", [64, NKEY], BF16)
            Vl = sb("Vl", [128, NKT, 128], BF16)
            _dma(S, "sp", KlT[:, 0:NCX], Dm["c_kl"], ["c_kl"], ["KlT"])
            _dma(S, "sp", KrT[:, 0:NCX], Dm["c_kr"], ["c_kr"], ["KrT"])
            _dma(S, "sp", Vl[:, 0:2, :], Dm["c_v"].rearrange("(t p) r -> p t r", p=128), ["c_v"], ["Vl"])
            for r in range(4):
                _dma(S, "sp", KlT[:, NCX + r * NL:NCX + (r + 1) * NL], Dm["g_kl"][r * 128:(r + 1) * 128, :], ["g_kl"], ["KlT"])
                _dma(S, "sp", KrT[:, NCX + r * NL:NCX + (r + 1) * NL], Dm["g_kr"][r * 64:(r + 1) * 64, :], ["g_kr"], ["KrT"])
                _dma(S, "sp", Vl[:, 2 + 16 * r:2 + 16 * (r + 1), :],
                     Dm["g_v"][r * NL:(r + 1) * NL, :].rearrange("(t p) r -> p t r", p=128), ["g_v"], ["Vl"])
            wout = sb("wout", [128, 8, D], BF16)
            _dma(S, "pool", wout[:], Dm["w_out"][lw].rearrange("(c p) n -> p c n", p=128), [], ["wout"])
            wuv = sb("wuv", [128, 512], BF16)
            _dma(S, "pool", wuv[:], Dm["w_uv"][lw], [], ["wuv"])
            oi = sb("oi", [128, 4, 512], F32)
            qf = sb("qf", [64, 4, 512], BF16)
            qb = sb("qb", [64, 4, 512], BF16)
            sr = sb("sr", [128, 4, 512], BF16)
            qp = sb("qp", [128, 4, 512], BF16)
            qr = sb("qr", [64, 4, 512], BF16)
            Sblk = sb("Sblk", [64, 4, 8, 128], BF16)
            mix = sb("mix", [128, 8, 512], BF16)
            pT = [sb("pT%d" % i, [128, 512], BF16) for i in range(3)]
            racc = sb("racc", [128, 512], F32)
            rinv = sb("rinv", [128, 512], F32)
            rtmp = sb("rtmp", [128, 512], F32)
            sqb = sb("sqb", [128, 512], BF16)
            otmp = sb("otmp", [128, 512], F32)
            oln = sb("oln", [128, 512], BF16)
            for (src, col, n0, N, g0) in blocks:
                ntile = N // 128
                t0 = g0 // 128
                _dma(S, "sp", oi[:, :, :N], Dm["st_oi"][:, :, g0:g0 + N], ["st_oi"], ["oi"])
                _dma(S, "sp", qf[:, :, :N], Dm["st_qf"][:, :, g0:g0 + N], ["st_qf"], ["qf"])
                _dma(S, "sp", qb[:, :, :N], Dm["st_qb"][:, :, g0:g0 + N], ["st_qb"], ["qb"])
                _dma(S, "sp", sr[:, :, :N], Dm["st_sr"][:, :, g0:g0 + N], ["st_sr"], ["sr"])
                _dma(S, "sp", qp[:, :, :N], Dm["st_qp"][:, :, g0:g0 + N], ["st_qp"], ["qp"])
                _dma(S, "sp", qr[:, :, :N], Dm["st_qr"][:, :, g0:g0 + N], ["st_qr"], ["qr"])
                _dma(S, "sp", Sblk[:, :ntile, :, :], Dm["st_S"][:, t0:t0 + ntile, :, :], ["st_S"], ["Sblk"])
                for h in range(4):
                    for t in range(ntile):
                        tsl = slice(t * 128, (t + 1) * 128)
                        _mm(S, PB[3][:, tsl], Sblk[:, t, h, :], qf[:, h, tsl], True, False, ["Sblk", "qf"], ["pb3"])
                        _mm(S, PB[3][:, tsl], Sblk[:, t, 4 + h, :], qb[:, h, tsl], False, True, ["Sblk", "qb"], ["pb3"])
                    _tt(S, "dve", oi[:, h, :N], oi[:, h, :N], PB[3][:, :N], ALU.add, ["oi", "pb3"], ["oi"])
                    _tt(S, "pool", sqb[:, :N], oi[:, h, :N], oi[:, h, :N], ALU.mult, ["oi"], ["sqb"])
                    _mm(S, PB[4][:, :N], ones[:], sqb[:, :N], True, True, ["sqb", "ones"], ["pb4"])
                    _act(S, rtmp[:, :N], PB[4][:, :N], AF.Sqrt, ["pb4"], ["rtmp"], scale=1.0 / 128, bias=K.eps_ap)
                    _recip(S, rinv[:, :N], rtmp[:, :N], ["rtmp"], ["rinv"])
                    _stt(S, "dve", otmp[:, :N], oi[:, h, :N], gvec[:, 0:1], rinv[:, :N], ALU.mult, ALU.mult,
                         ["oi", "gvec", "rinv"], ["otmp"])
                    _tt(S, "dve", mix[:, h, :N], otmp[:, :N], sr[:, h, :N], ALU.mult, ["otmp", "sr"], ["mix"])
                kts = list(range(NKT)) if src == "x" else [0, 1]
                _dbg = os.environ.get("MK_P2DBG%d" % li, "")
                if "nogla" in _dbg:
                    _memset(S, "dve", mix[:, 0:4, :N], 0.0, ["mix"])
                if "nomla" in _dbg:
                    _memset(S, "dve", mix[:, 4:8, :N], 0.0, ["mix"])
                for h in (range(4) if "nomla" not in _dbg else []):
                    sbanks = [(PB[0], "pb0"), (PB[1], "pb1"), (PB[7], "pb7")]

                    def qk(ki):
                        ps, pk = sbanks[ki % 3]
                        ksl = slice(kts[ki] * 128, (kts[ki] + 1) * 128)
                        _mm(S, ps[:, :N], KlT[:, ksl], qp[:, h, :N], True, False, ["KlT", "qp"], [pk])
                        _mm(S, ps[:, :N], KrT[:, ksl], qr[:, h, :N], False, True, ["KrT", "qr"], [pk])
                    qk(0)
                    if len(kts) > 1:
                        qk(1)
                    for ki, kt in enumerate(kts):
                        ps, pk = sbanks[ki % 3]
                        if ki + 2 < len(kts):
                            qk(ki + 2)
                        p_, ppk = pT[ki % 3], "pT%d" % (ki % 3)
                        _act(S, p_[:, :N], ps[:, :N], AF.Exp, [pk], [ppk], scale=MLA_SCALE)
                        _mm(S, PB[2][:, :N], Vl[:, kt, :], p_[:, :N], ki == 0, ki == len(kts) - 1, ["Vl", ppk], ["pb2"])
                        _mm(S, PB[6][:, :N], ones[:], p_[:, :N], ki == 0, ki == len(kts) - 1, ["ones", ppk], ["pb6"])
                    _recip(S, rinv[:, :N], PB[6][:, :N], ["pb6"], ["rinv"])
                    _tt(S, "dve", oln[:, :N], PB[2][:, :N], rinv[:, :N], ALU.mult, ["pb2", "rinv"], ["oln"])
                    _mm(S, PB[3][:, :N], wuv[:, h * 128:(h + 1) * 128], oln[:, :N], True, True, ["wuv", "oln"], ["pb3"])
                    _cp(S, "act", mix[:, 4 + h, :N], PB[3][:, :N], ["pb3"], ["mix"])
                g1 = par[:, col, 2, :]
                for dco in range(8):
                    b = 4 + dco % 2
                    ps, pk = PB[b], "pb%d" % b
                    for c in range(8):
                        _mm(S, ps[:, :N], wout[:, c, dco * 128:(dco + 1) * 128], mix[:, c, :N], c == 0, c == 7,
                            ["wout", "mix"], [pk])
                    _stt(S, "dve", xs[:, dco, g0:g0 + N], ps[:, :N], g1[:, dco:dco + 1], xs[:, dco, g0:g0 + N],
                         ALU.mult, ALU.add, [pk, "par", "xs"], ["xs"])
        S.barrier()

        with ExitStack() as es:
            def sb(name, shape, dt):
                return es.enter_context(nc.sbuf_tensor("p2f_%d_%s" % (li, name), shape, dt))
            h2 = sb("h2", [128, 8, NT], BF16)
            sq = [sb("sq%d" % i, [128, 512], BF16) for i in range(2)]
            rstd = sb("rstd", [128, 512], F32)
            rtmp = sb("rtmp", [128, 512], F32)
            htmp = [sb("htmp%d" % i, [128, 512], F32) for i in range(2)]
            if not dense:
                rw = sb("rw", [128, 8, NEXP], F32)
                _dma(S, "sp", rw[:], Dm["router"].rearrange("(dc p) e -> p dc e", p=128), [], ["rw"])
                lg = sb("lg", [128, 32], F32)
                mx8 = sb("mx8", [128, 8], F32)
                nv1 = sb("nv1", [128, 1], F32)
                msk = sb("msk", [128, 8], F32)
                ex = sb("ex", [128, 8], F32)
                ssum = sb("ssum", [128, 1], F32)
                comb = sb("comb", [128, 8], F32)
                cTs = sb("cTs", [8, 512], F32)
            for (src, col, n0, N, g0) in blocks:
                a2 = par[:, col, 4, :]
                sh2 = par[:, col, 3, :]
                for dc in range(8):
                    sqt, sqk = sq[dc % 2], "sq%d" % (dc % 2)
                    _tt(S, "pool", sqt[:, :N], xs[:, dc, g0:g0 + N], xs[:, dc, g0:g0 + N], ALU.mult, ["xs"], [sqk])
                    _mm(S, PB[0][:, :N], ones[:], sqt[:, :N], dc == 0, dc == 7, [sqk, "ones"], ["pb0"])
                _act(S, rtmp[:, :N], PB[0][:, :N], AF.Sqrt, ["pb0"], ["rtmp"], scale=1.0 / D, bias=K.eps_ap)
                _recip(S, rstd[:, :N], rtmp[:, :N], ["rtmp"], ["rstd"])
                for dc in range(8):
                    ht, hk = htmp[dc % 2], "htmp%d" % (dc % 2)
                    _stt(S, "dve", ht[:, :N], xs[:, dc, g0:g0 + N], a2[:, dc:dc + 1], rstd[:, :N], ALU.mult, ALU.mult,
                         ["xs", "par", "rstd"], [hk])
                    if dense:
                        _act(S, h2[:, dc, g0:g0 + N], ht[:, :N], AF.Identity, [hk, "par"], ["h2"], bias=sh2[:, dc:dc + 1])
                    else:
                        _act(S, ht[:, :N], ht[:, :N], AF.Identity, [hk, "par"], [hk], bias=sh2[:, dc:dc + 1])
                        _cp(S, "pool", h2[:, dc, g0:g0 + N], ht[:, :N], [hk], ["h2"])
                        for t in range(N // 128):
                            _mm(S, PB[1 + t][:, 0:8], ht[:, t * 128:(t + 1) * 128], rw[:, dc, :], dc == 0, dc == 7,
                                [hk, "rw"], ["pb%d" % (1 + t)])
                if not dense:
                    for t in range(N // 128):
                        _cp(S, "dve", lg[:, t * 8:(t + 1) * 8], PB[1 + t][:, 0:8], ["pb%d" % (1 + t)], ["lg"])
                    if "dbg_lg" in Dm:
                        _dma(S, "sp", Dm["dbg_lg"][:, (n0 // 512) * 32:(n0 // 512 + 1) * 32], lg[:], ["lg"], ["dbg_lg"])
                    for t in range(N // 128):
                        lsl = slice(t * 8, (t + 1) * 8)
                        S.op("dve", lambda e, lsl=lsl: e.max(out=mx8[:], in_=lg[:, lsl]), reads=["lg"], writes=["mx8"])
                        _ts(S, "dve", nv1[:], mx8[:, 0:1], -1.0, None, ALU.mult, None, ["mx8"], ["nv1"])
                        _ts(S, "dve", msk[:], lg[:, lsl], mx8[:, 1:2], None, ALU.is_ge, None, ["lg", "mx8"], ["msk"])
                        _act(S, ex[:], lg[:, lsl], AF.Exp, ["lg", "nv1"], ["ex"], bias=nv1[:, 0:1])
                        _tt(S, "dve", ex[:], ex[:], msk[:], ALU.mult, ["ex", "msk"], ["ex"])
                        S.op("dve", lambda e: e.reduce_sum(out=ssum[:], in_=ex[:], axis=mybir.AxisListType.X),
                             reads=["ex"], writes=["ssum"])
                        _recip(S, ssum[:], ssum[:], ["ssum"], ["ssum"])
                        _ts(S, "dve", comb[:], ex[:], ssum[:, 0:1], None, ALU.mult, None, ["ex", "ssum"], ["comb"])
                        _mm(S, PB[5][0:8, t * 128:(t + 1) * 128], comb[:], ident, True, True, ["comb", "cst"], ["pb5"])
                    _cp(S, "dve", cTs[:, :N], PB[5][0:8, :N], ["pb5"], ["cTs"])
                    _dma(S, "sp", Dm["combT"][:, n0:n0 + N], cTs[:, :N], ["cTs"], ["combT"])
            nsteps, nfin = [], None
            if K.mode == "FUSED" and li + 1 < DEPTH:
                nl = li + 1
                n_lnT = sb("n_lnT", [128, 16], F32)
                _dma(S, "sp", n_lnT[:], Dm["lnT"][nl], [], ["n_lnT"])
                n_bmodT = sb("n_bmodT", [128, 48], F32)
                _dma(S, "sp", n_bmodT[:], Dm["bmodT"][nl], [], ["n_bmodT"])
                n_cT = sb("n_cT", [128, 8, 2], F32)
                _dma(S, "sp", n_cT[:], Dm["cT"].rearrange("(dc p) c -> p dc c", p=128), [], ["n_cT"])
                n_sT = sb("n_sT", [128, 8, 2], F32)
                _act(S, n_sT[:], n_cT[:], AF.Silu, ["n_cT"], ["n_sT"])
                n_par = sb("n_par", [128, 2, 6, 8], F32)
                nsteps, nfin = mod_stage_steps(K, nl, sb, PB[7], "pb7", n_par, "n_par", n_sT, "n_sT",
                                               n_bmodT, "n_bmodT", n_lnT, "n_lnT")
                K.par_pre.add(nl)
            ybanks = 3 if nsteps else 4
            GS = 4
            wg = [sb("wg%d" % i, [128, 8, GS * 128], BF16) for i in range(2)]
            wu = [sb("wu%d" % i, [128, 8, GS * 128], BF16) for i in range(2)]
            wd = [sb("wd%d" % i, [128, GS, D], BF16) for i in range(2)]
            actb = [sb("actb%d" % i, [128, GS, 512], BF16) for i in range(2)]
            sg = [sb("sg%d" % i, [128, 512], BF16) for i in range(2)]
            ytmp = sb("ytmp", [128, 512], F32)
            if not dense:
                cb = [sb("cb%d" % i, [128, NL], F32) for i in range(2)]
            g2 = None
            nexp = 1 if dense else NEXP
            dff = DFF0 if dense else DFFE
            nch = dff // 128
            gctr = 0
            actr = 0
            for ex_i in (range(nexp) if "noffn" not in os.environ.get("MK_P2DBG%d" % li, "") else []):
                if dense:
                    Wg, Wu, Wd = Dm["ffn_g"], Dm["ffn_u"], Dm["ffn_d"]
                else:
                    Wg, Wu, Wd = Dm["exp_g"][ex_i], Dm["exp_u"][ex_i], Dm["exp_d"][ex_i]
                    cbb, cbk = cb[ex_i % 2], "cb%d" % (ex_i % 2)
                    _dma(S, "sp", cbb[:], Dm["combT"][ex_i:ex_i + 1, :].partition_broadcast(128), ["combT"], [cbk])
                for f0 in range(0, nch, GS):
                    gs = min(GS, nch - f0)
                    wi = gctr % 2
                    gctr += 1
                    kg, ku, kd = "wg%d" % wi, "wu%d" % wi, "wd%d" % wi
                    _dma(S, "pool", wg[wi][:, :, :gs * 128],
                         Wg[:, f0 * 128:(f0 + gs) * 128].rearrange("(dc p) n -> p dc n", p=128), [], [kg])
                    _dma(S, "pool", wu[wi][:, :, :gs * 128],
                         Wu[:, f0 * 128:(f0 + gs) * 128].rearrange("(dc p) n -> p dc n", p=128), [], [ku])
                    _dma(S, "pool", wd[wi][:, :gs, :],
                         Wd[f0 * 128:(f0 + gs) * 128, :].rearrange("(j p) n -> p j n", p=128), [], [kd])
                    for (src, col, n0, N, g0) in blocks:
                        g2 = par[:, col, 5, :]
                        ai = actr % 2
                        actr += 1
                        ak = "actb%d" % ai
                        for j in range(gs):
                            b = j % 2
                            pg, pgk = PB[b], "pb%d" % b
                            pu, puk = PB[2 + b], "pb%d" % (2 + b)
                            for dc in range(8):
                                _mm(S, pg[:, :N], wg[wi][:, dc, j * 128:(j + 1) * 128], h2[:, dc, g0:g0 + N], dc == 0, dc == 7,
                                    [kg, "h2"], [pgk])
                            for dc in range(8):
                                _mm(S, pu[:, :N], wu[wi][:, dc, j * 128:(j + 1) * 128], h2[:, dc, g0:g0 + N], dc == 0, dc == 7,
                                    [ku, "h2"], [puk])
                            _act(S, sg[b][:, :N], pg[:, :N], AF.Silu, [pgk], ["sg%d" % b])
                            _tt(S, "dve", actb[ai][:, j, :N], sg[b][:, :N], pu[:, :N], ALU.mult, ["sg%d" % b, puk], [ak])
                        for dco in range(8):
                            b = 4 + dco % ybanks
                            py, pyk = PB[b], "pb%d" % b
                            for j in range(gs):
                                _mm(S, py[:, :N], wd[wi][:, j, dco * 128:(dco + 1) * 128], actb[ai][:, j, :N], j == 0, j == gs - 1,
                                    [kd, ak], [pyk])
                            if dense:
                                _stt(S, "dve", xs[:, dco, g0:g0 + N], py[:, :N], g2[:, dco:dco + 1], xs[:, dco, g0:g0 + N],
                                     ALU.mult, ALU.add, [pyk, "par", "xs"], ["xs"])
                            else:
                                _tt(S, "dve", ytmp[:, :N], py[:, :N], cbb[:, n0:n0 + N], ALU.mult, [pyk, cbk], ["ytmp"])
                                _stt(S, "dve", xs[:, dco, g0:g0 + N], ytmp[:, :N], g2[:, dco:dco + 1], xs[:, dco, g0:g0 + N],
                                     ALU.mult, ALU.add, ["ytmp", "par", "xs"], ["xs"])
                        if nsteps:
                            nsteps.pop(0)()
            while nsteps:
                nsteps.pop(0)()
            if nfin is not None:
                nfin()
                _dma(S, "sp", Dm["st_par2"], n_par[:], ["n_par"], ["st_par2"])
            if is_last:
                fing = sb("fing", [128, 8], F32)
                _dma(S, "sp", fing[:], Dm["fin_g"], [], ["fing"])
                K.final_dmas = []
                for (src, col, n0, N, g0) in blocks:
                    if src != "x":
                        continue
                    for dc in range(8):
                        sqt, sqk = sq[dc % 2], "sq%d" % (dc % 2)
                        _tt(S, "pool", sqt[:, :N], xs[:, dc, g0:g0 + N], xs[:, dc, g0:g0 + N], ALU.mult, ["xs"], [sqk])
                        _mm(S, PB[0][:, :N], ones[:], sqt[:, :N], dc == 0, dc == 7, [sqk, "ones"], ["pb0"])
                    _act(S, rtmp[:, :N], PB[0][:, :N], AF.Sqrt, ["pb0"], ["rtmp"], scale=1.0 / D, bias=K.eps_ap)
                    _recip(S, rstd[:, :N], rtmp[:, :N], ["rtmp"], ["rstd"])
                    for dc in range(8):
                        ht, hk = htmp[dc % 2], "htmp%d" % (dc % 2)
                        _stt(S, "dve", ht[:, :N], xs[:, dc, g0:g0 + N], fing[:, dc:dc + 1], rstd[:, :N], ALU.mult, ALU.mult,
                             ["xs", "fing", "rstd"], [hk])
                        d = _dma(S, "sp", Dm["outT"][dc * 128:(dc + 1) * 128, n0:n0 + N], ht[:, :N], [hk], ["outT"])
                        K.final_dmas.append(d)
            else:
                K.final_dmas = []
                d = _dma(S, "sp", K.xout.rearrange("(dc p) n -> p dc n", p=128), xs[:, :, 0:NL], ["xs"], [K.xout_key])
                K.final_dmas.append(d)
                d = _dma(S, "sp", K.cout.rearrange("(dc p) n -> p dc n", p=128), xs[:, :, NL:NT], ["xs"], [K.cout_key])
                K.final_dmas.append(d)
        S.barrier()


def build(mode, li):
    nc = bass.Bass("TRN2", target_bir_lowering=False)
    K = Ctx()
    K.nc = nc
    K.S = Sched(nc, same_engine_sync=not os.environ.get('MK_NOSES'))
    K.mode = mode
    K.par_pre = set()
    S = K.S
    _CUR[0] = S
    layers = [li] if mode != "FUSED" else list(range(DEPTH))
    K.nlay = len(layers)
    K.has_dense = any(l % 2 == 0 for l in layers)
    K.has_moe = any(l % 2 == 1 for l in layers)
    K.has_last = (DEPTH - 1) in layers
    make_dram(K, mode)
    Dm = K.dram
    with ExitStack() as es:
        K.pb = [es.enter_context(nc.psum_tensor("pb%d" % i, [128, 512], F32)) for i in range(8)]
        cst = es.enter_context(nc.sbuf_tensor("cst_sb", [128, 640], F32))
        _dma(S, "sp", cst[:], Dm["cst"], [], ["cst"])
        K.cst = cst
        ones_bf = es.enter_context(nc.sbuf_tensor("ones_bf", [128, 128], BF16))
        _memset(S, "dve", ones_bf[:], 1.0, ["ones"])
        K.ones_bf = ones_bf
        ones_f = es.enter_context(nc.sbuf_tensor("ones_f", [128, 128], F32))
        _memset(S, "dve", ones_f[:], 1.0, ["onesf"])
        K.ones_f = ones_f
        cvec = es.enter_context(nc.sbuf_tensor("cvec", [128, 2], F32))
        _memset(S, "dve", cvec[:, 0:1], EPS, ["cvec"])
        _memset(S, "dve", cvec[:, 1:2], 1.0, ["cvec"])
        K.eps_ap = cvec[:, 0:1]
        K.one_ap = cvec[:, 1:2]
        K.final_dmas = []
        if mode == "P1":
            try:
                phase1(K, 0, Dm["xT"], "xT", Dm["ctxT"], "ctxT")
            except _Stop:
                pass
        elif mode == "P2":
            if not K.has_last:
                K.xout, K.xout_key = Dm["xT_out"], "xT_out"
                K.cout, K.cout_key = Dm["ctxT_out"], "ctxT_out"
            phase2(K, li, 0, Dm["xT"], "xT", Dm["ctxT"], "ctxT", K.has_last)
        else:
            K.xout, K.xout_key = Dm["xT_out"], "xT_out"
            K.cout, K.cout_key = Dm["ctxT_out"], "ctxT_out"
            groups = [[0, 1, 2, 3], [4, 5, 6, 7]]
            for l in range(DEPTH):
                if l == 0:
                    xin, xkey, cin, ckey = Dm["xT"], "xT", Dm["ctxT"], "ctxT"
                else:
                    xin, xkey, cin, ckey = Dm["xT_out"], "xT_out", Dm["ctxT_out"], "ctxT_out"
                phase1(K, l, xin, xkey, cin, ckey)
                for nm in ("kl", "kr", "v", "L", "A"):
                    src, dst = Dm["x_" + nm], Dm["g_" + nm]
                    S.coll("pool", lambda e, src=src, dst=dst: e.collective_compute(
                        "AllGather", ALU.bypass, replica_groups=groups, ins=[src.opt()], outs=[dst.opt()]),
                        reads=["x_" + nm], writes=["g_" + nm])
                S.barrier()
                phase2(K, l, l, xin, xkey, cin, ckey, l == DEPTH - 1)
        S.emit()
    return nc


def _bf(a):
    return np.ascontiguousarray(a)


_PERM = np.concatenate([np.arange(16, 32), np.arange(0, 16), np.arange(48, 64), np.arange(32, 48)])


def host_prep(inp):
    f = lambda a: np.ascontiguousarray(np.asarray(a, dtype=np.float32))
    W = {}
    nl = DEPTH
    w_in = f(inp["w_in"])
    W["w_in"] = np.ascontiguousarray(np.concatenate([w_in, w_in[:, :, 1952:2016][:, :, _PERM]], axis=2))
    W["w_mod"] = f(inp["w_mod"])
    W["bmodT"] = np.ascontiguousarray(f(inp["b_mod"]).reshape(nl, 48, 128).transpose(0, 2, 1))
    ln1 = f(inp["ln1_g"]).reshape(nl, 8, 128).transpose(0, 2, 1)
    ln2 = f(inp["ln2_g"]).reshape(nl, 8, 128).transpose(0, 2, 1)
    W["lnT"] = np.ascontiguousarray(np.concatenate([ln1, ln2], axis=2))
    wg2 = np.zeros((nl, 33, 512), np.float32)
    g2 = f(inp["w_gla_g2"])
    b2 = f(inp["b_gla_g2"])
    wg2[:, 0:16, 0:256] = g2[:, 0]
    wg2[:, 16:32, 256:512] = g2[:, 1]
    wg2[:, 32, 0:256] = b2[:, 0]
    wg2[:, 32, 256:512] = b2[:, 1]
    W["wg2"] = wg2
    gvec = np.zeros((nl, 128, 8), np.float32)
    gvec[:, :, 0] = f(inp["gla_norm_g"])
    qn = f(inp["mla_q_norm_g"])
    gvec[:, :, 1] = qn[:, 0:128]
    gvec[:, :, 2] = qn[:, 128:256]
    gvec[:, :, 3] = f(inp["mla_kv_norm_g"])
    W["gvec"] = gvec
    W["kvg_row"] = np.ascontiguousarray(f(inp["mla_kv_norm_g"]).reshape(nl, 1, 128))
    wuq = f(inp["w_uq"])
    wq = np.zeros((nl, 256, 1024), np.float32)
    for h in range(4):
        wq[:, :, h * 128:(h + 1) * 128] = wuq[:, :, 192 * h:192 * h + 128]
        rp = wuq[:, :, 192 * h + 128:192 * h + 192]
        wq[:, :, 512 + h * 64:512 + (h + 1) * 64] = rp
        wq[:, :, 768 + h * 64:768 + (h + 1) * 64] = rp[:, :, _PERM]
    W["w_uq"] = wq
    wukv = f(inp["w_ukv"])
    ukT = np.zeros((nl, 128, 512), np.float32)
    uv = np.zeros((nl, 128, 512), np.float32)
    for h in range(4):
        ukT[:, :, h * 128:(h + 1) * 128] = wukv[:, :, 256 * h:256 * h + 128].transpose(0, 2, 1)
        uv[:, :, h * 128:(h + 1) * 128] = wukv[:, :, 256 * h + 128:256 * h + 256]
    W["w_ukT"] = ukT
    W["w_uv"] = uv
    W["w_out"] = f(inp["w_out"])
    W["ffn_g"] = f(inp["ffn_w_gate"])[0]
    W["ffn_u"] = f(inp["ffn_w_up"])[0]
    W["ffn_d"] = f(inp["ffn_w_down"])[0]
    W["router"] = f(inp["router_w"])[0]
    W["exp_g"] = f(inp["exp_w_gate"])[0]
    W["exp_u"] = f(inp["exp_w_up"])[0]
    W["exp_d"] = f(inp["exp_w_down"])[0]
    W["fin_g"] = np.ascontiguousarray(f(inp["final_norm_g"]).reshape(8, 128).T)
    i = np.arange(128)
    ident = (i[:, None] == i[None, :]).astype(np.float32)
    triF = (i[:, None] <= i[None, :]).astype(np.float32)
    triB = (i[:, None] >= i[None, :]).astype(np.float32)
    triSU = (i[:, None] > i[None, :]).astype(np.float32)
    triSL = (i[:, None] < i[None, :]).astype(np.float32)
    W["cst"] = np.ascontiguousarray(np.concatenate([ident, triF, triB, triSU, triSL], axis=1))
    x = f(inp["x"])
    c = f(inp["c"])
    ctx = f(inp["ctx"])
    cc = f(inp["c_ctx"])
    inv = (np.float32(10000.0) ** (-np.arange(16, dtype=np.float32) / np.float32(16))).astype(np.float32)
    cores = []
    for core in range(8):
        b, j = core // 4, core % 4
        t0 = j * NL
        P = {}
        P["xT"] = np.ascontiguousarray(x[b, t0:t0 + NL].T)
        P["ctxT"] = np.ascontiguousarray(ctx[b].T)
        P["cT"] = np.ascontiguousarray(np.stack([c[b], cc], axis=1))
        pos = np.arange(t0, t0 + NL)
        row = (pos // 64).astype(np.float32)
        colp = (pos % 64).astype(np.float32)
        ar = (inv[:, None] * row[None, :]).astype(np.float32)
        ac = (inv[:, None] * colp[None, :]).astype(np.float32)
        P["ropec"] = np.ascontiguousarray(np.concatenate([np.cos(ar), np.cos(ar), np.cos(ac), np.cos(ac)], axis=0).astype(np.float32))
        P["ropes"] = np.ascontiguousarray(np.concatenate([-np.sin(ar), np.sin(ar), -np.sin(ac), np.sin(ac)], axis=0).astype(np.float32))
        rm = np.zeros((64, 8), np.float32)
        for r in range(4):
            rm[:, r] = 1.0 if r < j else 0.0
            rm[:, 4 + r] = 1.0 if r > j else 0.0
        P["rmask"] = rm
        cores.append(P)
    return W, cores


_LAYER_KEYS = ["w_mod", "bmodT", "lnT", "w_in", "wg2", "gvec", "kvg_row", "w_uq", "w_ukT", "w_uv", "w_out"]
_NC_CACHE = {}


def _get_nc(mode, li):
    key = (mode, li)
    if key not in _NC_CACHE:
        _NC_CACHE[key] = build(mode, li)
    return _NC_CACHE[key]


def _run(mode, li, W, per_core):
    nc = _get_nc(mode, li)
    names = [n for n, ap in _dram_inputs(nc)]
    in_maps = []
    for core in range(8):
        m = {}
        for n in names:
            if n in per_core[core]:
                m[n] = per_core[core][n]
            elif n in _LAYER_KEYS and mode != "FUSED":
                m[n] = W[n][li:li + 1]
            else:
                m[n] = W[n]
        in_maps.append(m)
    ncores = int(os.environ.get("MK_NCORES", "8"))
    res = run_bass_kernel_spmd(nc, in_maps[:ncores], core_ids=list(range(ncores)))
    return res.results


_INPUT_NAMES = {}


def _dram_inputs(nc):
    return [(n, None) for n in _INPUT_NAMES[id(nc)]]


def kernel_unfused(**inp):
    W, cores = host_prep(inp)
    out = np.zeros((2, 8192, D), np.float32)
    state = [dict(xT=cores[i]["xT"], ctxT=cores[i]["ctxT"]) for i in range(8)]
    for li in range(DEPTH):
        pc = []
        for i in range(8):
            m = dict(cores[i])
            m.update(state[i])
            pc.append(m)
        r1 = _run("P1", li, W, pc)
        for i in range(8):
            b = i // 4
            grp = [r1[4 * b + r] for r in range(4)]
            for k in r1[i]:
                if k.startswith("st_") or k.startswith("c_"):
                    pc[i][k] = r1[i][k]
            pc[i]["g_kl"] = np.concatenate([g["x_kl"] for g in grp], axis=0)
            pc[i]["g_kr"] = np.concatenate([g["x_kr"] for g in grp], axis=0)
            pc[i]["g_v"] = np.concatenate([g["x_v"] for g in grp], axis=0)
            pc[i]["g_L"] = np.concatenate([g["x_L"] for g in grp], axis=0)
            pc[i]["g_A"] = np.concatenate([g["x_A"] for g in grp], axis=0)
        r2 = _run("P2", li, W, pc)
        if li == DEPTH - 1:
            for i in range(8):
                b, j = i // 4, i % 4
                out[b, j * NL:(j + 1) * NL, :] = r2[i]["outT"].T
        else:
            for i in range(8):
                state[i] = dict(xT=r2[i]["xT_out"], ctxT=r2[i]["ctxT_out"])
    return out


def kernel(**inp):
    W, cores = host_prep(inp)
    r = _run("FUSED", 0, W, cores)
    out = np.zeros((2, 8192, D), np.float32)
    for i in range(8):
        b, j = i // 4, i % 4
        out[b, j * NL:(j + 1) * NL, :] = r[i]["outT"].T
    return out
```

```python
import math
from contextlib import ExitStack

import numpy as np
import ml_dtypes

import concourse.bass as bass
import concourse.mybir as mybir
from concourse.bass_utils import run_bass_kernel_spmd

F32 = mybir.dt.float32
BF16 = mybir.dt.bfloat16
AF = mybir.ActivationFunctionType
ALU = mybir.AluOpType

D = 1024
NL = 2048
NCX = 256
NT = NL + NCX
DEPTH = 2
EPS = 1e-6
DIN = 2016
DINA = 2080
NKEY = NCX + 4 * NL
NKT = NKEY // 128
DFF0 = 2816
DFFE = 3584
NEXP = 8
MLA_SCALE = (128 + 64) ** -0.5

ENG_NAMES = ("pe", "act", "dve", "pool", "sp")


class Op:
    __slots__ = ("eng", "fn", "reads", "writes", "is_dma", "idx", "waits", "marked",
                 "semval", "dma_sem", "dma_val")

    def __init__(self, eng, fn, reads, writes, is_dma):
        self.eng = eng
        self.fn = fn
        self.reads = reads
        self.writes = writes
        self.is_dma = is_dma
        self.waits = []
        self.marked = False
        self.semval = 0
        self.dma_sem = None
        self.dma_val = 0


class Sched:
    def __init__(self, nc, n_dma_sems=12, same_engine_sync=True):
        self.nc = nc
        self.ops = {e: [] for e in ENG_NAMES}
        self.order = []
        self.n_dma_sems = n_dma_sems
        self.same_engine_sync = same_engine_sync
        self.cc_ops = set()

    muted = False

    def op(self, eng, fn, reads=(), writes=()):
        writes = tuple(writes) + tuple(k for k in reads if k.startswith("pb") and k not in writes)
        o = Op(eng, fn, tuple(reads), tuple(writes), False)
        if self.muted:
            return o
        o.idx = len(self.ops[eng])
        self.ops[eng].append(o)
        self.order.append(o)
        return o

    def dma(self, eng, fn, reads=(), writes=()):
        o = Op(eng, fn, tuple(reads), tuple(writes), True)
        if self.muted:
            return o
        o.idx = len(self.ops[eng])
        self.ops[eng].append(o)
        self.order.append(o)
        return o

    def barrier(self):
        self.order.append("BARRIER")

    def coll(self, eng, fn, reads=(), writes=()):
        o = self.dma(eng, fn, reads, writes)
        self.cc_ops.add(id(o))
        return o

    def analyze(self):
        last_w = {}
        readers = {}
        seen = {e: {f: -1 for f in ENG_NAMES} for e in ENG_NAMES}
        seen_dma = {e: set() for e in ENG_NAMES}
        dma_ctr = {e: 0 for e in ENG_NAMES}
        dma_cnt = {}
        dma_last = {}
        bar_deps = []
        bar_pending = {e: False for e in ENG_NAMES}
        last_op = {}
        for o in self.order:
            if isinstance(o, str):
                bar_deps = list(last_op.values()) + list(dma_last.values())
                bar_pending = {e: True for e in ENG_NAMES}
                continue
            deps = []
            if bar_pending[o.eng]:
                deps.extend(bar_deps)
                bar_pending[o.eng] = False
            if not o.is_dma:
                last_op[o.eng] = o
            for k in o.reads:
                w = last_w.get(k)
                if w is not None:
                    deps.append(w)
            for k in o.writes:
                w = last_w.get(k)
                if w is not None:
                    deps.append(w)
                for r in readers.get(k, ()):
                    deps.append(r)
            if o.is_dma:
                e = o.eng
                if id(o) in self.cc_ops:
                    sidx = ("cc", 0)
                    inc = 1
                else:
                    sidx = (e, dma_ctr[e] % self.n_dma_sems)
                    dma_ctr[e] += 1
                    inc = 16
                prev = dma_last.get(sidx)
                if prev is not None:
                    deps.append(prev)
                dma_cnt[sidx] = dma_cnt.get(sidx, 0) + inc
                o.dma_sem = sidx
                o.dma_val = dma_cnt[sidx]
                dma_last[sidx] = o
            need = {}
            for d in deps:
                if d is o:
                    continue
                if d.is_dma:
                    if id(d) in seen_dma[o.eng]:
                        continue
                    need[("dma", id(d))] = d
                else:
                    if d.eng == o.eng and not o.is_dma:
                        if o.eng == "pe" or not self.same_engine_sync:
                            continue
                    if seen[o.eng][d.eng] >= d.idx:
                        continue
                    cur = need.get(("eng", d.eng))
                    if cur is None or cur.idx < d.idx:
                        need[("eng", d.eng)] = d
            for key, d in need.items():
                d.marked = True
                o.waits.append(d)
                if d.is_dma:
                    seen_dma[o.eng].add(id(d))
                else:
                    seen[o.eng][d.eng] = d.idx
            for k in o.writes:
                last_w[k] = o
                readers[k] = []
            for k in o.reads:
                if k not in o.writes:
                    readers.setdefault(k, []).append(o)
        self.dma_last_all = list(dma_last.values())
        for e in ENG_NAMES:
            c = 0
            for o in self.ops[e]:
                if not o.is_dma and o.marked:
                    c += 1
                    o.semval = c

    def emit(self, final_wait_ops=()):
        nc = self.nc
        self.analyze()
        with ExitStack() as es:
            esem = {e: es.enter_context(nc.semaphore("sem_" + e)) for e in ENG_NAMES}
            dsem = {}
            for e in ENG_NAMES:
                if any(o.is_dma for o in self.ops[e]):
                    for i in range(self.n_dma_sems):
                        dsem[(e, i)] = es.enter_context(nc.semaphore("dsem_%s_%d" % (e, i)))
            if self.cc_ops:
                dsem[("cc", 0)] = es.enter_context(nc.semaphore("ccsem"))
            block = es.enter_context(nc.Block())
            hw = {"pe": block.tensor, "act": block.scalar, "dve": block.vector,
                  "pool": block.gpsimd, "sp": block.sync}

            def make(e):
                def body(eng):
                    for o in self.ops[e]:
                        for d in o.waits:
                            if d.is_dma:
                                eng.wait_ge(dsem[d.dma_sem], d.dma_val)
                            else:
                                eng.wait_ge(esem[d.eng], d.semval)
                        ins = o.fn(eng)
                        if o.is_dma and id(o) in self.cc_ops:
                            ins.then_inc(dsem[o.dma_sem])
                        elif o.is_dma:
                            ins.then_inc(dsem[o.dma_sem], 16)
                        elif o.marked:
                            ins.then_inc(esem[e], 1)
                    if e == "sp":
                        for d in self.dma_last_all:
                            eng.wait_ge(dsem[d.dma_sem], d.dma_val)
                return body

            for e in ENG_NAMES:
                if self.ops[e] or e == "sp":
                    hw[e](make(e))


class Ctx:
    pass


class _Stop(Exception):
    pass


import os
_STOP = float(os.environ.get("MK_STOP", "99"))


_CUR = [None]


def _stage(n):
    if _STOP <= n:
        _CUR[0].muted = True


def _mm(S, out, lhsT, rhs, start, stop, r, w):
    S.op("pe", lambda e: e.matmul(out, lhsT=lhsT, rhs=rhs, start=start, stop=stop), reads=r, writes=w)


def _act(S, out, in_, func, r, w, scale=1.0, bias=None, accum=None):
    def f(e):
        kw = {}
        if bias is not None:
            kw["bias"] = bias
        if accum is not None:
            kw["accum_out"] = accum
        return e.activation(out=out, in_=in_, func=func, scale=scale, **kw)
    S.op("act", f, reads=r, writes=w)


def _tt(S, eng, out, in0, in1, op, r, w):
    S.op(eng, lambda e: e.tensor_tensor(out=out, in0=in0, in1=in1, op=op), reads=r, writes=w)


def _stt(S, eng, out, in0, scalar, in1, op0, op1, r, w):
    S.op(eng, lambda e: e.scalar_tensor_tensor(out=out, in0=in0, scalar=scalar, in1=in1, op0=op0, op1=op1),
         reads=r, writes=w)


def _ts(S, eng, out, in0, s1, s2, op0, op1, r, w):
    if s2 is None:
        S.op(eng, lambda e: e.tensor_scalar(out=out, in0=in0, scalar1=s1, scalar2=None, op0=op0), reads=r, writes=w)
    else:
        S.op(eng, lambda e: e.tensor_scalar(out=out, in0=in0, scalar1=s1, scalar2=s2, op0=op0, op1=op1),
             reads=r, writes=w)


def _cp(S, eng, out, in_, r, w):
    if eng == "act":
        S.op(eng, lambda e: e.activation(out=out, in_=in_, func=AF.Identity, scale=1.0), reads=r, writes=w)
    else:
        S.op(eng, lambda e: e.tensor_copy(out=out, in_=in_), reads=r, writes=w)


def _dma(S, q, out, in_, r, w):
    return S.dma(q, lambda e: e.dma_start(out=out, in_=in_), reads=r, writes=w)


def _memset(S, eng, ap, val, w):
    S.op(eng, lambda e: e.memset(ap, val), writes=w)


def _recip(S, out, in_, r, w):
    S.op("dve", lambda e: e.reciprocal(out=out, in_=in_), reads=r, writes=w)


def make_dram(K, mode):
    nc = K.nc
    K.dram = {}
    P1 = mode in ("P1", "FUSED")
    P2 = mode in ("P2", "FUSED")

    def decl(name, shape, dt, role, need=True):
        if not need:
            return
        if role == "in":
            kind = "ExternalInput"
        elif role == "st":
            kind = {"P1": "ExternalOutput", "P2": "ExternalInput", "FUSED": "Internal"}[mode]
        elif role == "own":
            kind = {"P1": "ExternalOutput", "P2": None, "FUSED": "Internal"}[mode]
        elif role == "gat":
            kind = {"P1": None, "P2": "ExternalInput", "FUSED": "Internal"}[mode]
        elif role == "xout":
            kind = {"P1": None, "P2": "ExternalOutput", "FUSED": "Internal"}[mode]
        elif role == "p2s":
            kind = {"P1": None, "P2": "Internal", "FUSED": "Internal"}[mode]
        elif role == "out":
            kind = "ExternalOutput"
        if kind is None:
            return
        if kind == "Internal":
            t = nc.dram_tensor(name, shape, dt)
        else:
            t = nc.dram_tensor(name, shape, dt, kind=kind)
        if kind == "ExternalInput":
            _INPUT_NAMES.setdefault(id(nc), []).append(name)
        K.dram[name] = t.ap()

    nlay = K.nlay
    decl("xT", [D, NL], F32, "in")
    decl("ctxT", [D, NCX], F32, "in")
    decl("cT", [D, 2], F32, "in", P1)
    decl("cst", [128, 5 * 128], F32, "in")
    decl("rmask", [64, 8], F32, "in", P2)
    decl("ropec", [64, NL], F32, "in", P1)
    decl("ropes", [64, NL], F32, "in", P1)
    decl("w_mod", [nlay, D, 6 * D], F32, "in", P1)
    decl("bmodT", [nlay, 128, 48], F32, "in", P1)
    decl("lnT", [nlay, 128, 16], F32, "in", P1)
    decl("w_in", [nlay, D, DINA], F32, "in", P1)
    decl("wg2", [nlay, 33, 512], F32, "in", P1)
    decl("gvec", [nlay, 128, 8], F32, "in")
    decl("kvg_row", [nlay, 1, 128], F32, "in", P1)
    decl("w_uq", [nlay, 256, 1024], F32, "in", P1)
    decl("w_ukT", [nlay, 128, 512], F32, "in", P1)
    decl("w_uv", [nlay, 128, 512], F32, "in", P2)
    decl("w_out", [nlay, D, D], F32, "in", P2)
    decl("ffn_g", [D, DFF0], F32, "in", P2 and K.has_dense)
    decl("ffn_u", [D, DFF0], F32, "in", P2 and K.has_dense)
    decl("ffn_d", [DFF0, D], F32, "in", P2 and K.has_dense)
    decl("router", [D, NEXP], F32, "in", P2 and K.has_moe)
    decl("exp_g", [NEXP, D, DFFE], F32, "in", P2 and K.has_moe)
    decl("exp_u", [NEXP, D, DFFE], F32, "in", P2 and K.has_moe)
    decl("exp_d", [NEXP, DFFE, D], F32, "in", P2 and K.has_moe)
    decl("combT", [NEXP, NL], F32, "xout", P2 and K.has_moe)
    decl("fin_g", [128, 8], F32, "in", P2 and K.has_last)
    decl("dbg_lg", [128, 128], F32, "out", mode == "P2" and K.has_moe and bool(os.environ.get("MK_DBGLG")))
    decl("st_qp", [128, 4, NT], BF16, "st")
    decl("st_qr", [64, 4, NT], BF16, "st")
    decl("st_oi", [128, 4, NT], F32, "st")
    decl("st_qf", [64, 4, NT], BF16, "st")
    decl("st_qb", [64, 4, NT], BF16, "st")
    decl("st_sr", [128, 4, NT], BF16, "st")
    decl("st_U", [64, 18, 8, 128], F32, "st")
    decl("st_dec", [64, 18, 8], F32, "st")
    decl("st_par", [128, 2, 6, 8], F32, "st")
    decl("st_par2", [128, 2, 6, 8], F32, "p2s", mode == "FUSED")
    decl("st_S", [64, 18, 8, 128], BF16, "p2s")
    decl("c_kl", [128, NCX], BF16, "st")
    decl("c_kr", [64, NCX], BF16, "st")
    decl("c_v", [NCX, 128], BF16, "st")
    decl("x_kl", [128, NL], BF16, "own")
    decl("x_kr", [64, NL], BF16, "own")
    decl("x_v", [NL, 128], BF16, "own")
    decl("x_L", [64, 1024], F32, "own")
    decl("x_A", [64, 8], F32, "own")
    decl("g_kl", [4 * 128, NL], BF16, "gat")
    decl("g_kr", [4 * 64, NL], BF16, "gat")
    decl("g_v", [4 * NL, 128], BF16, "gat")
    decl("g_L", [4 * 64, 1024], F32, "gat")
    decl("g_A", [4 * 64, 8], F32, "gat")
    decl("xT_out", [D, NL], F32, "xout", P2 and not (mode == "P2" and K.has_last))
    decl("ctxT_out", [D, NCX], F32, "xout", P2 and not (mode == "P2" and K.has_last))
    decl("outT", [D, NL], F32, "out", P2 and K.has_last)


def mod_stage_steps(K, lw, sb, bank, bkey, par, pkey, sT, skey, bmodT, bkey2, lnT, lkey):
    S, Dm = K.S, K.dram
    modraw = sb("modraw", [128, 96], F32)
    wm = [sb("wm%d" % i, [128, 8, 256], F32) for i in range(2)]
    mk = "modraw_%d" % lw

    def step(j):
        buf = wm[j % 2]
        key = "wm%d_%d" % (j % 2, lw)
        _dma(S, "sp", buf[:], Dm["w_mod"][lw, :, j * 256:(j + 1) * 256].rearrange("(dc p) n -> p dc n", p=128),
             [], [key])
        for f in range(2):
            fc = j * 2 + f
            for dc in range(8):
                _mm(S, bank[:, fc * 2:fc * 2 + 2], buf[:, dc, f * 128:(f + 1) * 128], sT[:, dc, :],
                    dc == 0, dc == 7, [key, skey], [bkey])

    def fin():
        _cp(S, "dve", modraw[:], bank[:, 0:96], [bkey], [mk])
        mr = modraw[:].rearrange("p (f c) -> p c f", c=2)
        for col in range(2):
            _tt(S, "dve", par[:, col, :, :].rearrange("p a b -> p (a b)"), mr[:, col, :], bmodT[:], ALU.add,
                [mk, bkey2], [pkey])
        for col in range(2):
            _stt(S, "dve", par[:, col, 1, :], par[:, col, 1, :], 1.0, lnT[:, 0:8], ALU.add, ALU.mult,
                 [pkey, lkey], [pkey])
            _stt(S, "dve", par[:, col, 4, :], par[:, col, 4, :], 1.0, lnT[:, 8:16], ALU.add, ALU.mult,
                 [pkey, lkey], [pkey])

    return [(lambda j=j: step(j)) for j in range(24)], fin


BLOCKS = [("x", 0, i * 512, 512, i * 512) for i in range(4)] + [("c", 1, 0, 256, NL)]


def phase1(K, lw, xin, xkey, cin, ckey):
    nc, S, Dm = K.nc, K.S, K.dram
    with ExitStack() as es:
        def sb(name, shape, dt):
            return es.enter_context(nc.sbuf_tensor("p1_%d_%s" % (lw, name), shape, dt))
        cst = K.cst
        triF = cst[:, 128:256]
        triB = cst[:, 256:384]
        triSU = cst[:, 384:512]
        triSL = cst[:, 512:640]
        PB = K.pb
        ones = K.ones_bf
        onesf = K.ones_f
        win = sb("win", [128, 8, DINA], BF16)
        for dc in range(8):
            _dma(S, "pool", win[:, dc, :], Dm["w_in"][lw, dc * 128:(dc + 1) * 128, :], [], ["win"])
        wuq = sb("wuq", [128, 2, 1024], BF16)
        for kc in range(2):
            _dma(S, "pool", wuq[:, kc, :], Dm["w_uq"][lw, kc * 128:(kc + 1) * 128, :], [], ["wuq"])
        wukT = sb("wukT", [128, 512], BF16)
        _dma(S, "pool", wukT[:], Dm["w_ukT"][lw], [], ["wukT"])
        wg2 = sb("wg2", [33, 512], F32)
        _dma(S, "sp", wg2[:], Dm["wg2"][lw], [], ["wg2"])
        gvec = sb("gvec", [128, 8], F32)
        _dma(S, "sp", gvec[:], Dm["gvec"][lw], [], ["gvec"])
        kvgb = sb("kvgb", [128, 128], F32)
        _dma(S, "sp", kvgb[:], Dm["kvg_row"][lw].partition_broadcast(128), [], ["kvgb"])
        lnT = sb("lnT", [128, 16], F32)
        _dma(S, "sp", lnT[:], Dm["lnT"][lw], [], ["lnT"])
        bmodT = sb("bmodT", [128, 48], F32)
        _dma(S, "sp", bmodT[:], Dm["bmodT"][lw], [], ["bmodT"])
        cT = sb("cT", [128, 8, 2], F32)
        _dma(S, "sp", cT[:], Dm["cT"].rearrange("(dc p) c -> p dc c", p=128), [], ["cT"])
        sT = sb("sT", [128, 8, 2], F32)
        _act(S, sT[:], cT[:], AF.Silu, ["cT"], ["sT"])
        par = sb("par", [128, 2, 6, 8], F32)
        if lw in K.par_pre:
            _dma(S, "sp", par[:], Dm["st_par2"], ["st_par2"], ["par"])
        else:
            steps, fin = mod_stage_steps(K, lw, sb, PB[0], "pb0", par, "par", sT, "sT", bmodT, "bmodT", lnT, "lnT")
            for st_ in steps:
                st_()
            fin()
        _dma(S, "sp", Dm["st_par"], par[:], ["par"], ["st_par"])
        _stage(1)
        xb = sb("xb", [128, 8, 512], F32)
        sq = [sb("sq%d" % i, [128, 512], BF16) for i in range(2)]
        rstd = sb("rstd", [128, 512], F32)
        rtmp = sb("rtmp", [128, 512], F32)
        hT = sb("hT", [128, 8, 512], BF16)
        htmp = [sb("htmp%d" % i, [128, 512], F32) for i in range(2)]
        qf = sb("qf", [64, 4, 512], F32)
        kf = sb("kf", [64, 4, 512], F32)
        srs = sb("srs", [128, 4, 512], BF16)
        cqf = sb("cqf", [128, 2, 512], F32)
        cqn = sb("cqn", [128, 2, 512], BF16)
        ckvf = sb("ckvf", [128, 512], F32)
        krf = sb("krf", [64, 2, 512], F32)
        codT = sb("codT", [33, 512], F32)
        _memset(S, "dve", codT[32:33, :], 1.0, ["codT"])
        vtm = sb("vtm", [128, 4, 512], BF16)
        ktm = sb("ktm", [128, 4, 256], F32)
        qnh = sb("qnh", [128, 512], BF16)
        qps = sb("qps", [128, 4, 512], BF16)
        qrs = sb("qrs", [64, 4, 512], BF16)
        rt1 = sb("rt1", [64, 512], F32)
        rt2 = sb("rt2", [64, 512], F32)
        cosb = sb("cosb", [64, 512], F32)
        sinb = sb("sinb", [64, 512], F32)
        kls = sb("kls", [128, 512], BF16)
        krs = sb("krs", [64, 512], BF16)
        vls = sb("vls", [128, 4, 128], BF16)
        vss = sb("vss", [128, 4], F32)
        vjunk = sb("vjunk", [128, 128], F32)
        vsq = sb("vsq", [128, 128], F32)
        vs1 = sb("vs1", [128, 4], F32)
        vs2 = sb("vs2", [128, 4], F32)
        Gn = sb("Gn", [128, 512], F32)
        Ge = sb("Ge", [128, 512], F32)
        E1 = sb("E1", [64, 2, 512], F32)
        E2 = sb("E2", [64, 2, 512], F32)
        E3 = sb("E3", [128, 512], F32)
        qtf = sb("qtf", [64, 4, 512], BF16)
        qtb = sb("qtb", [64, 4, 512], BF16)
        ktf = sb("ktf", [64, 4, 128], BF16)
        ktb = sb("ktb", [64, 4, 128], BF16)
        khf = sb("khf", [128, 256], BF16)
        khb = sb("khb", [128, 256], BF16)
        amt = sb("amt", [128, 128], BF16)
        am1 = sb("am1", [128, 128], F32)
        am2 = sb("am2", [128, 128], F32)
        ois = sb("ois", [128, 4, 512], F32)
        Us = sb("Us", [64, 8, 128], F32)
        decs = sb("decs", [64, 4, 8], F32)
        Lf = sb("Lf", [64, 4, 128], F32)
        Lb = sb("Lb", [64, 4, 128], F32)
        PA = sb("PA", [64, 8], F32)

        for (src, col, n0, N, g0) in BLOCKS:
            xdr = (xin if src == "x" else cin)
            ntile = N // 128
            a1 = par[:, col, 1, :]
            sh1 = par[:, col, 0, :]
            _dma(S, "sp", xb[:, :, :N], xdr[:, n0:n0 + N].rearrange("(dc p) n -> p dc n", p=128),
                 [xkey if src == "x" else ckey], ["xb"])
            for dc in range(8):
                sqb, sqk = sq[dc % 2], "sq%d" % (dc % 2)
                _tt(S, "pool", sqb[:, :N], xb[:, dc, :N], xb[:, dc, :N], ALU.mult, ["xb"], [sqk])
                _mm(S, PB[2][:, :N], ones[:], sqb[:, :N], dc == 0, dc == 7, [sqk, "ones"], ["pb2"])
            _stage(1.3)
            _act(S, rtmp[:, :N], PB[2][:, :N], AF.Sqrt, ["pb2"], ["rtmp"], scale=1.0 / D, bias=K.eps_ap)
            _recip(S, rstd[:, :N], rtmp[:, :N], ["rtmp"], ["rstd"])
            _stage(1.6)
            for dc in range(8):
                ht = htmp[dc % 2]
                hk = "htmp%d" % (dc % 2)
                _stt(S, "dve", ht[:, :N], xb[:, dc, :N], a1[:, dc:dc + 1], rstd[:, :N], ALU.mult, ALU.mult,
                     ["xb", "par", "rstd"], [hk])
                _act(S, hT[:, dc, :N], ht[:, :N], AF.Identity, [hk, "par"], ["hT"], bias=sh1[:, dc:dc + 1])
            _stage(2)
            pbi = [0]

            def nextpb():
                b = pbi[0] % 2
                pbi[0] += 1
                return PB[b], "pb%d" % b

            def proj(c0, ncols, evac):
                ps, pk = nextpb()
                for dc in range(8):
                    _mm(S, ps[0:ncols, :N], win[:, dc, c0:c0 + ncols], hT[:, dc, :N], dc == 0, dc == 7,
                        ["win", "hT"], [pk])
                evac(ps, pk)

            for h in range(4):
                proj(h * 64, 64, lambda ps, pk, h=h: _ts(S, "dve", qf[:, h, :N], ps[0:64, :N], 0.125, None, ALU.mult, None,
                                                         [pk], ["qf"]))
            for h in range(4):
                proj(256 + h * 64, 64, lambda ps, pk, h=h: _cp(S, "dve", kf[:, h, :N], ps[0:64, :N], [pk], ["kf"]))
            for c in range(4):
                proj(1056 + c * 128, 128, lambda ps, pk, c=c: _act(S, srs[:, c, :N], ps[:, :N], AF.Silu, [pk], ["srs"]))
            _dma(S, "sp", Dm["st_sr"][:, :, g0:g0 + N], srs[:, :, :N], ["srs"], ["st_sr"])
            for c in range(2):
                proj(1568 + c * 128, 128, lambda ps, pk, c=c: _cp(S, "dve", cqf[:, c, :N], ps[:, :N], [pk], ["cqf"]))
            proj(1824, 128, lambda ps, pk: _cp(S, "dve", ckvf[:, :N], ps[:, :N], [pk], ["ckvf"]))
            proj(1952, 64, lambda ps, pk: _cp(S, "dve", krf[:, 0, :N], ps[0:64, :N], [pk], ["krf"]))
            proj(2016, 64, lambda ps, pk: _cp(S, "dve", krf[:, 1, :N], ps[0:64, :N], [pk], ["krf"]))
            proj(1024, 32, lambda ps, pk: _cp(S, "dve", codT[0:32, :N], ps[0:32, :N], [pk], ["codT"]))
            _stage(3)
            _memset(S, "dve", vss[:], 0.0, ["vss"])
            for t in range(ntile):
                tsl = slice(t * 128, (t + 1) * 128)
                ps, pk = nextpb()
                for dc in range(8):
                    _mm(S, ps[:, 0:512], hT[:, dc, tsl], win[:, dc, 512:1024], dc == 0, dc == 7, ["win", "hT"], [pk])
                _cp(S, "act", vtm[:, t, :], ps[:, 0:512], [pk], ["vtm"])
                ps, pk = nextpb()
                for dc in range(8):
                    _mm(S, ps[:, 0:256], hT[:, dc, tsl], win[:, dc, 256:512], dc == 0, dc == 7, ["win", "hT"], [pk])
                for dc in range(8):
                    _mm(S, ps[:, 256:384], hT[:, dc, tsl], win[:, dc, 1824:1952], dc == 0, dc == 7, ["win", "hT"], [pk])
                _stage(3.2)
                _cp(S, "dve", ktm[:, t, :], ps[:, 0:256], [pk], ["ktm"])
                _stage(3.4)
                _cp(S, "dve", vjunk[:], ps[:, 256:384], [pk], ["vjunk"])
                _tt(S, "pool", vsq[:], vjunk[:], vjunk[:], ALU.mult, ["vjunk"], ["vsq"])
                S.op("dve", lambda e, t=t: e.reduce_sum(out=vss[:, t:t + 1], in_=vsq[:], axis=mybir.AxisListType.X),
                     reads=["vsq"], writes=["vss"])
                _stage(3.5)
                _act(S, vs1[:, t:t + 1], vss[:, t:t + 1], AF.Sqrt, ["vss"], ["vs1"], scale=1.0 / 128, bias=K.eps_ap)
                _stage(3.6)
                _recip(S, vs2[:, t:t + 1], vs1[:, t:t + 1], ["vs1"], ["vs2"])
                _stage(3.7)
                _stt(S, "dve", vls[:, t, :], vjunk[:], vs2[:, t:t + 1], kvgb[:], ALU.mult, ALU.mult,
                     ["vjunk", "vs2", "kvgb"], ["vls"])
            vdst = (Dm["x_v"][n0:n0 + N, :] if src == "x" else Dm["c_v"][:, :]).rearrange("(t p) r -> p t r", p=128)
            vkey = "x_v" if src == "x" else "c_v"
            _stage(3.8)
            _dma(S, "sp", vdst, vls[:, :ntile, :], ["vls"], [vkey])
            _stage(4)
            for c in range(2):
                sqb, sqk = sq[c % 2], "sq%d" % (c % 2)
                _tt(S, "pool", sqb[:, :N], cqf[:, c, :N], cqf[:, c, :N], ALU.mult, ["cqf"], [sqk])
                _mm(S, PB[2][:, :N], ones[:], sqb[:, :N], c == 0, c == 1, [sqk, "ones"], ["pb2"])
            _act(S, rtmp[:, :N], PB[2][:, :N], AF.Sqrt, ["pb2"], ["rtmp"], scale=1.0 / 256, bias=K.eps_ap)
            _recip(S, rstd[:, :N], rtmp[:, :N], ["rtmp"], ["rstd"])
            for c in range(2):
                _stt(S, "dve", cqn[:, c, :N], cqf[:, c, :N], gvec[:, 1 + c:2 + c], rstd[:, :N], ALU.mult, ALU.mult,
                     ["cqf", "gvec", "rstd"], ["cqn"])
            if src == "x":
                _dma(S, "sp", cosb[:, :N], Dm["ropec"][:, n0:n0 + N], [], ["cosb"])
                _dma(S, "sp", sinb[:, :N], Dm["ropes"][:, n0:n0 + N], [], ["sinb"])
            for h in range(4):
                ps, pk = nextpb()
                for kc in range(2):
                    _mm(S, ps[:, :N], wuq[:, kc, h * 128:(h + 1) * 128], cqn[:, kc, :N], kc == 0, kc == 1,
                        ["wuq", "cqn"], [pk])
                _cp(S, "act", qnh[:, :N], ps[:, :N], [pk], ["qnh"])
                ps, pk = nextpb()
                _mm(S, ps[:, :N], wukT[:, h * 128:(h + 1) * 128], qnh[:, :N], True, True, ["wukT", "qnh"], [pk])
                _cp(S, "dve", qps[:, h, :N], ps[:, :N], [pk], ["qps"])
                ps, pk = nextpb()
                for kc in range(2):
                    _mm(S, ps[0:64, :N], wuq[:, kc, 512 + h * 64:512 + (h + 1) * 64], cqn[:, kc, :N], kc == 0, kc == 1,
                        ["wuq", "cqn"], [pk])
                if src == "x":
                    ps2, pk2 = nextpb()
                    for kc in range(2):
                        _mm(S, ps2[0:64, :N], wuq[:, kc, 768 + h * 64:768 + (h + 1) * 64], cqn[:, kc, :N], kc == 0, kc == 1,
                            ["wuq", "cqn"], [pk2])
                    _tt(S, "dve", rt1[:, :N], ps[0:64, :N], cosb[:, :N], ALU.mult, [pk, "cosb"], ["rt1"])
                    _tt(S, "dve", rt2[:, :N], ps2[0:64, :N], sinb[:, :N], ALU.mult, [pk2, "sinb"], ["rt2"])
                    _tt(S, "pool", qrs[:, h, :N], rt1[:, :N], rt2[:, :N], ALU.add, ["rt1", "rt2"], ["qrs"])
                else:
                    _cp(S, "dve", qrs[:, h, :N], ps[0:64, :N], [pk], ["qrs"])
            _dma(S, "sp", Dm["st_qp"][:, :, g0:g0 + N], qps[:, :, :N], ["qps"], ["st_qp"])
            _dma(S, "sp", Dm["st_qr"][:, :, g0:g0 + N], qrs[:, :, :N], ["qrs"], ["st_qr"])
            _tt(S, "pool", sq[0][:, :N], ckvf[:, :N], ckvf[:, :N], ALU.mult, ["ckvf"], ["sq0"])
            _mm(S, PB[2][:, :N], ones[:], sq[0][:, :N], True, True, ["sq0", "ones"], ["pb2"])
            _act(S, rtmp[:, :N], PB[2][:, :N], AF.Sqrt, ["pb2"], ["rtmp"], scale=1.0 / 128, bias=K.eps_ap)
            _recip(S, rstd[:, :N], rtmp[:, :N], ["rtmp"], ["rstd"])
            _stt(S, "dve", kls[:, :N], ckvf[:, :N], gvec[:, 3:4], rstd[:, :N], ALU.mult, ALU.mult,
                 ["ckvf", "gvec", "rstd"], ["kls"])
            if src == "x":
                _tt(S, "dve", rt1[:, :N], krf[:, 0, :N], cosb[:, :N], ALU.mult, ["krf", "cosb"], ["rt1"])
                _tt(S, "dve", rt2[:, :N], krf[:, 1, :N], sinb[:, :N], ALU.mult, ["krf", "sinb"], ["rt2"])
                _tt(S, "pool", krs[:, :N], rt1[:, :N], rt2[:, :N], ALU.add, ["rt1", "rt2"], ["krs"])
                _dma(S, "sp", Dm["x_kl"][:, n0:n0 + N], kls[:, :N], ["kls"], ["x_kl"])
                _dma(S, "sp", Dm["x_kr"][:, n0:n0 + N], krs[:, :N], ["krs"], ["x_kr"])
            else:
                _cp(S, "dve", krs[:, :N], krf[:, 0, :N], ["krf"], ["krs"])
                _dma(S, "sp", Dm["c_kl"][:, :], kls[:, :N], ["kls"], ["c_kl"])
                _dma(S, "sp", Dm["c_kr"][:, :], krs[:, :N], ["krs"], ["c_kr"])
            _stage(5)
            if src == "x" and n0 == 0:
                _memset(S, "dve", Lf[:], 0.0, ["Lf"])
                _memset(S, "dve", Lb[:], 0.0, ["Lb"])
                _memset(S, "dve", PA[:], 1.0, ["PA"])
            for t in range(ntile):
                tsl = slice(t * 128, (t + 1) * 128)
                gt = (g0 // 128) + t
                _mm(S, PB[3][:, :], codT[0:33, tsl], wg2[:], True, True, ["codT", "wg2"], ["pb3"])
                _act(S, Ge[:], PB[3][:, :], AF.Exp, ["pb3"], ["Ge"], scale=-1.0)
                _act(S, Gn[:], Ge[:], AF.Ln, ["Ge"], ["Gn"], scale=1.0, bias=K.one_ap)
                _stage(5.1)
                for h in range(4):
                    _mm(S, PB[4][0:64, h * 128:(h + 1) * 128], Gn[:, h * 64:(h + 1) * 64], triF, True, True,
                        ["Gn", "cst"], ["pb4"])
                    _mm(S, PB[5][0:64, h * 128:(h + 1) * 128], Gn[:, 256 + h * 64:256 + (h + 1) * 64], triB, True, True,
                        ["Gn", "cst"], ["pb5"])
                _mm(S, PB[3][:, 0:256], triSU, Gn[:, 0:256], True, True, ["Gn", "cst"], ["pb3"])
                _mm(S, PB[3][:, 256:512], triSL, Gn[:, 256:512], True, True, ["Gn", "cst"], ["pb3"])
                _stage(5.3)
                _act(S, E1[:, 0, :], PB[4][0:64, :], AF.Exp, ["pb4"], ["E1"], scale=-1.0 / 16)
                _act(S, E1[:, 1, :], PB[5][0:64, :], AF.Exp, ["pb5"], ["E1"], scale=-1.0 / 16)
                _act(S, E2[:, 0, :], PB[4][0:64, :], AF.Exp, ["pb4"], ["E2"], scale=1.0 / 16)
                _act(S, E2[:, 1, :], PB[5][0:64, :], AF.Exp, ["pb5"], ["E2"], scale=1.0 / 16)
                _act(S, E3[:], PB[3][:, :], AF.Exp, ["pb3"], ["E3"], scale=-1.0 / 16)
                _stage(5.4)
                e1f = E1[:, 0, :].rearrange("p (h t) -> p h t", h=4)
                e1b = E1[:, 1, :].rearrange("p (h t) -> p h t", h=4)
                e2f = E2[:, 0, :].rearrange("p (h t) -> p h t", h=4)
                e2b = E2[:, 1, :].rearrange("p (h t) -> p h t", h=4)
                _tt(S, "dve", qtf[:, :, tsl], qf[:, :, tsl], e1f, ALU.mult, ["qf", "E1"], ["qtf"])
                _tt(S, "pool", qtb[:, :, tsl], qf[:, :, tsl], e1b, ALU.mult, ["qf", "E1"], ["qtb"])
                _tt(S, "dve", ktf[:], kf[:, :, tsl], e2f, ALU.mult, ["kf", "E2"], ["ktf"])
                _tt(S, "pool", ktb[:], kf[:, :, tsl], e2b, ALU.mult, ["kf", "E2"], ["ktb"])
                _cp(S, "dve", decs[:, t, 0:4], e1f[:, :, 127], ["E1"], ["decs"])
                _cp(S, "dve", decs[:, t, 4:8], e1b[:, :, 0], ["E1"], ["decs"])
                _tt(S, "dve", khf[:], ktm[:, t, :], E3[:, 0:256], ALU.mult, ["ktm", "E3"], ["khf"])
                _tt(S, "pool", khb[:], ktm[:, t, :], E3[:, 256:512], ALU.mult, ["ktm", "E3"], ["khb"])
                _stage(5.5)
                for h in range(int(os.environ.get("MK_NH", "4"))):
                    ps, pk = PB[6], "pb6"
                    _mm(S, ps[:, 0:128], ktf[:, h, :], qtf[:, h, tsl], True, True, ["ktf", "qtf"], [pk])
                    _mm(S, ps[:, 128:256], ktb[:, h, :], qtb[:, h, tsl], True, True, ["ktb", "qtb"], [pk])
                    _stage(5.6)
                    _tt(S, "dve", am1[:], ps[:, 0:128], triF, ALU.mult, [pk, "cst"], ["am1"])
                    _tt(S, "dve", am2[:], ps[:, 128:256], triB, ALU.mult, [pk, "cst"], ["am2"])
                    _tt(S, "pool", amt[:], am1[:], am2[:], ALU.add, ["am1", "am2"], ["amt"])
                    _stage(5.7)
                    _mm(S, PB[7][:, 0:128], vtm[:, t, h * 128:(h + 1) * 128], amt[:], True, True, ["vtm", "amt"], ["pb7"])
                    _mm(S, PB[7][0:64, 128:256], khf[:, h * 64:(h + 1) * 64], vtm[:, t, h * 128:(h + 1) * 128], True, True,
                        ["khf", "vtm"], ["pb7"])
                    _mm(S, PB[7][0:64, 256:384], khb[:, h * 64:(h + 1) * 64], vtm[:, t, h * 128:(h + 1) * 128], True, True,
                        ["khb", "vtm"], ["pb7"])
                    _stage(5.8)
                    _cp(S, "act", ois[:, h, tsl], PB[7][:, 0:128], ["pb7"], ["ois"])
                    _cp(S, "dve", Us[:, h, :], PB[7][0:64, 128:256], ["pb7"], ["Us"])
                    _cp(S, "dve", Us[:, 4 + h, :], PB[7][0:64, 256:384], ["pb7"], ["Us"])
                    _stage(5.85)
                    if src == "x" and not os.environ.get("MK_NOL"):
                        _stt(S, "dve", Lf[:, h, :], Lf[:, h, :], decs[:, t, h:h + 1], Us[:, h, :], ALU.mult, ALU.add,
                             ["Lf", "decs", "Us"], ["Lf"])
                        if not os.environ.get("MK_NOLB"):
                            _stt(S, "dve", Lb[:, h, :], Us[:, 4 + h, :], PA[:, 4 + h:5 + h], Lb[:, h, :], ALU.mult, ALU.add,
                                 ["Lb", "PA", "Us"], ["Lb"])
                _stage(5.9)
                _dma(S, "sp", Dm["st_U"][:, gt, :, :], Us[:], ["Us"], ["st_U"])
                _stage(5.92)
                if src == "x":
                    _tt(S, "dve", PA[:], PA[:], decs[:, t, :], ALU.mult, ["PA", "decs"], ["PA"])
            t0 = g0 // 128
            _stage(5.95)
            _dma(S, "sp", Dm["st_oi"][:, :, g0:g0 + N], ois[:, :, :N], ["ois"], ["st_oi"])
            _dma(S, "sp", Dm["st_qf"][:, :, g0:g0 + N], qtf[:, :, :N], ["qtf"], ["st_qf"])
            _dma(S, "sp", Dm["st_qb"][:, :, g0:g0 + N], qtb[:, :, :N], ["qtb"], ["st_qb"])
            _dma(S, "sp", Dm["st_dec"][:, t0:t0 + ntile, :], decs[:, :ntile, :], ["decs"], ["st_dec"])
            if src == "x" and n0 == NL - 512:
                _dma(S, "sp", Dm["x_L"][:, 0:512].rearrange("p (h v) -> p h v", h=4), Lf[:], ["Lf"], ["x_L"])
                _dma(S, "sp", Dm["x_L"][:, 512:1024].rearrange("p (h v) -> p h v", h=4), Lb[:], ["Lb"], ["x_L"])
                _dma(S, "sp", Dm["x_A"][:, :], PA[:], ["PA"], ["x_A"])
    S.barrier()


def phase2(K, li, lw, xin, xkey, cin, ckey, is_last):
    nc, S, Dm = K.nc, K.S, K.dram
    need_ctx = li < DEPTH - 1
    dense = (li % 2 == 0)
    PB = K.pb
    ones = K.ones_bf
    onesf = K.ones_f
    cst = K.cst
    ident = cst[:, 0:128]
    blocks = [b for b in BLOCKS if (b[0] == "x" or need_ctx)]
    with ExitStack() as es0:
        def sb0(name, shape, dt):
            return es0.enter_context(nc.sbuf_tensor("p2_%d_%s" % (li, name), shape, dt))
        xs = sb0("xs", [128, 8, NT], F32)
        _dma(S, "sp", xs[:, :, 0:NL], xin.rearrange("(dc p) n -> p dc n", p=128), [xkey], ["xs"])
        _dma(S, "sp", xs[:, :, NL:NT], cin.rearrange("(dc p) n -> p dc n", p=128), [ckey], ["xs"])
        par = sb0("par", [128, 2, 6, 8], F32)
        _dma(S, "sp", par[:], Dm["st_par"], ["st_par"], ["par"])
        gvec = sb0("gvec", [128, 8], F32)
        _dma(S, "sp", gvec[:], Dm["gvec"][lw], [], ["gvec"])

        with ExitStack() as es:
            def sb(name, shape, dt):
                return es.enter_context(nc.sbuf_tensor("p2c_%d_%s" % (li, name), shape, dt))
            dec = sb("dec", [64, 18, 8], F32)
            _dma(S, "sp", dec[:], Dm["st_dec"], ["st_dec"], ["dec"])
            gL = sb("gL", [64, 4, 1024], F32)
            _dma(S, "sp", gL[:], Dm["g_L"].rearrange("(r p) n -> p r n", p=64), ["g_L"], ["gL"])
            gA = sb("gA", [64, 4, 8], F32)
            _dma(S, "sp", gA[:], Dm["g_A"].rearrange("(r p) n -> p r n", p=64), ["g_A"], ["gA"])
            rmask = sb("rmask", [64, 8], F32)
            _dma(S, "sp", rmask[:], Dm["rmask"], [], ["rmask"])
            Ub = [sb("U%d" % i, [64, 8, 128], F32) for i in range(2)]
            cur = sb("cur", [64, 8, 128], F32)
            Sb_ = [sb("Sb%d" % i, [64, 8, 128], BF16) for i in range(2)]
            uctr = [0]

            def loadU(gt):
                i = uctr[0] % 2
                uctr[0] += 1
                _dma(S, "sp", Ub[i][:], Dm["st_U"][:, gt, :, :], ["st_U"], ["U%d" % i])
                return Ub[i], "U%d" % i

            U16, k16 = loadU(16)
            U17, k17 = loadU(17)
            _memset(S, "dve", Sb_[0][:], 0.0, ["Sb0"])
            _memset(S, "dve", Sb_[1][:], 0.0, ["Sb1"])
            _cp(S, "dve", Sb_[1][:, 0:4, :], U16[:, 0:4, :], [k16], ["Sb1"])
            _cp(S, "dve", Sb_[0][:, 4:8, :], U17[:, 4:8, :], [k17], ["Sb0"])
            _dma(S, "sp", Dm["st_S"][:, 16, :, :], Sb_[0][:], ["Sb0"], ["st_S"])
            _dma(S, "sp", Dm["st_S"][:, 17, :, :], Sb_[1][:], ["Sb1"], ["st_S"])
            for h in range(4):
                _stt(S, "dve", cur[:, h, :], U16[:, h, :], dec[:, 17, h:h + 1], U17[:, h, :], ALU.mult, ALU.add,
                     [k16, k17, "dec"], ["cur"])
                _stt(S, "dve", cur[:, 4 + h, :], U17[:, 4 + h, :], dec[:, 16, 4 + h:5 + h], U16[:, 4 + h, :], ALU.mult, ALU.add,
                     [k16, k17, "dec"], ["cur"])
            Ap = sb("Ap", [64, 4, 8], F32)
            for r in range(4):
                _ts(S, "dve", Ap[:, r, 0:4], gA[:, r, 0:4], -1.0, rmask[:, r:r + 1], ALU.add, ALU.mult, ["gA", "rmask"], ["Ap"])
                _ts(S, "dve", Ap[:, r, 4:8], gA[:, r, 4:8], -1.0, rmask[:, 4 + r:5 + r], ALU.add, ALU.mult, ["gA", "rmask"], ["Ap"])
            _ts(S, "dve", Ap[:], Ap[:], 1.0, None, ALU.add, None, ["Ap"], ["Ap"])
            for r in range(4):
                _ts(S, "dve", gL[:, r, 0:512], gL[:, r, 0:512], rmask[:, r:r + 1], None, ALU.mult, None, ["gL", "rmask"], ["gL"])
                _ts(S, "dve", gL[:, r, 512:1024], gL[:, r, 512:1024], rmask[:, 4 + r:5 + r], None, ALU.mult, None,
                    ["gL", "rmask"], ["gL"])
            for r in range(4):
                for h in range(4):
                    _stt(S, "dve", cur[:, h, :], cur[:, h, :], Ap[:, r, h:h + 1], gL[:, r, h * 128:(h + 1) * 128],
                         ALU.mult, ALU.add, ["cur", "Ap", "gL"], ["cur"])
            for r in (3, 2, 1, 0):
                for h in range(4):
                    _stt(S, "dve", cur[:, 4 + h, :], cur[:, 4 + h, :], Ap[:, r, 4 + h:5 + h],
                         gL[:, r, 512 + h * 128:512 + (h + 1) * 128], ALU.mult, ALU.add, ["cur", "Ap", "gL"], ["cur"])
            Sf = sb("Sf", [64, 16, 4, 128], BF16)
            for i in range(16):
                U, uk = loadU(i)
                _cp(S, "pool", Sf[:, i, :, :], cur[:, 0:4, :], ["cur"], ["Sf"])
                for h in range(4):
                    _stt(S, "dve", cur[:, h, :], cur[:, h, :], dec[:, i, h:h + 1], U[:, h, :], ALU.mult, ALU.add,
                         ["cur", "dec", uk], ["cur"])
            sctr = 0
            for i in range(15, -1, -1):
                U, uk = loadU(i)
                sbuf_, sk = Sb_[sctr % 2], "Sb%d" % (sctr % 2)
                sctr += 1
                _cp(S, "pool", sbuf_[:, 0:4, :], Sf[:, i, :, :], ["Sf"], [sk])
                _cp(S, "pool", sbuf_[:, 4:8, :], cur[:, 4:8, :], ["cur"], [sk])
                _dma(S, "sp", Dm["st_S"][:, i, :, :], sbuf_[:], [sk], ["st_S"])
                for h in range(4):
                    _stt(S, "dve", cur[:, 4 + h, :], cur[:, 4 + h, :], dec[:, i, 4 + h:5 + h], U[:, 4 + h, :], ALU.mult, ALU.add,
                         ["cur", "dec", uk], ["cur"])
        S.barrier()

        with ExitStack() as es:
            def sb(name, shape, dt):
                return es.enter_context(nc.sbuf_tensor("p2a_%d_%s" % (li, name), shape, dt))
            KlT = sb("KlT", [128, NKEY], BF16)
            KrT = sb("KrT", [128, NKEY], BF16)
            _memset(S, "dve", KrT[64:128, :], 0.0, ["KrT"])
            Vl = sb("Vl", [128, NKT, 128], BF16)
            _dma(S, "sp", KlT[:, 0:NCX], Dm["c_kl"], ["c_kl"], ["KlT"])
            _dma(S, "sp", KrT[0:64, 0:NCX], Dm["c_kr"], ["c_kr"], ["KrT"])
            _dma(S, "sp", Vl[:, 0:2, :], Dm["c_v"].rearrange("(t p) r -> p t r", p=128), ["c_v"], ["Vl"])
            for r in range(4):
                _dma(S, "sp", KlT[:, NCX + r * NL:NCX + (r + 1) * NL], Dm["g_kl"][r * 128:(r + 1) * 128, :], ["g_kl"], ["KlT"])
                _dma(S, "sp", KrT[0:64, NCX + r * NL:NCX + (r + 1) * NL], Dm["g_kr"][r * 64:(r + 1) * 64, :], ["g_kr"], ["KrT"])
                _dma(S, "sp", Vl[:, 2 + 16 * r:2 + 16 * (r + 1), :],
                     Dm["g_v"][r * NL:(r + 1) * NL, :].rearrange("(t p) r -> p t r", p=128), ["g_v"], ["Vl"])
            wout = sb("wout", [128, 8, D], BF16)
            _dma(S, "pool", wout[:], Dm["w_out"][lw].rearrange("(c p) n -> p c n", p=128), [], ["wout"])
            wuv = sb("wuv", [128, 512], BF16)
            _dma(S, "pool", wuv[:], Dm["w_uv"][lw], [], ["wuv"])
            oi = sb("oi", [128, 4, 512], F32)
            qf = sb("qf", [64, 4, 512], BF16)
            qb = sb("qb", [64, 4, 512], BF16)
            sr = sb("sr", [128, 4, 512], BF16)
            qp = sb("qp", [128, 4, 512], BF16)
            qr = sb("qr", [128, 4, 512], BF16)
            _memset(S, "dve", qr[64:128, :, :], 0.0, ["qr"])
            Sblk = sb("Sblk", [64, 4, 8, 128], BF16)
            mix = sb("mix", [128, 8, 512], BF16)
            pT = [sb("pT%d" % i, [128, 512], BF16) for i in range(3)]
            racc = sb("racc", [128, 512], F32)
            rinv = sb("rinv", [128, 512], F32)
            rtmp = sb("rtmp", [128, 512], F32)
            sqb = sb("sqb", [128, 512], BF16)
            otmp = sb("otmp", [128, 512], F32)
            oln = sb("oln", [128, 512], BF16)
            for (src, col, n0, N, g0) in blocks:
                ntile = N // 128
                t0 = g0 // 128
                _dma(S, "sp", oi[:, :, :N], Dm["st_oi"][:, :, g0:g0 + N], ["st_oi"], ["oi"])
                _dma(S, "sp", qf[:, :, :N], Dm["st_qf"][:, :, g0:g0 + N], ["st_qf"], ["qf"])
                _dma(S, "sp", qb[:, :, :N], Dm["st_qb"][:, :, g0:g0 + N], ["st_qb"], ["qb"])
                _dma(S, "sp", sr[:, :, :N], Dm["st_sr"][:, :, g0:g0 + N], ["st_sr"], ["sr"])
                _dma(S, "sp", qp[:, :, :N], Dm["st_qp"][:, :, g0:g0 + N], ["st_qp"], ["qp"])
                _dma(S, "sp", qr[0:64, :, :N], Dm["st_qr"][:, :, g0:g0 + N], ["st_qr"], ["qr"])
                _dma(S, "sp", Sblk[:, :ntile, :, :], Dm["st_S"][:, t0:t0 + ntile, :, :], ["st_S"], ["Sblk"])
                for h in range(4):
                    for t in range(ntile):
                        tsl = slice(t * 128, (t + 1) * 128)
                        _mm(S, PB[3][:, tsl], Sblk[:, t, h, :], qf[:, h, tsl], True, False, ["Sblk", "qf"], ["pb3"])
                        _mm(S, PB[3][:, tsl], Sblk[:, t, 4 + h, :], qb[:, h, tsl], False, True, ["Sblk", "qb"], ["pb3"])
                    _tt(S, "dve", oi[:, h, :N], oi[:, h, :N], PB[3][:, :N], ALU.add, ["oi", "pb3"], ["oi"])
                    _tt(S, "pool", sqb[:, :N], oi[:, h, :N], oi[:, h, :N], ALU.mult, ["oi"], ["sqb"])
                    _mm(S, PB[4][:, :N], ones[:], sqb[:, :N], True, True, ["sqb", "ones"], ["pb4"])
                    _act(S, rtmp[:, :N], PB[4][:, :N], AF.Sqrt, ["pb4"], ["rtmp"], scale=1.0 / 128, bias=K.eps_ap)
                    _recip(S, rinv[:, :N], rtmp[:, :N], ["rtmp"], ["rinv"])
                    _stt(S, "dve", otmp[:, :N], oi[:, h, :N], gvec[:, 0:1], rinv[:, :N], ALU.mult, ALU.mult,
                         ["oi", "gvec", "rinv"], ["otmp"])
                    _tt(S, "dve", mix[:, h, :N], otmp[:, :N], sr[:, h, :N], ALU.mult, ["otmp", "sr"], ["mix"])
                kts = list(range(NKT)) if src == "x" else [0, 1]
                _dbg = os.environ.get("MK_P2DBG%d" % li, "")
                if "nogla" in _dbg:
                    _memset(S, "dve", mix[:, 0:4, :N], 0.0, ["mix"])
                if "nomla" in _dbg:
                    _memset(S, "dve", mix[:, 4:8, :N], 0.0, ["mix"])
                for h in (range(4) if "nomla" not in _dbg else []):
                    sbanks = [(PB[0], "pb0"), (PB[1], "pb1"), (PB[7], "pb7")]

                    def qk(ki):
                        ps, pk = sbanks[ki % 3]
                        ksl = slice(kts[ki] * 128, (kts[ki] + 1) * 128)
                        _mm(S, ps[:, :N], KlT[:, ksl], qp[:, h, :N], True, False, ["KlT", "qp"], [pk])
                        _mm(S, ps[:, :N], KrT[:, ksl], qr[:, h, :N], False, True, ["KrT", "qr"], [pk])
                    qk(0)
                    if len(kts) > 1:
                        qk(1)
                    for ki, kt in enumerate(kts):
                        ps, pk = sbanks[ki % 3]
                        if ki + 2 < len(kts):
                            qk(ki + 2)
                        p_, ppk = pT[ki % 3], "pT%d" % (ki % 3)
                        _act(S, p_[:, :N], ps[:, :N], AF.Exp, [pk], [ppk], scale=MLA_SCALE)
                        _mm(S, PB[2][:, :N], Vl[:, kt, :], p_[:, :N], ki == 0, ki == len(kts) - 1, ["Vl", ppk], ["pb2"])
                        _mm(S, PB[6][:, :N], ones[:], p_[:, :N], ki == 0, ki == len(kts) - 1, ["ones", ppk], ["pb6"])
                    _recip(S, rinv[:, :N], PB[6][:, :N], ["pb6"], ["rinv"])
                    _tt(S, "dve", oln[:, :N], PB[2][:, :N], rinv[:, :N], ALU.mult, ["pb2", "rinv"], ["oln"])
                    _mm(S, PB[3][:, :N], wuv[:, h * 128:(h + 1) * 128], oln[:, :N], True, True, ["wuv", "oln"], ["pb3"])
                    _cp(S, "act", mix[:, 4 + h, :N], PB[3][:, :N], ["pb3"], ["mix"])
                g1 = par[:, col, 2, :]
                for dco in range(8):
                    b = 4 + dco % 2
                    ps, pk = PB[b], "pb%d" % b
                    for c in range(8):
                        _mm(S, ps[:, :N], wout[:, c, dco * 128:(dco + 1) * 128], mix[:, c, :N], c == 0, c == 7,
                            ["wout", "mix"], [pk])
                    _stt(S, "dve", xs[:, dco, g0:g0 + N], ps[:, :N], g1[:, dco:dco + 1], xs[:, dco, g0:g0 + N],
                         ALU.mult, ALU.add, [pk, "par", "xs"], ["xs"])
        S.barrier()

        with ExitStack() as es:
            def sb(name, shape, dt):
                return es.enter_context(nc.sbuf_tensor("p2f_%d_%s" % (li, name), shape, dt))
            h2 = sb("h2", [128, 8, NT], BF16)
            sq = [sb("sq%d" % i, [128, 512], BF16) for i in range(2)]
            rstd = sb("rstd", [128, 512], F32)
            rtmp = sb("rtmp", [128, 512], F32)
            htmp = [sb("htmp%d" % i, [128, 512], F32) for i in range(2)]
            if not dense:
                rw = sb("rw", [128, 8, NEXP], F32)
                _dma(S, "sp", rw[:], Dm["router"].rearrange("(dc p) e -> p dc e", p=128), [], ["rw"])
                lg = sb("lg", [128, 32], F32)
                mx8 = sb("mx8", [128, 8], F32)
                nv1 = sb("nv1", [128, 1], F32)
                msk = sb("msk", [128, 8], F32)
                ex = sb("ex", [128, 8], F32)
                ssum = sb("ssum", [128, 1], F32)
                comb = sb("comb", [128, 8], F32)
                cTs = sb("cTs", [8, 512], F32)
            for (src, col, n0, N, g0) in blocks:
                a2 = par[:, col, 4, :]
                sh2 = par[:, col, 3, :]
                for dc in range(8):
                    sqt, sqk = sq[dc % 2], "sq%d" % (dc % 2)
                    _tt(S, "pool", sqt[:, :N], xs[:, dc, g0:g0 + N], xs[:, dc, g0:g0 + N], ALU.mult, ["xs"], [sqk])
                    _mm(S, PB[0][:, :N], ones[:], sqt[:, :N], dc == 0, dc == 7, [sqk, "ones"], ["pb0"])
                _act(S, rtmp[:, :N], PB[0][:, :N], AF.Sqrt, ["pb0"], ["rtmp"], scale=1.0 / D, bias=K.eps_ap)
                _recip(S, rstd[:, :N], rtmp[:, :N], ["rtmp"], ["rstd"])
                for dc in range(8):
                    ht, hk = htmp[dc % 2], "htmp%d" % (dc % 2)
                    _stt(S, "dve", ht[:, :N], xs[:, dc, g0:g0 + N], a2[:, dc:dc + 1], rstd[:, :N], ALU.mult, ALU.mult,
                         ["xs", "par", "rstd"], [hk])
                    if dense:
                        _act(S, h2[:, dc, g0:g0 + N], ht[:, :N], AF.Identity, [hk, "par"], ["h2"], bias=sh2[:, dc:dc + 1])
                    else:
                        _act(S, ht[:, :N], ht[:, :N], AF.Identity, [hk, "par"], [hk], bias=sh2[:, dc:dc + 1])
                        _cp(S, "pool", h2[:, dc, g0:g0 + N], ht[:, :N], [hk], ["h2"])
                        for t in range(N // 128):
                            _mm(S, PB[1 + t][:, 0:8], ht[:, t * 128:(t + 1) * 128], rw[:, dc, :], dc == 0, dc == 7,
                                [hk, "rw"], ["pb%d" % (1 + t)])
                if not dense:
                    for t in range(N // 128):
                        _cp(S, "dve", lg[:, t * 8:(t + 1) * 8], PB[1 + t][:, 0:8], ["pb%d" % (1 + t)], ["lg"])
                    if "dbg_lg" in Dm:
                        _dma(S, "sp", Dm["dbg_lg"][:, (n0 // 512) * 32:(n0 // 512 + 1) * 32], lg[:], ["lg"], ["dbg_lg"])
                    for t in range(N // 128):
                        lsl = slice(t * 8, (t + 1) * 8)
                        S.op("dve", lambda e, lsl=lsl: e.max(out=mx8[:], in_=lg[:, lsl]), reads=["lg"], writes=["mx8"])
                        _ts(S, "dve", nv1[:], mx8[:, 0:1], -1.0, None, ALU.mult, None, ["mx8"], ["nv1"])
                        _ts(S, "dve", msk[:], lg[:, lsl], mx8[:, 1:2], None, ALU.is_ge, None, ["lg", "mx8"], ["msk"])
                        _act(S, ex[:], lg[:, lsl], AF.Exp, ["lg", "nv1"], ["ex"], bias=nv1[:, 0:1])
                        _tt(S, "dve", ex[:], ex[:], msk[:], ALU.mult, ["ex", "msk"], ["ex"])
                        S.op("dve", lambda e: e.reduce_sum(out=ssum[:], in_=ex[:], axis=mybir.AxisListType.X),
                             reads=["ex"], writes=["ssum"])
                        _recip(S, ssum[:], ssum[:], ["ssum"], ["ssum"])
                        _ts(S, "dve", comb[:], ex[:], ssum[:, 0:1], None, ALU.mult, None, ["ex", "ssum"], ["comb"])
                        _mm(S, PB[5][0:8, t * 128:(t + 1) * 128], comb[:], ident, True, True, ["comb", "cst"], ["pb5"])
                    _cp(S, "dve", cTs[:, :N], PB[5][0:8, :N], ["pb5"], ["cTs"])
                    _dma(S, "sp", Dm["combT"][:, n0:n0 + N], cTs[:, :N], ["cTs"], ["combT"])
            nsteps, nfin = [], None
            if K.mode == "FUSED" and li + 1 < DEPTH:
                nl = li + 1
                n_lnT = sb("n_lnT", [128, 16], F32)
                _dma(S, "sp", n_lnT[:], Dm["lnT"][nl], [], ["n_lnT"])
                n_bmodT = sb("n_bmodT", [128, 48], F32)
                _dma(S, "sp", n_bmodT[:], Dm["bmodT"][nl], [], ["n_bmodT"])
                n_cT = sb("n_cT", [128, 8, 2], F32)
                _dma(S, "sp", n_cT[:], Dm["cT"].rearrange("(dc p) c -> p dc c", p=128), [], ["n_cT"])
                n_sT = sb("n_sT", [128, 8, 2], F32)
                _act(S, n_sT[:], n_cT[:], AF.Silu, ["n_cT"], ["n_sT"])
                n_par = sb("n_par", [128, 2, 6, 8], F32)
                nsteps, nfin = mod_stage_steps(K, nl, sb, PB[7], "pb7", n_par, "n_par", n_sT, "n_sT",
                                               n_bmodT, "n_bmodT", n_lnT, "n_lnT")
                K.par_pre.add(nl)
            ybanks = 3 if nsteps else 4
            GS = 4
            wg = [sb("wg%d" % i, [128, 8, GS * 128], BF16) for i in range(2)]
            wu = [sb("wu%d" % i, [128, 8, GS * 128], BF16) for i in range(2)]
            wd = [sb("wd%d" % i, [128, GS, D], BF16) for i in range(2)]
            actb = [sb("actb%d" % i, [128, GS, 512], BF16) for i in range(2)]
            sg = [sb("sg%d" % i, [128, 512], BF16) for i in range(2)]
            ytmp = sb("ytmp", [128, 512], F32)
            if not dense:
                cb = [sb("cb%d" % i, [128, NL], F32) for i in range(2)]
            g2 = None
            nexp = 1 if dense else NEXP
            dff = DFF0 if dense else DFFE
            nch = dff // 128
            gctr = 0
            actr = 0
            for ex_i in (range(nexp) if "noffn" not in os.environ.get("MK_P2DBG%d" % li, "") else []):
                if dense:
                    Wg, Wu, Wd = Dm["ffn_g"], Dm["ffn_u"], Dm["ffn_d"]
                else:
                    Wg, Wu, Wd = Dm["exp_g"][ex_i], Dm["exp_u"][ex_i], Dm["exp_d"][ex_i]
                    cbb, cbk = cb[ex_i % 2], "cb%d" % (ex_i % 2)
                    _dma(S, "sp", cbb[:], Dm["combT"][ex_i:ex_i + 1, :].partition_broadcast(128), ["combT"], [cbk])
                for f0 in range(0, nch, GS):
                    gs = min(GS, nch - f0)
                    wi = gctr % 2
                    gctr += 1
                    kg, ku, kd = "wg%d" % wi, "wu%d" % wi, "wd%d" % wi
                    _dma(S, "pool", wg[wi][:, :, :gs * 128],
                         Wg[:, f0 * 128:(f0 + gs) * 128].rearrange("(dc p) n -> p dc n", p=128), [], [kg])
                    _dma(S, "pool", wu[wi][:, :, :gs * 128],
                         Wu[:, f0 * 128:(f0 + gs) * 128].rearrange("(dc p) n -> p dc n", p=128), [], [ku])
                    _dma(S, "pool", wd[wi][:, :gs, :],
                         Wd[f0 * 128:(f0 + gs) * 128, :].rearrange("(j p) n -> p j n", p=128), [], [kd])
                    for (src, col, n0, N, g0) in blocks:
                        g2 = par[:, col, 5, :]
                        ai = actr % 2
                        actr += 1
                        ak = "actb%d" % ai
                        for j in range(gs):
                            b = j % 2
                            pg, pgk = PB[b], "pb%d" % b
                            pu, puk = PB[2 + b], "pb%d" % (2 + b)
                            for dc in range(8):
                                _mm(S, pg[:, :N], wg[wi][:, dc, j * 128:(j + 1) * 128], h2[:, dc, g0:g0 + N], dc == 0, dc == 7,
                                    [kg, "h2"], [pgk])
                            for dc in range(8):
                                _mm(S, pu[:, :N], wu[wi][:, dc, j * 128:(j + 1) * 128], h2[:, dc, g0:g0 + N], dc == 0, dc == 7,
                                    [ku, "h2"], [puk])
                            _act(S, sg[b][:, :N], pg[:, :N], AF.Silu, [pgk], ["sg%d" % b])
                            _tt(S, "dve", actb[ai][:, j, :N], sg[b][:, :N], pu[:, :N], ALU.mult, ["sg%d" % b, puk], [ak])
                        for dco in range(8):
                            b = 4 + dco % ybanks
                            py, pyk = PB[b], "pb%d" % b
                            for j in range(gs):
                                _mm(S, py[:, :N], wd[wi][:, j, dco * 128:(dco + 1) * 128], actb[ai][:, j, :N], j == 0, j == gs - 1,
                                    [kd, ak], [pyk])
                            if dense:
                                _stt(S, "dve", xs[:, dco, g0:g0 + N], py[:, :N], g2[:, dco:dco + 1], xs[:, dco, g0:g0 + N],
                                     ALU.mult, ALU.add, [pyk, "par", "xs"], ["xs"])
                            else:
                                _tt(S, "dve", ytmp[:, :N], py[:, :N], cbb[:, n0:n0 + N], ALU.mult, [pyk, cbk], ["ytmp"])
                                _stt(S, "dve", xs[:, dco, g0:g0 + N], ytmp[:, :N], g2[:, dco:dco + 1], xs[:, dco, g0:g0 + N],
                                     ALU.mult, ALU.add, ["ytmp", "par", "xs"], ["xs"])
                        if nsteps:
                            nsteps.pop(0)()
            while nsteps:
                nsteps.pop(0)()
            if nfin is not None:
                nfin()
                _dma(S, "sp", Dm["st_par2"], n_par[:], ["n_par"], ["st_par2"])
            if is_last:
                fing = sb("fing", [128, 8], F32)
                _dma(S, "sp", fing[:], Dm["fin_g"], [], ["fing"])
                K.final_dmas = []
                for (src, col, n0, N, g0) in blocks:
                    if src != "x":
                        continue
                    for dc in range(8):
                        sqt, sqk = sq[dc % 2], "sq%d" % (dc % 2)
                        _tt(S, "pool", sqt[:, :N], xs[:, dc, g0:g0 + N], xs[:, dc, g0:g0 + N], ALU.mult, ["xs"], [sqk])
                        _mm(S, PB[0][:, :N], ones[:], sqt[:, :N], dc == 0, dc == 7, [sqk, "ones"], ["pb0"])
                    _act(S, rtmp[:, :N], PB[0][:, :N], AF.Sqrt, ["pb0"], ["rtmp"], scale=1.0 / D, bias=K.eps_ap)
                    _recip(S, rstd[:, :N], rtmp[:, :N], ["rtmp"], ["rstd"])
                    for dc in range(8):
                        ht, hk = htmp[dc % 2], "htmp%d" % (dc % 2)
                        _stt(S, "dve", ht[:, :N], xs[:, dc, g0:g0 + N], fing[:, dc:dc + 1], rstd[:, :N], ALU.mult, ALU.mult,
                             ["xs", "fing", "rstd"], [hk])
                        d = _dma(S, "sp", Dm["outT"][dc * 128:(dc + 1) * 128, n0:n0 + N], ht[:, :N], [hk], ["outT"])
                        K.final_dmas.append(d)
            else:
                K.final_dmas = []
                d = _dma(S, "sp", K.xout.rearrange("(dc p) n -> p dc n", p=128), xs[:, :, 0:NL], ["xs"], [K.xout_key])
                K.final_dmas.append(d)
                d = _dma(S, "sp", K.cout.rearrange("(dc p) n -> p dc n", p=128), xs[:, :, NL:NT], ["xs"], [K.cout_key])
                K.final_dmas.append(d)
        S.barrier()


def build(mode, li):
    nc = bass.Bass("TRN2", target_bir_lowering=False)
    K = Ctx()
    K.nc = nc
    K.S = Sched(nc, same_engine_sync=not os.environ.get('MK_NOSES'))
    K.mode = mode
    K.par_pre = set()
    S = K.S
    _CUR[0] = S
    layers = [li] if mode != "FUSED" else list(range(DEPTH))
    K.nlay = len(layers)
    K.has_dense = any(l % 2 == 0 for l in layers)
    K.has_moe = any(l % 2 == 1 for l in layers)
    K.has_last = (DEPTH - 1) in layers
    make_dram(K, mode)
    Dm = K.dram
    with ExitStack() as es:
        K.pb = [es.enter_context(nc.psum_tensor("pb%d" % i, [128, 512], F32)) for i in range(8)]
        cst = es.enter_context(nc.sbuf_tensor("cst_sb", [128, 640], F32))
        _dma(S, "sp", cst[:], Dm["cst"], [], ["cst"])
        K.cst = cst
        ones_bf = es.enter_context(nc.sbuf_tensor("ones_bf", [128, 128], BF16))
        _memset(S, "dve", ones_bf[:], 1.0, ["ones"])
        K.ones_bf = ones_bf
        ones_f = es.enter_context(nc.sbuf_tensor("ones_f", [128, 128], F32))
        _memset(S, "dve", ones_f[:], 1.0, ["onesf"])
        K.ones_f = ones_f
        cvec = es.enter_context(nc.sbuf_tensor("cvec", [128, 2], F32))
        _memset(S, "dve", cvec[:, 0:1], EPS, ["cvec"])
        _memset(S, "dve", cvec[:, 1:2], 1.0, ["cvec"])
        K.eps_ap = cvec[:, 0:1]
        K.one_ap = cvec[:, 1:2]
        K.final_dmas = []
        if mode == "P1":
            try:
                phase1(K, 0, Dm["xT"], "xT", Dm["ctxT"], "ctxT")
            except _Stop:
                pass
        elif mode == "P2":
            if not K.has_last:
                K.xout, K.xout_key = Dm["xT_out"], "xT_out"
                K.cout, K.cout_key = Dm["ctxT_out"], "ctxT_out"
            phase2(K, li, 0, Dm["xT"], "xT", Dm["ctxT"], "ctxT", K.has_last)
        else:
            K.xout, K.xout_key = Dm["xT_out"], "xT_out"
            K.cout, K.cout_key = Dm["ctxT_out"], "ctxT_out"
            groups = [[0, 1, 2, 3], [4, 5, 6, 7]]
            for l in range(DEPTH):
                if l == 0:
                    xin, xkey, cin, ckey = Dm["xT"], "xT", Dm["ctxT"], "ctxT"
                else:
                    xin, xkey, cin, ckey = Dm["xT_out"], "xT_out", Dm["ctxT_out"], "ctxT_out"
                phase1(K, l, xin, xkey, cin, ckey)
                for nm in ("kl", "kr", "v", "L", "A"):
                    src, dst = Dm["x_" + nm], Dm["g_" + nm]
                    S.coll("pool", lambda e, src=src, dst=dst: e.collective_compute(
                        "AllGather", ALU.bypass, replica_groups=groups, ins=[src.opt()], outs=[dst.opt()]),
                        reads=["x_" + nm], writes=["g_" + nm])
                S.barrier()
                phase2(K, l, l, xin, xkey, cin, ckey, l == DEPTH - 1)
        S.emit()
    return nc


def _bf(a):
    return np.ascontiguousarray(a)


_PERM = np.concatenate([np.arange(16, 32), np.arange(0, 16), np.arange(48, 64), np.arange(32, 48)])


def host_prep(inp):
    f = lambda a: np.ascontiguousarray(np.asarray(a, dtype=np.float32))
    W = {}
    nl = DEPTH
    w_in = f(inp["w_in"])
    W["w_in"] = np.ascontiguousarray(np.concatenate([w_in, w_in[:, :, 1952:2016][:, :, _PERM]], axis=2))
    W["w_mod"] = f(inp["w_mod"])
    W["bmodT"] = np.ascontiguousarray(f(inp["b_mod"]).reshape(nl, 48, 128).transpose(0, 2, 1))
    ln1 = f(inp["ln1_g"]).reshape(nl, 8, 128).transpose(0, 2, 1)
    ln2 = f(inp["ln2_g"]).reshape(nl, 8, 128).transpose(0, 2, 1)
    W["lnT"] = np.ascontiguousarray(np.concatenate([ln1, ln2], axis=2))
    wg2 = np.zeros((nl, 33, 512), np.float32)
    g2 = f(inp["w_gla_g2"])
    b2 = f(inp["b_gla_g2"])
    wg2[:, 0:16, 0:256] = g2[:, 0]
    wg2[:, 16:32, 256:512] = g2[:, 1]
    wg2[:, 32, 0:256] = b2[:, 0]
    wg2[:, 32, 256:512] = b2[:, 1]
    W["wg2"] = wg2
    gvec = np.zeros((nl, 128, 8), np.float32)
    gvec[:, :, 0] = f(inp["gla_norm_g"])
    qn = f(inp["mla_q_norm_g"])
    gvec[:, :, 1] = qn[:, 0:128]
    gvec[:, :, 2] = qn[:, 128:256]
    gvec[:, :, 3] = f(inp["mla_kv_norm_g"])
    W["gvec"] = gvec
    W["kvg_row"] = np.ascontiguousarray(f(inp["mla_kv_norm_g"]).reshape(nl, 1, 128))
    wuq = f(inp["w_uq"])
    wq = np.zeros((nl, 256, 1024), np.float32)
    for h in range(4):
        wq[:, :, h * 128:(h + 1) * 128] = wuq[:, :, 192 * h:192 * h + 128]
        rp = wuq[:, :, 192 * h + 128:192 * h + 192]
        wq[:, :, 512 + h * 64:512 + (h + 1) * 64] = rp
        wq[:, :, 768 + h * 64:768 + (h + 1) * 64] = rp[:, :, _PERM]
    W["w_uq"] = wq
    wukv = f(inp["w_ukv"])
    ukT = np.zeros((nl, 128, 512), np.float32)
    uv = np.zeros((nl, 128, 512), np.float32)
    for h in range(4):
        ukT[:, :, h * 128:(h + 1) * 128] = wukv[:, :, 256 * h:256 * h + 128].transpose(0, 2, 1)
        uv[:, :, h * 128:(h + 1) * 128] = wukv[:, :, 256 * h + 128:256 * h + 256]
    W["w_ukT"] = ukT
    W["w_uv"] = uv
    W["w_out"] = f(inp["w_out"])
    W["ffn_g"] = f(inp["ffn_w_gate"])[0]
    W["ffn_u"] = f(inp["ffn_w_up"])[0]
    W["ffn_d"] = f(inp["ffn_w_down"])[0]
    W["router"] = f(inp["router_w"])[0]
    W["exp_g"] = f(inp["exp_w_gate"])[0]
    W["exp_u"] = f(inp["exp_w_up"])[0]
    W["exp_d"] = f(inp["exp_w_down"])[0]
    W["fin_g"] = np.ascontiguousarray(f(inp["final_norm_g"]).reshape(8, 128).T)
    i = np.arange(128)
    ident = (i[:, None] == i[None, :]).astype(np.float32)
    triF = (i[:, None] <= i[None, :]).astype(np.float32)
    triB = (i[:, None] >= i[None, :]).astype(np.float32)
    triSU = (i[:, None] > i[None, :]).astype(np.float32)
    triSL = (i[:, None] < i[None, :]).astype(np.float32)
    W["cst"] = np.ascontiguousarray(np.concatenate([ident, triF, triB, triSU, triSL], axis=1))
    x = f(inp["x"])
    c = f(inp["c"])
    ctx = f(inp["ctx"])
    cc = f(inp["c_ctx"])
    inv = (np.float32(10000.0) ** (-np.arange(16, dtype=np.float32) / np.float32(16))).astype(np.float32)
    cores = []
    for core in range(8):
        b, j = core // 4, core % 4
        t0 = j * NL
        P = {}
        P["xT"] = np.ascontiguousarray(x[b, t0:t0 + NL].T)
        P["ctxT"] = np.ascontiguousarray(ctx[b].T)
        P["cT"] = np.ascontiguousarray(np.stack([c[b], cc], axis=1))
        pos = np.arange(t0, t0 + NL)
        row = (pos // 64).astype(np.float32)
        colp = (pos % 64).astype(np.float32)
        ar = (inv[:, None] * row[None, :]).astype(np.float32)
        ac = (inv[:, None] * colp[None, :]).astype(np.float32)
        P["ropec"] = np.ascontiguousarray(np.concatenate([np.cos(ar), np.cos(ar), np.cos(ac), np.cos(ac)], axis=0).astype(np.float32))
        P["ropes"] = np.ascontiguousarray(np.concatenate([-np.sin(ar), np.sin(ar), -np.sin(ac), np.sin(ac)], axis=0).astype(np.float32))
        rm = np.zeros((64, 8), np.float32)
        for r in range(4):
            rm[:, r] = 1.0 if r < j else 0.0
            rm[:, 4 + r] = 1.0 if r > j else 0.0
        P["rmask"] = rm
        cores.append(P)
    return W, cores


_LAYER_KEYS = ["w_mod", "bmodT", "lnT", "w_in", "wg2", "gvec", "kvg_row", "w_uq", "w_ukT", "w_uv", "w_out"]
_NC_CACHE = {}


def _get_nc(mode, li):
    key = (mode, li)
    if key not in _NC_CACHE:
        _NC_CACHE[key] = build(mode, li)
    return _NC_CACHE[key]


def _run(mode, li, W, per_core):
    nc = _get_nc(mode, li)
    names = [n for n, ap in _dram_inputs(nc)]
    in_maps = []
    for core in range(8):
        m = {}
        for n in names:
            if n in per_core[core]:
                m[n] = per_core[core][n]
            elif n in _LAYER_KEYS and mode != "FUSED":
                m[n] = W[n][li:li + 1]
            else:
                m[n] = W[n]
        in_maps.append(m)
    ncores = int(os.environ.get("MK_NCORES", "8"))
    res = run_bass_kernel_spmd(nc, in_maps[:ncores], core_ids=list(range(ncores)))
    return res.results


_INPUT_NAMES = {}


def _dram_inputs(nc):
    return [(n, None) for n in _INPUT_NAMES[id(nc)]]


def kernel_unfused(**inp):
    W, cores = host_prep(inp)
    out = np.zeros((2, 8192, D), np.float32)
    state = [dict(xT=cores[i]["xT"], ctxT=cores[i]["ctxT"]) for i in range(8)]
    for li in range(DEPTH):
        pc = []
        for i in range(8):
            m = dict(cores[i])
            m.update(state[i])
            pc.append(m)
        r1 = _run("P1", li, W, pc)
        for i in range(8):
            b = i // 4
            grp = [r1[4 * b + r] for r in range(4)]
            for k in r1[i]:
                if k.startswith("st_") or k.startswith("c_"):
                    pc[i][k] = r1[i][k]
            pc[i]["g_kl"] = np.concatenate([g["x_kl"] for g in grp], axis=0)
            pc[i]["g_kr"] = np.concatenate([g["x_kr"] for g in grp], axis=0)
            pc[i]["g_v"] = np.concatenate([g["x_v"] for g in grp], axis=0)
            pc[i]["g_L"] = np.concatenate([g["x_L"] for g in grp], axis=0)
            pc[i]["g_A"] = np.concatenate([g["x_A"] for g in grp], axis=0)
        r2 = _run("P2", li, W, pc)
        if li == DEPTH - 1:
            for i in range(8):
                b, j = i // 4, i % 4
                out[b, j * NL:(j + 1) * NL, :] = r2[i]["outT"].T
        else:
            for i in range(8):
                state[i] = dict(xT=r2[i]["xT_out"], ctxT=r2[i]["ctxT_out"])
    return out


def kernel(**inp):
    W, cores = host_prep(inp)
    r = _run("FUSED", 0, W, cores)
    out = np.zeros((2, 8192, D), np.float32)
    for i in range(8):
        b, j = i // 4, i % 4
        out[b, j * NL:(j + 1) * NL, :] = r[i]["outT"].T
    return out
```
